# Optimizing a Trainium2 kernel written in Bass

```python
import jax
import jax.numpy as jnp
from jax import lax
import numpy as np

D_MODEL = 1024
BATCH = 8
SEQ = 4096
DEPTH = 4

GRID_W = 64
CTX_LEN = 256
N_MOD = 6

POOL_GROUPS = 4
POOL_WINDOWS = (2, 4, 8, 16)
POOL_WIDTH = D_MODEL // 4
POOL_GROUP_DIM = POOL_WIDTH // POOL_GROUPS

NA_HEAD_DIM = 64
NA_HEADS = (D_MODEL - POOL_WIDTH) // NA_HEAD_DIM
NA_WIDTH = NA_HEADS * NA_HEAD_DIM
WIN_H = 8
WIN_W = 16
COL_BLOCK = 16
COL_BAND = COL_BLOCK + WIN_W
N_COL_BLOCKS = GRID_W // COL_BLOCK
AB_IN = POOL_WIDTH + 3 * NA_WIDTH
AB_OUT = POOL_WIDTH + NA_WIDTH

GLA_HEADS = 4
GLA_DK = D_MODEL // 2 // GLA_HEADS
GLA_DV = D_MODEL // GLA_HEADS
GATE_RANK = 16
GATE_NORM = 16.0
GLA_CHUNK = 64
GLA_QK = GLA_HEADS * GLA_DK
GLA_V = GLA_HEADS * GLA_DV
GLA_IN = 2 * GLA_QK + 2 * GLA_V + 2 * GATE_RANK
ROPE_BASE = 10000.0

N_EXPERTS = 32
TOP_K = 4
D_EXPERT = D_MODEL
SWIGLU_LIMIT = 7.0
SWIGLU_ALPHA = 1.702
MOE_BLOCK = 256

DEEPNORM_ALPHA = (2.0 * DEPTH) ** 0.25
DEEPNORM_BETA = (8.0 * DEPTH) ** -0.25
N_EVEN = (DEPTH + 1) // 2
N_ODD = DEPTH // 2
LN_EPS = 1e-5
RMS_EPS = 1e-6
NEG_INF = -1e30

kernel_name = 'hybrid_pool_natten_gla_moe_dit'


def layer_norm(x, g, b):
    xf = x.astype(jnp.float32)
    mu = jnp.mean(xf, axis=-1, keepdims=True)
    var = jnp.mean(jnp.square(xf - mu), axis=-1, keepdims=True)
    return ((xf - mu) * lax.rsqrt(var + LN_EPS) * g + b).astype(x.dtype)


def multiscale_pool(u):
    T = u.shape[1]
    uf = u.astype(jnp.float32)
    csum = jnp.concatenate([jnp.zeros_like(uf[:, :1]), jnp.cumsum(uf, axis=1)], axis=1)
    w = jnp.array(POOL_WINDOWS, dtype=jnp.int32)
    t = jnp.arange(T)[:, None]
    lo = jnp.clip(t - w // 2, 0, T)
    hi = jnp.clip(t - w // 2 + w, 0, T)
    g = jnp.arange(POOL_GROUPS)[None, :]
    mean = (csum[:, hi, g] - csum[:, lo, g]) / (hi - lo).astype(jnp.float32)[None, :, :, None]
    return (mean - uf).astype(u.dtype)


def axial_rope_tables(T, dim):
    t = jnp.arange(T)
    row = (t // GRID_W).astype(jnp.float32)
    col = (t % GRID_W).astype(jnp.float32)
    nf = dim // 4
    freqs = ROPE_BASE ** (-jnp.arange(nf, dtype=jnp.float32) / nf)
    ang = jnp.concatenate([row[:, None] * freqs, col[:, None] * freqs], axis=-1)
    return jnp.cos(ang), jnp.sin(ang)


def apply_rope(x, cos, sin):
    x1, x2 = jnp.split(x, 2, axis=-1)
    cs = cos[None, :, None, :]
    sn = sin[None, :, None, :]
    return jnp.concatenate([x1 * cs - x2 * sn, x1 * sn + x2 * cs], axis=-1).astype(x.dtype)


def neighborhood_attention(q, k, v, kc, vc, rpb):
    B, T, H, Dh = q.shape
    rows = T // GRID_W
    kh = min(WIN_H, rows)
    scale = Dh ** -0.5

    def grid(a):
        return a.reshape(B, rows, GRID_W, H, Dh).transpose(0, 3, 1, 2, 4)

    qg = grid(q).reshape(B, H, rows, N_COL_BLOCKS, COL_BLOCK, Dh)
    kg, vg = grid(k), grid(v)
    r = jnp.arange(rows)
    row_start = jnp.clip(r - kh // 2, 0, rows - kh)
    key_rows = row_start[:, None] + jnp.arange(kh)
    jb = jnp.arange(N_COL_BLOCKS)
    band_start = jnp.clip(jb * COL_BLOCK - WIN_W // 2, 0, GRID_W - COL_BAND)
    key_cols = band_start[:, None] + jnp.arange(COL_BAND)
    ridx = key_rows[:, :, None, None]
    cidx = key_cols[None, None, :, :]
    kb = kg[:, :, ridx, cidx]
    vb = vg[:, :, ridx, cidx]

    qcol = jb[:, None] * COL_BLOCK + jnp.arange(COL_BLOCK)
    win_start = jnp.clip(qcol - WIN_W // 2, 0, GRID_W - WIN_W)
    kcol = key_cols[:, None, :]
    col_ok = (kcol >= win_start[..., None]) & (kcol < win_start[..., None] + WIN_W)
    dcol = jnp.clip(kcol - qcol[..., None] + WIN_W - 1, 0, 2 * WIN_W - 2)
    drow = key_rows - r[:, None] + WIN_H - 1
    bias = rpb[:, drow[:, None, None, :, None], dcol[None, :, :, None, :]]

    s_band = jnp.einsum('bhrjqd,bhrkjcd->bhrjqkc', qg, kb).astype(jnp.float32) * scale + bias
    s_band = jnp.where(col_ok[:, :, None, :], s_band, NEG_INF)
    s_ctx = jnp.einsum('bhrjqd,bnhd->bhrjqn', qg, kc).astype(jnp.float32) * scale
    nb = kh * COL_BAND
    logits = jnp.concatenate([s_band.reshape(B, H, rows, N_COL_BLOCKS, COL_BLOCK, nb), s_ctx], axis=-1)
    p = jax.nn.softmax(logits, axis=-1).astype(v.dtype)
    p_band = p[..., :nb].reshape(B, H, rows, N_COL_BLOCKS, COL_BLOCK, kh, COL_BAND)
    p_ctx = p[..., nb:]
    o = jnp.einsum('bhrjqkc,bhrkjcd->bhrjqd', p_band, vb) + jnp.einsum('bhrjqn,bnhd->bhrjqd', p_ctx, vc)
    return o.reshape(B, H, rows, GRID_W, Dh).transpose(0, 2, 3, 1, 4).reshape(B, T, H * Dh)


def context_self_attention(q, k, v):
    B, N, H, Dh = q.shape
    s = jnp.einsum('bnhd,bmhd->bhnm', q, k).astype(jnp.float32) * Dh ** -0.5
    p = jax.nn.softmax(s, axis=-1).astype(v.dtype)
    return jnp.einsum('bhnm,bmhd->bnhd', p, v).reshape(B, N, H * Dh)


def pool_na_mixer(hx, hc, w_in, pool_w, pool_scale, rpb, w_out, with_ctx_out):
    def split_heads(p):
        u = p[..., :POOL_WIDTH]
        q, k, v = jnp.split(p[..., POOL_WIDTH:], 3, axis=-1)
        shp = p.shape[:2] + (NA_HEADS, NA_HEAD_DIM)
        return u, q.reshape(shp), k.reshape(shp), v.reshape(shp)

    def pool_branch(u):
        ug = u.reshape(u.shape[:2] + (POOL_GROUPS, POOL_GROUP_DIM))
        y = jnp.einsum('btgd,gde->btge', multiscale_pool(ug), pool_w)
        return y.reshape(u.shape) * pool_scale

    ux, qx, kx, vx = split_heads(hx @ w_in)
    uc, qc, kc, vc = split_heads(hc @ w_in)
    ax = neighborhood_attention(qx, kx, vx, kc, vc, rpb)
    ox = jnp.concatenate([pool_branch(ux), ax], axis=-1) @ w_out
    if not with_ctx_out:
        return ox, None
    ac = context_self_attention(qc, kc, vc)
    oc = jnp.concatenate([pool_branch(uc), ac], axis=-1) @ w_out
    return ox, oc


def gla_chunked(q, k, v, log_a, s0):
    B, H, T, dk = q.shape
    dv = v.shape[-1]
    n = T // GLA_CHUNK

    def chunks(a):
        return a.reshape(B, H, n, GLA_CHUNK, a.shape[-1]).transpose(2, 0, 1, 3, 4)

    lower = jnp.tril(jnp.ones((GLA_CHUNK, GLA_CHUNK), dtype=bool))[:, :, None]

    def step(S, inp):
        qc, kc, vc, gc = inp
        b = jnp.cumsum(gc.astype(jnp.float32), axis=2)
        b_last = b[:, :, -1:, :]
        o_inter = jnp.einsum('bhtd,bhde->bhte', qc * jnp.exp(b), S)
        diff = b[:, :, :, None, :] - b[:, :, None, :, :]
        decay = jnp.where(lower, jnp.exp(jnp.where(lower, diff, 0.0)), 0.0)
        att = jnp.einsum('bhtd,bhsd,bhtsd->bhts', qc, kc, decay)
        o = o_inter + jnp.einsum('bhts,bhse->bhte', att, vc)
        S_new = S * jnp.exp(b_last)[:, :, 0, :, None] + jnp.einsum('bhsd,bhse->bhde', kc * jnp.exp(b_last - b), vc)
        return S_new, o

    S, o = lax.scan(step, s0, (chunks(q), chunks(k), chunks(v), chunks(log_a)))
    return o.transpose(1, 2, 0, 3, 4).reshape(B, H, T, dv).astype(v.dtype), S


def bidir_gla_mixer(hx, hc, w_in, w_gate, b_gate, norm_g, w_out, cos, sin, with_ctx_out):
    def project(h, rotary):
        B, T, _ = h.shape
        p = h @ w_in
        q, k, v, r, g = jnp.split(p, [GLA_QK, 2 * GLA_QK, 2 * GLA_QK + GLA_V, 2 * GLA_QK + 2 * GLA_V], axis=-1)
        q = q.reshape(B, T, GLA_HEADS, GLA_DK)
        k = k.reshape(B, T, GLA_HEADS, GLA_DK)
        if rotary:
            q = apply_rope(q, cos, sin)
            k = apply_rope(k, cos, sin)
        q = q * GLA_DK ** -0.5
        v = v.reshape(B, T, GLA_HEADS, GLA_DV)
        g = g.reshape(B, T, 2, GATE_RANK)
        la = jax.nn.log_sigmoid((jnp.einsum('btzr,zre->btze', g, w_gate) + b_gate).astype(jnp.float32)) / GATE_NORM
        la = la.reshape(B, T, 2, GLA_HEADS, GLA_DK)
        heads = lambda a: a.transpose(0, 2, 1, 3)
        return heads(q), heads(k), heads(v), r, heads(la[:, :, 0]), heads(la[:, :, 1])

    def flip(a):
        return jnp.flip(a, axis=2)

    def output(o, r):
        B, H, T, dv = o.shape
        of = o.astype(jnp.float32)
        of = of * lax.rsqrt(jnp.mean(of * of, axis=-1, keepdims=True) + RMS_EPS) * norm_g
        of = of.transpose(0, 2, 1, 3).reshape(B, T, H * dv)
        return (of.astype(r.dtype) * jax.nn.silu(r)) @ w_out

    qc, kc, vc, rc, af_c, ab_c = project(hc, False)
    qx, kx, vx, rx, af_x, ab_x = project(hx, True)
    B = hx.shape[0]
    zero = jnp.zeros((B, GLA_HEADS, GLA_DK, GLA_DV), jnp.float32)
    of_c, s_f = gla_chunked(qc, kc, vc, af_c, zero)
    ob_c, s_b = gla_chunked(flip(qc), flip(kc), flip(vc), flip(ab_c), zero)
    of_x, _ = gla_chunked(qx, kx, vx, af_x, s_f)
    ob_x, _ = gla_chunked(flip(qx), flip(kx), flip(vx), flip(ab_x), s_b)
    ox = output(of_x + flip(ob_x), rx)
    if not with_ctx_out:
        return ox, None
    oc = output(of_c + flip(ob_c), rc)
    return ox, oc


def moe_ffn(h, router_w, router_b, w1, b1, w2, b2):
    lead = h.shape[:-1]
    d = h.shape[-1]
    xf = h.reshape(-1, d)
    n_tok = xf.shape[0]
    m = n_tok * TOP_K
    logits = (xf @ router_w + router_b).astype(jnp.float32)
    top_logit, top_idx = lax.top_k(logits, TOP_K)
    gates = jax.nn.softmax(top_logit, axis=-1).reshape(m)
    flat_e = top_idx.reshape(m)
    order = jnp.argsort(flat_e)
    e_sorted = flat_e[order]
    tok_sorted = order // TOP_K
    sizes = jnp.bincount(flat_e, length=N_EXPERTS)
    starts = jnp.cumsum(sizes) - sizes
    padded = (sizes + MOE_BLOCK - 1) // MOE_BLOCK * MOE_BLOCK
    pad_ends = jnp.cumsum(padded)
    pad_starts = pad_ends - padded
    dest = pad_starts[e_sorted] + jnp.arange(m) - starts[e_sorted]
    n_blocks = (m + N_EXPERTS * (MOE_BLOCK - 1)) // MOE_BLOCK + 1
    x_pad = jnp.zeros((n_blocks * MOE_BLOCK, d), xf.dtype).at[dest].set(xf[tok_sorted])
    block_start = jnp.arange(n_blocks) * MOE_BLOCK
    block_expert = jnp.minimum(jnp.searchsorted(pad_ends, block_start, side='right'), N_EXPERTS - 1)

    def expert_block(args):
        xb, e = args
        hid = xb @ w1[e] + b1[e]
        x_glu, x_lin = jnp.split(hid, 2, axis=-1)
        x_glu = jnp.minimum(x_glu, SWIGLU_LIMIT)
        x_lin = jnp.clip(x_lin, -SWIGLU_LIMIT, SWIGLU_LIMIT)
        act = x_glu * jax.nn.sigmoid(SWIGLU_ALPHA * x_glu) * (x_lin + 1.0)
        return act @ w2[e] + b2[e]

    y_pad = lax.map(expert_block, (x_pad.reshape(n_blocks, MOE_BLOCK, d), block_expert))
    y_sorted = y_pad.reshape(n_blocks * MOE_BLOCK, d)[dest] * gates[order][:, None]
    y = jax.ops.segment_sum(y_sorted, tok_sorted, num_segments=n_tok)
    return y.reshape(lead + (d,)).astype(h.dtype)


def setup_inputs(seed: int = 0) -> dict:
    key = jax.random.key(seed)
    ks = jax.random.split(key, 24)
    D = D_MODEL

    def nrm(k, shape, scale):
        return jax.random.normal(k, shape, jnp.float32) * scale

    return {
        'x': nrm(ks[0], (BATCH, SEQ, D), 1.0),
        'c': nrm(ks[1], (BATCH, D), 1.0),
        'ctx': nrm(ks[2], (BATCH, CTX_LEN, D), 1.0),
        'c_ctx': nrm(ks[3], (D,), 1.0),
        'ada_w': nrm(ks[4], (DEPTH, D, N_MOD * D), 0.5 * D ** -0.5),
        'ada_b': nrm(ks[5], (DEPTH, N_MOD * D), 0.02),
        'ln_g': 1.0 + nrm(ks[6], (DEPTH, 2, D), 0.02),
        'ln_b': nrm(ks[7], (DEPTH, 2, D), 0.02),
        'ab_w_in': nrm(ks[8], (N_EVEN, D, AB_IN), D ** -0.5),
        'ab_pool_w': nrm(ks[9], (N_EVEN, POOL_GROUPS, POOL_GROUP_DIM, POOL_GROUP_DIM), POOL_GROUP_DIM ** -0.5),
        'ab_pool_scale': 1.0 + nrm(ks[10], (N_EVEN, POOL_WIDTH), 0.1),
        'ab_rpb': nrm(ks[11], (N_EVEN, NA_HEADS, 2 * WIN_H - 1, 2 * WIN_W - 1), 0.1),
        'ab_w_out': nrm(ks[12], (N_EVEN, AB_OUT, D), DEEPNORM_BETA * AB_OUT ** -0.5),
        'gla_w_in': nrm(ks[13], (N_ODD, D, GLA_IN), D ** -0.5),
        'gla_w_gate': nrm(ks[14], (N_ODD, 2, GATE_RANK, GLA_QK), GATE_RANK ** -0.5),
        'gla_b_gate': nrm(ks[15], (N_ODD, 2, GLA_QK), 0.5),
        'gla_norm_g': 1.0 + nrm(ks[16], (N_ODD, GLA_DV), 0.02),
        'gla_w_out': nrm(ks[17], (N_ODD, GLA_V, D), DEEPNORM_BETA * GLA_V ** -0.5),
        'router_w': nrm(ks[18], (DEPTH, D, N_EXPERTS), D ** -0.5),
        'router_b': nrm(ks[19], (DEPTH, N_EXPERTS), 0.01),
        'exp_w1': nrm(ks[20], (DEPTH, N_EXPERTS, D, 2 * D_EXPERT), D ** -0.5),
        'exp_b1': nrm(ks[21], (DEPTH, N_EXPERTS, 2 * D_EXPERT), 0.01),
        'exp_w2': nrm(ks[22], (DEPTH, N_EXPERTS, D_EXPERT, D), DEEPNORM_BETA * D_EXPERT ** -0.5),
        'exp_b2': nrm(ks[23], (DEPTH, N_EXPERTS, D), 0.01),
    }


def reference(x, c, ctx, c_ctx, ada_w, ada_b, ln_g, ln_b, ab_w_in, ab_pool_w, ab_pool_scale, ab_rpb,
              ab_w_out, gla_w_in, gla_w_gate, gla_b_gate, gla_norm_g, gla_w_out, router_w, router_b,
              exp_w1, exp_b1, exp_w2, exp_b2):
    T = x.shape[1]
    n_ctx = ctx.shape[1]
    cos, sin = axial_rope_tables(T, GLA_DK)
    sc = jax.nn.silu(c)
    scc = jax.nn.silu(c_ctx)
    for i in range(DEPTH):
        last = i == DEPTH - 1
        j = i // 2
        sh1, s1, g1, sh2, s2, g2 = jnp.split((sc @ ada_w[i] + ada_b[i])[:, None, :], N_MOD, axis=-1)
        ch1, cs1, cg1, ch2, cs2, cg2 = jnp.split(scc @ ada_w[i] + ada_b[i], N_MOD, axis=-1)
        hx = x * (1.0 + s1) + sh1
        hc = ctx * (1.0 + cs1) + ch1
        if i % 2 == 0:
            ox, oc = pool_na_mixer(hx, hc, ab_w_in[j], ab_pool_w[j], ab_pool_scale[j], ab_rpb[j],
                                   ab_w_out[j], not last)
        else:
            ox, oc = bidir_gla_mixer(hx, hc, gla_w_in[j], gla_w_gate[j], gla_b_gate[j], gla_norm_g[j],
                                     gla_w_out[j], cos, sin, not last)
        x = layer_norm(DEEPNORM_ALPHA * x + g1 * ox, ln_g[i, 0], ln_b[i, 0])
        hx = x * (1.0 + s2) + sh2
        if last:
            y = moe_ffn(hx, router_w[i], router_b[i], exp_w1[i], exp_b1[i], exp_w2[i], exp_b2[i])
            x = layer_norm(DEEPNORM_ALPHA * x + g2 * y, ln_g[i, 1], ln_b[i, 1])
        else:
            ctx = layer_norm(DEEPNORM_ALPHA * ctx + cg1 * oc, ln_g[i, 0], ln_b[i, 0])
            hc = ctx * (1.0 + cs2) + ch2
            y = moe_ffn(jnp.concatenate([hc, hx], axis=1), router_w[i], router_b[i], exp_w1[i], exp_b1[i],
                        exp_w2[i], exp_b2[i])
            x = layer_norm(DEEPNORM_ALPHA * x + g2 * y[:, n_ctx:], ln_g[i, 1], ln_b[i, 1])
            ctx = layer_norm(DEEPNORM_ALPHA * ctx + cg2 * y[:, :n_ctx], ln_g[i, 1], ln_b[i, 1])
    return x
```

```python
import contextlib
import numpy as np
import concourse.bass as bass
import concourse.mybir as mybir
from concourse.bass_utils import run_bass_kernel_spmd

F32 = mybir.dt.float32
BF16 = mybir.dt.bfloat16
AF = mybir.ActivationFunctionType
ALU = mybir.AluOpType
AX = mybir.AxisListType

D = 1024
TL = 4096
NCTX = 256
T = TL + NCTX
NT = T // 128
NTL = TL // 128
DEPTH = 4
ALPHA = (2.0 * DEPTH) ** 0.25
NE = 32
GW = 64
BLK = 512
NBLK = (T * 4 + NE * (BLK - 1)) // BLK + 1
I32 = mybir.dt.int32

SAME_ENGINE_SYNC = True


class Buf:
    __slots__ = ("name", "t", "w", "r", "dsem", "dcnt", "excl")

    def __init__(self, name, t):
        self.excl = False
        self.name = name
        self.t = t
        self.w = None
        self.r = {}
        self.dsem = None
        self.dcnt = 0

    def __getitem__(self, k):
        return self.t[k]


class Eng:
    def __init__(self, name, h, sem):
        self.name = name
        self.h = h
        self.sem = sem
        self.cnt = 0
        self.seen = {}


class Sched:
    def __init__(self, nc):
        self.nc = nc
        self.es = contextlib.ExitStack()
        self.nsem = 0
        self.dma_latest = {}
        self.free_dsems = []
        self.free_dsems_sw = []
        self.sw_sems = set()
        self.scope_bufs = [[]]
        self.pe = self._eng("pe", nc.tensor)
        self.act = self._eng("act", nc.scalar)
        self.dve = self._eng("dve", nc.vector)
        self.pool = self._eng("pool", nc.gpsimd)
        self.sp = self._eng("sp", nc.sync)
        self.engs = [self.pe, self.act, self.dve, self.pool, self.sp]
        self.out_toks = []
        self.n_ins = 0

    def new_sem(self, name):
        self.nsem += 1
        return self.es.enter_context(self.nc.semaphore(f"{name}_{self.nsem}"))

    def _eng(self, name, h):
        return Eng(name, h, self.new_sem("e" + name))

    def sbuf(self, name, shape, dtype):
        st = self.scope_stacks[-1] if getattr(self, "scope_stacks", None) else self.es
        self.nbuf = getattr(self, "nbuf", 0) + 1
        t = st.enter_context(self.nc.sbuf_tensor(f"{name}_{self.nbuf}", list(shape), dtype))
        b = Buf(name, t)
        self.scope_bufs[-1].append(b)
        return b

    def psum(self, name, shape, dtype):
        t = self.es.enter_context(self.nc.psum_tensor(name, list(shape), dtype))
        b = Buf(name, t)
        b.excl = True
        return b

    def dram(self, name, shape, dtype):
        t = self.nc.dram_tensor(name, list(shape), dtype, kind="Internal")
        return t.ap()

    def view(self, name, ap):
        return Buf(name, ap)

    @contextlib.contextmanager
    def scope(self):
        if not getattr(self, "scope_stacks", None):
            self.scope_stacks = []
        st = contextlib.ExitStack()
        self.scope_stacks.append(st)
        self.scope_bufs.append([])
        try:
            yield
        finally:
            self.barrier()
            for b in self.scope_bufs.pop():
                if b.dsem is not None:
                    for sw, ent in b.dsem.items():
                        (self.free_dsems_sw if sw else self.free_dsems).append((ent[0], ent[1]))
                    b.dsem = None
            self.scope_stacks.pop()
            st.close()

    def _wait(self, eng, sem, val):
        if sem in self.dma_latest:
            val = max(val, self.dma_latest[sem])
        if sem is eng.sem and not SAME_ENGINE_SYNC:
            return
        if eng.seen.get(sem, 0) >= val:
            return
        eng.h.wait_ge(sem, val)
        eng.seen[sem] = val
        self.n_ins += 1

    def _deps(self, eng, reads, writes, skip_sem=None):
        for b in reads:
            if b.w is not None:
                self._wait(eng, *b.w)
        for b in writes:
            if b.w is not None and b.w[0] is not skip_sem:
                self._wait(eng, *b.w)
            for s, v in b.r.items():
                self._wait(eng, s, v)

    def _commit(self, tok, reads, writes):
        for b in writes:
            b.w = tok
            b.r = {}
        s, v = tok
        for b in reads:
            if b in writes:
                continue
            if b.r.get(s, 0) < v:
                b.r[s] = v

    def op(self, eng, fn, reads=(), writes=()):
        ex = [b for b in reads if b.excl and b not in writes]
        if ex:
            writes = list(writes) + ex
            reads = [b for b in reads if not b.excl]
        self._deps(eng, reads, writes)
        ins = fn(eng.h)
        eng.cnt += 1
        ins.then_inc(eng.sem, 1)
        self.n_ins += 1
        self._commit((eng.sem, eng.cnt), reads, writes)

    def dma(self, q, out_ap, in_ap, sb, reads=(), writes=(), is_output=False, **kw):
        sw = q is self.pool
        cur = sb.dsem[sw][0] if (sb.dsem and sw in sb.dsem) else None
        self._deps(q, reads, writes, skip_sem=cur if sb in writes else None)
        sem, cnt = self.dma_sem(sb, sw)
        q.h.dma_start(out=out_ap, in_=in_ap, **kw).then_inc(sem, 16)
        self.n_ins += 1
        tok = (sem, cnt)
        self._commit(tok, reads, writes)
        if is_output:
            self.out_toks.append(tok)

    def dma_sem(self, sb, sw):
        if sb.dsem is None:
            sb.dsem = {}
        if sw not in sb.dsem:
            fl = self.free_dsems_sw if sw else self.free_dsems
            if fl:
                sb.dsem[sw] = list(fl.pop())
            else:
                sb.dsem[sw] = [self.new_sem("dsw" if sw else "d"), 0]
                if sw:
                    self.sw_sems.add(sb.dsem[sw][0])
        ent = sb.dsem[sw]
        ent[1] += 16
        self.dma_latest[ent[0]] = ent[1]
        return ent[0], ent[1]

    def idma(self, out_ap, in_ap, sb, idx_ap, scatter, bound, reads=(), writes=()):
        q = self.pool
        cur = sb.dsem[True][0] if (sb.dsem and True in sb.dsem) else None
        self._deps(q, reads, writes, skip_sem=cur if sb in writes else None)
        sem, cnt = self.dma_sem(sb, True)
        off = bass.IndirectOffsetOnAxis(ap=idx_ap, axis=0)
        q.h.indirect_dma_start(out=out_ap, out_offset=off if scatter else None, in_=in_ap,
                               in_offset=None if scatter else off).then_inc(sem, 16)
        self.n_ins += 1
        self._commit((sem, cnt), reads, writes)

    def barrier(self):
        for e in self.engs:
            for f in self.engs:
                if f is not e and f.cnt > 0:
                    self._wait(e, f.sem, f.cnt)
            for s, v in self.dma_latest.items():
                self._wait(e, s, v)

    def finish(self):
        self.barrier()

    def close(self):
        self.es.close()


class LazyInputs(dict):
    def __init__(self, prog):
        super().__init__()
        self.prog = prog

    def __missing__(self, name):
        shape = list(self.prog.shapes[name])
        dev = self.prog.cfg.get("dev_slice")
        if dev and name in dev:
            shape = list(dev[name])
        ap = self.prog.nc.dram_tensor(name, shape, F32, kind="ExternalInput").ap()
        self[name] = ap
        return ap


class Prog:
    def __init__(self, cfg):
        self.cfg = cfg
        nc = self.nc = bass.Bass("TRN2", target_bir_lowering=False)
        S = self.S = Sched(nc)
        self.taps = {}

        self.shapes = {
            "xc": [T, D], "cvec": [128, 8, 2], "ada_w": [DEPTH, D, 6 * D], "ada_b": [DEPTH, 6 * D],
            "ln_g": [DEPTH, 2, D], "ln_b": [DEPTH, 2, D], "router_w": [DEPTH, D, NE], "router_b": [DEPTH, NE],
            "exp_w1": [DEPTH, NE, D, 2 * D], "exp_b1L": [DEPTH, 128, NE, 16], "exp_w2": [DEPTH, NE, D, D],
            "exp_b2": [DEPTH, NE, D], "ident": [128, 128],
            "ab_w_in": [2, D, 2560], "ab_pool_w": [2, 4, 64, 64], "ab_pool_scale": [2, 256],
            "ab_w_out": [2, D, D], "nabias": [2, 5, 12, 128, 576], "poolA": [20, 128, 128],
            "gla_w_in": [2, D, 3104], "gla_w_gate": [2, 2, 16, 512], "gla_bgL": [2, 128, 2, 4],
            "gla_norm_g": [2, 256], "gla_w_out": [2, D, D], "ropecs": [TL, 2, 64], "trimask": [3, 128, 128], "blkstart": [128, 1], "rowbase": [128, 9], "exp_b1T": [DEPTH, NE * 128, 16],
        }
        self.I = LazyInputs(self)
        self.out = nc.dram_tensor("out", [TL, D], F32, kind="ExternalOutput").ap()

        self.X = S.dram("X", [T, D], F32)
        self.Xt = [S.view(f"X{i}", self.X[i * 128:(i + 1) * 128, :]) for i in range(NT)]
        self.XCt = [S.view(f"XC{i}", self.I["xc"][i * 128:(i + 1) * 128, :]) for i in range(NT)]
        self.OUTt = [S.view(f"O{i}", self.out[i * 128:(i + 1) * 128, :]) for i in range(NTL)]
        self.MOD = S.dram("MOD", [DEPTH, 2, 6 * D], F32)
        self.MODb = [S.view(f"MOD{l}", self.MOD[l]) for l in range(DEPTH)]
        self.src_is_input = True
        self.QT = S.dram("QT", [768, T], BF16)
        self.KT = S.dram("KT", [768, T], BF16)
        self.V = S.dram("V", [T, 1024], BF16)
        self.U = S.dram("U", [T, 1024], BF16)
        self.XS = S.dram("XS", [NBLK * BLK, D], BF16)
        self.XSb = [S.view(f"XS{b}", self.XS) for b in range(NBLK)]
        self.XSall = S.view("XSall", self.XS)
        self.YS = S.dram("YS", [NBLK * BLK, D], F32)
        self.YSb = [S.view(f"YS{b}", self.YS) for b in range(NBLK)]
        self.HB = S.dram("HB", [T, D], BF16)
        self.HBt = [S.view(f"HB{i}", self.HB) for i in range(NT)]
        self.EB = S.dram("EB", [128, 1], I32)
        self.EBb = S.view("EBb", self.EB)
        self.xs_zeroed = False
        self.OF = S.dram("OF", [2, T, D], F32)
        self.OFt = [[S.view(f"OF{z}_{i}", self.OF) for i in range(NT)] for z in range(2)]
        self.GS = S.dram("GS", [2, 16, T], F32)
        self.GSs = [S.view(f"GS{i}", self.GS) for i in range(9)]
        self.QTs = [S.view(f"QT{i}", self.QT) for i in range(9)]
        self.KTs = [S.view(f"KT{i}", self.KT) for i in range(9)]
        self.Vt = [S.view(f"V{i}", self.V) for i in range(NT)]
        self.Ut = [S.view(f"U{i}", self.U) for i in range(NT)]

        self.idf = S.sbuf("idf", [128, 128], F32)
        self.idb = S.sbuf("idb", [128, 128], BF16)
        S.dma(S.sp, self.idf[:], self.I["ident"], self.idf, writes=[self.idf])
        S.op(S.dve, lambda e: e.tensor_copy(self.idb[:], self.idf[:]), reads=[self.idf], writes=[self.idb])
        self.ps = [S.psum(f"ps{i}", [128, 512], F32) for i in range(6)]
        self.pb = [S.psum(f"pb{i}", [128, 1024], BF16) for i in range(2)]

    def tap(self, name, shape, dt=F32):
        ap = self.nc.dram_tensor(name, list(shape), dt, kind="ExternalOutput").ap()
        self.taps[name] = ap
        return ap

    def xsrc(self, i):
        return self.XCt[i] if self.src_is_input else self.Xt[i]

    def adaln(self, layers):
        S, I = self.S, self.I
        with S.scope():
            cv = S.sbuf("cv", [128, 8, 2], F32)
            sc = S.sbuf("sc", [128, 8, 2], F32)
            S.dma(S.sp, cv[:], I["cvec"], cv, writes=[cv])
            S.op(S.act, lambda e: e.activation(sc[:], cv[:], AF.Silu), reads=[cv], writes=[sc])
            wts = [S.sbuf(f"adw{i}", [128, 8, 512], F32) for i in range(2)]
            bts = [S.sbuf(f"adb{i}", [2, 512], F32) for i in range(2)]
            msb = S.sbuf("msb", [2, 6 * D], F32)
            n = 0
            for l in layers:
                wv = I["ada_w"][l].rearrange("(c p) n -> p c n", p=128)
                for blk in range(12):
                    wt, bt = wts[n % 2], bts[n % 2]
                    ps = self.ps[n % 2]
                    n += 1
                    cs = slice(blk * 512, (blk + 1) * 512)
                    S.dma(S.sp, wt[:], wv[:, :, cs], wt, writes=[wt])
                    S.dma(S.sp, bt[:], I["ada_b"][l, cs].partition_broadcast(2), bt, writes=[bt])

                    def mm(e, wt=wt, ps=ps):
                        for k in range(8):
                            ins = e.matmul(ps[0:2, :], sc[:, k, :], wt[:, k, :], start=(k == 0), stop=(k == 7))
                        return ins
                    S.op(S.pe, mm, reads=[sc, wt], writes=[ps])
                    S.op(S.dve, lambda e, ps=ps, bt=bt, cs=cs: e.tensor_tensor(msb[:, cs], ps[0:2, :], bt[:], ALU.add),
                         reads=[ps, bt], writes=[msb])
                for j in (1, 4):
                    S.op(S.dve, lambda e, j=j: e.tensor_scalar_add(msb[:, j * D:(j + 1) * D], msb[:, j * D:(j + 1) * D], 1.0),
                         reads=[msb], writes=[msb])
                S.dma(S.sp, self.MOD[l], msb[:], msb, reads=[msb], writes=[self.MODb[l]])

    def load_mod(self, dst, l, which, j):
        S = self.S
        S.dma(S.sp, dst[:], self.MOD[l, which, j * D:(j + 1) * D].partition_broadcast(128), dst,
              reads=[self.MODb[l]], writes=[dst])

    def load_vec(self, dst, ap1d):
        S = self.S
        S.dma(S.sp, dst[:], ap1d.partition_broadcast(128), dst, writes=[dst])

    def layernorm(self, z, o, lng, lnb, sm):
        S = self.S

        S.op(S.dve, lambda e: e.bn_stats(sm[:, 0:6], z[:, 0:512]), reads=[z], writes=[sm])
        S.op(S.dve, lambda e: e.bn_stats(sm[:, 6:12], z[:, 512:1024]), reads=[z], writes=[sm])
        S.op(S.dve, lambda e: e.bn_aggr(sm[:, 12:14], sm[:, 0:12]), reads=[sm], writes=[sm])
        S.op(S.dve, lambda e: e.tensor_scalar_add(sm[:, 14:15], sm[:, 13:14], 1e-5), reads=[sm], writes=[sm])
        S.op(S.act, lambda e: e.sqrt(sm[:, 14:15], sm[:, 14:15]), reads=[sm], writes=[sm])
        S.op(S.dve, lambda e: e.reciprocal(sm[:, 14:15], sm[:, 14:15]), reads=[sm], writes=[sm])
        S.op(S.dve, lambda e: e.tensor_scalar(sm[:, 15:16], sm[:, 12:13], sm[:, 14:15], -1.0, ALU.mult, ALU.mult),
             reads=[sm], writes=[sm])
        S.op(S.act, lambda e: e.activation(o[:], z[:], AF.Identity, bias=sm[:, 15:16], scale=sm[:, 14:15]),
             reads=[z, sm], writes=[o])
        S.op(S.dve, lambda e: e.tensor_tensor(o[:], o[:], lng[:], ALU.mult), reads=[o, lng], writes=[o])
        S.op(S.dve, lambda e: e.tensor_tensor(o[:], o[:], lnb[:], ALU.add), reads=[o, lnb], writes=[o])

    def ffn(self, l, last):
        S, I = self.S, self.I
        if self.cfg.get("moe", "sparse") == "sparse":
            self.ffn_sparse(l, last)
            self.src_is_input = False
            return
        ntiles = NTL if last else NT
        groups = []
        t = 0
        while t < ntiles:
            groups.append((t, min(t + 12, ntiles)))
            t += 12
        for (t0, t1) in groups:
            self.ffn_group(l, last, t0, t1)
        self.src_is_input = False

    def ffn_sparse(self, l, last):
        S, I = self.S, self.I
        ntile = NTL if last else NT
        nblk = (ntile * 128 * 4 + NE * (BLK - 1)) // BLK + 1
        lw = 0 if "exp_w1" in (self.cfg.get("dev_slice") or {}) else l
        with S.scope():
            G = S.sbuf("G", [128, ntile, NE], F32)
            G4 = S.sbuf("G4", [128, ntile, 4], F32)
            D4 = S.sbuf("D4", [128, ntile * 4], I32)
            IDX = S.sbuf("IDX", [128, 128 * 8], I32)
            IDXB = S.sbuf("IDXB", [128, 128], I32)
            if not self.xs_zeroed:
                self.xs_zeroed = True
                with S.scope():
                    zt = S.sbuf("zt", [128, 4, D], BF16)
                    S.op(S.dve, lambda e: e.memset(zt[:], 0.0), writes=[zt])
                    for b in range(NBLK):
                        S.dma(S.sp, self.XS[b * BLK:(b + 1) * BLK, :].rearrange("(c p) d -> p c d", p=128), zt[:], zt, reads=[zt], writes=[self.XSb[b]])
            with S.scope():
                MK = S.sbuf("MK", [128, ntile, NE], F32)
                RK = S.sbuf("RK", [128, ntile, NE], F32)
                s2 = S.sbuf("s2", [128, D], F32)
                h2 = S.sbuf("h2", [128, D], F32)
                rw = S.sbuf("rw", [128, 8, NE], F32)
                rb = S.sbuf("rb", [128, NE], F32)
                tri = S.sbuf("tri", [128, 128], F32)
                ustr = S.sbuf("ustr", [128, 128], BF16)
                onesb = S.sbuf("onesb", [128, 128], BF16)
                onesf = S.sbuf("onesf", [128, NE], F32)
                blk = S.sbuf("blk", [128, 1], F32)
                cnt = S.sbuf("cnt", [128, NE], F32)
                xb = [S.sbuf(f"xb{i}", [128, D], F32) for i in range(2)]
                h32 = [S.sbuf(f"h32{i}", [128, D], F32) for i in range(2)]
                hbs = [S.sbuf(f"hbs{i}", [128, D], BF16) for i in range(2)]
                hT32s = [S.sbuf(f"hT32{i}", [128, 8, 128], F32) for i in range(2)]
                sms_ = [S.sbuf(f"smr{i}", [128, 160], F32) for i in range(2)]
                mkbs = [S.sbuf(f"mkb{i}", [128, NE], BF16) for i in range(2)]
                sm = sms_[0]
                S.dma(S.sp, rw[:], I["router_w"][l].rearrange("(c p) n -> p c n", p=128), rw, writes=[rw])
                self.load_vec(rb, I["router_b"][l])
                S.dma(S.sp, tri[:], I["trimask"][2], tri, writes=[tri])
                S.dma(S.sp, blk[:], I["blkstart"], blk, writes=[blk])
                S.op(S.dve, lambda e: e.tensor_copy(ustr[:], tri[:]), reads=[tri], writes=[ustr])
                S.op(S.dve, lambda e: e.memset(onesb[:], 1.0), writes=[onesb])
                S.op(S.dve, lambda e: e.memset(onesf[:], 1.0), writes=[onesf])
                S.op(S.dve, lambda e: e.memset(cnt[:], 0.0), writes=[cnt])
                kind = None
                for ti in range(ntile):
                    i = ti
                    k2 = 0 if i < NTL else 1
                    if k2 != kind:
                        kind = k2
                        self.load_mod(s2, l, kind, 4)
                        self.load_mod(h2, l, kind, 3)
                    x, h, hb_ = xb[ti % 2], h32[ti % 2], hbs[ti % 2]
                    hT32, sm, mkb = hT32s[ti % 2], sms_[ti % 2], mkbs[ti % 2]
                    S.dma(S.sp, x[:], self.xsrc(i).t, x, reads=[self.xsrc(i)], writes=[x])
                    S.op(S.dve, lambda e, h=h, x=x: e.tensor_tensor(h[:], x[:], s2[:], ALU.mult), reads=[x, s2], writes=[h])
                    S.op(S.dve, lambda e, h=h: e.tensor_tensor(h[:], h[:], h2[:], ALU.add), reads=[h, h2], writes=[h])
                    S.op(S.act, lambda e, h=h, hb_=hb_: e.copy(hb_[:], h[:]), reads=[h], writes=[hb_])
                    S.dma(S.sp, self.HB[i * 128:(i + 1) * 128, :], hb_[:], hb_, reads=[hb_], writes=[self.HBt[i]])
                    cols = slice(ti * 128, (ti + 1) * 128)
                    for half in range(2):
                        pp = self.ps[half + 2 * (ti % 2)]

                        def tr(e, pp=pp, half=half, h=h):
                            for c in range(4):
                                cc = half * 4 + c
                                ins = e.transpose(pp[:, c * 128:(c + 1) * 128], h[:, cc * 128:(cc + 1) * 128], self.idf[:])
                            return ins
                        S.op(S.pe, tr, reads=[h, self.idf], writes=[pp])
                        S.op(S.act, lambda e, pp=pp, half=half, hT32=hT32: e.copy(hT32[:, half * 4:(half + 1) * 4, :],
                                                                          pp[:].rearrange("p (c t) -> p c t", c=4)),
                             reads=[pp], writes=[hT32])
                    pr = self.ps[4 + ti % 2]

                    def rmm(e, pr=pr, hT32=hT32):
                        for k in range(8):
                            ins = e.matmul(pr[:, 0:NE], hT32[:, k, :], rw[:, k, :], start=(k == 0), stop=(k == 7))
                        return ins
                    S.op(S.pe, rmm, reads=[hT32, rw], writes=[pr])
                    lg, top8, nmx, ex, ssum = sm[:, 0:32], sm[:, 32:40], sm[:, 40:41], sm[:, 48:80], sm[:, 112:113]
                    smB = sm
                    mk = MK[:, ti, :]
                    S.op(S.dve, lambda e: e.tensor_tensor(lg, pr[:, 0:NE], rb[:], ALU.add), reads=[pr, rb], writes=[sm])
                    S.op(S.dve, lambda e: e.max(top8, lg), reads=[sm], writes=[sm])
                    S.op(S.dve, lambda e: e.tensor_scalar_mul(nmx, top8[:, 0:1], -1.0), reads=[sm], writes=[sm])
                    S.op(S.dve, lambda e, mk=mk: e.tensor_scalar(mk, lg, top8[:, 3:4], None, ALU.is_ge), reads=[sm], writes=[MK])
                    S.op(S.act, lambda e: e.activation(ex, lg, AF.Exp, bias=nmx, scale=1.0), reads=[sm], writes=[sm])
                    S.op(S.dve, lambda e, mk=mk: e.tensor_tensor(ex, ex, mk, ALU.mult), reads=[sm, MK], writes=[sm])
                    S.op(S.dve, lambda e: e.reduce_sum(ssum, ex, AX.X), reads=[sm], writes=[sm])
                    S.op(S.dve, lambda e: e.reciprocal(ssum, ssum), reads=[sm], writes=[sm])
                    S.op(S.dve, lambda e, ti=ti: e.tensor_scalar_mul(G[:, ti, :], ex, ssum), reads=[sm], writes=[G])
                    S.op(S.dve, lambda e, mk=mk: e.tensor_copy(mkb[:], mk), reads=[MK], writes=[mkb])
                    pk = self.ps[4]

                    def rkmm(e):
                        e.matmul(pk[:, 0:NE], ustr[:], mkb[:], start=True, stop=True)
                        return e.matmul(pk[:, NE:2 * NE], onesb[:], mkb[:], start=True, stop=True)
                    S.op(S.pe, rkmm, reads=[ustr, onesb, mkb], writes=[pk])
                    S.op(S.dve, lambda e, ti=ti: e.tensor_tensor(RK[:, ti, :], pk[:, 0:NE], cnt[:], ALU.add), reads=[pk, cnt], writes=[RK])
                    S.op(S.dve, lambda e: e.tensor_tensor(cnt[:], cnt[:], pk[:, NE:2 * NE], ALU.add), reads=[pk, cnt], writes=[cnt])
                pad, pend, ps1, tmp = sm[:, 0:32], sm[:, 32:64], sm[:, 64:96], sm[:, 96:128]
                S.op(S.dve, lambda e: e.tensor_single_scalar(tmp, cnt[:], 0.0, ALU.is_gt), reads=[cnt], writes=[sm])
                for m in range(1, (T + BLK - 1) // BLK):
                    S.op(S.dve, lambda e, m=m: e.scalar_tensor_tensor(tmp, cnt[:], float(m * BLK), tmp, ALU.is_gt, ALU.add), reads=[cnt, sm], writes=[sm])
                S.op(S.dve, lambda e: e.tensor_scalar_mul(pad, tmp, float(BLK)), reads=[sm], writes=[sm])
                S.op(S.dve, lambda e: e.tensor_tensor_scan(pend, onesf[:], pad, 0.0, ALU.mult, ALU.add), reads=[sm, onesf], writes=[sm])
                S.op(S.dve, lambda e: e.tensor_tensor(ps1, pend, pad, ALU.subtract), reads=[sm], writes=[sm])
                S.op(S.dve, lambda e: e.tensor_scalar_add(ps1, ps1, 1.0), reads=[sm], writes=[sm])
                S.op(S.dve, lambda e: e.tensor_scalar(tmp, pend, blk[:, 0:1], None, ALU.is_le), reads=[sm, blk], writes=[sm])
                S.op(S.dve, lambda e: e.reduce_sum(sm[:, 128:129], tmp, AX.X), reads=[sm], writes=[sm])
                S.op(S.dve, lambda e: e.tensor_scalar_min(sm[:, 128:129], sm[:, 128:129], float(NE - 1)), reads=[sm], writes=[sm])
                ebi = S.sbuf("ebi", [128, 1], I32)
                S.op(S.dve, lambda e: e.tensor_copy(ebi[:], sm[:, 128:129]), reads=[sm], writes=[ebi])
                S.dma(S.sp, self.EB, ebi[:], ebi, reads=[ebi], writes=[self.EBb])
                ebB = S.sbuf("ebB", [128, 128], I32)
                ebF = S.sbuf("ebF", [128, 128], F32)
                rbase = S.sbuf("rbase", [128, 9], F32)
                idxf = S.sbuf("idxf", [128, 128 * 8], F32)
                S.dma(S.sp, ebB[:], self.EB.rearrange("p o -> (p o)").partition_broadcast(128), ebB, reads=[self.EBb], writes=[ebB])
                S.dma(S.sp, rbase[:], I["rowbase"], rbase, writes=[rbase])
                S.op(S.dve, lambda e: e.tensor_copy(ebF[:], ebB[:]), reads=[ebB], writes=[ebF])
                S.op(S.dve, lambda e: e.scalar_tensor_tensor(idxf[:].rearrange("p (b k) -> p b k", k=8),
                                                             ebF[:].rearrange("p (b o) -> p b o", o=1).to_broadcast([128, 128, 8]), 1024.0,
                                                             rbase[:, 0:8].rearrange("p (o k) -> p o k", o=1).to_broadcast([128, 128, 8]),
                                                             ALU.mult, ALU.add), reads=[ebF, rbase], writes=[idxf])
                if lw:
                    S.op(S.dve, lambda e: e.tensor_scalar_add(idxf[:], idxf[:], float(lw * NE * D)), reads=[idxf], writes=[idxf])
                S.op(S.dve, lambda e: e.tensor_copy(IDX[:], idxf[:]), reads=[idxf], writes=[IDX])
                S.op(S.dve, lambda e: e.tensor_scalar(idxf[:, 0:128], ebF[:], 128.0, rbase[:, 8:9], ALU.mult, ALU.add), reads=[ebF, rbase, idxf], writes=[idxf])
                if l:
                    S.op(S.dve, lambda e: e.tensor_scalar_add(idxf[:, 0:128], idxf[:, 0:128], float(l * NE * 128)), reads=[idxf], writes=[idxf])
                S.op(S.dve, lambda e: e.tensor_copy(IDXB[:], idxf[:, 0:128]), reads=[idxf], writes=[IDXB])
                key, t8, eq, d4f = sm[:, 0:32], sm[:, 96:104], sm[:, 104:136], sm[:, 136:140]
                for ti in range(ntile):
                    hb_ = hbs[ti % 2]
                    S.dma(S.sp, hb_[:], self.HB[ti * 128:(ti + 1) * 128, :], hb_, reads=[self.HBt[ti]], writes=[hb_])
                    S.op(S.dve, lambda e, ti=ti: e.tensor_tensor(key, RK[:, ti, :], ps1, ALU.add), reads=[RK, sm], writes=[sm])
                    S.op(S.dve, lambda e, ti=ti: e.tensor_tensor(key, key, MK[:, ti, :], ALU.mult), reads=[MK, sm], writes=[sm])
                    S.op(S.dve, lambda e: e.max(t8, key), reads=[sm], writes=[sm])
                    S.op(S.dve, lambda e: e.tensor_scalar_add(d4f, t8[:, 0:4], -1.0), reads=[sm], writes=[sm])
                    S.op(S.dve, lambda e, ti=ti: e.tensor_copy(D4[:, ti * 4:ti * 4 + 4], d4f), reads=[sm], writes=[D4])
                    for k in range(4):
                        S.op(S.dve, lambda e, k=k: e.tensor_scalar(eq, key, t8[:, k:k + 1], None, ALU.is_equal), reads=[sm], writes=[sm])
                        S.op(S.dve, lambda e, ti=ti: e.tensor_tensor(eq, eq, G[:, ti, :], ALU.mult), reads=[sm, G], writes=[sm])
                        S.op(S.dve, lambda e, ti=ti, k=k: e.reduce_sum(G4[:, ti, k:k + 1], eq, AX.X), reads=[sm], writes=[G4])
                    for k in range(4):
                        S.idma(self.XS[:, :], hb_[:, :], hb_, D4[:, ti * 4 + k:ti * 4 + k + 1], True, NBLK * BLK - 1,
                               reads=[hb_, D4], writes=[self.XSall] + self.XSb)
            if self.cfg.get("tap_route"):
                tg = self.tap("t_g4", [128, ntile, 4])
                S.dma(S.sp, tg, G4[:], G4, reads=[G4], is_output=True)
                td = self.tap("t_d4", [128, ntile * 4], I32)
                S.dma(S.sp, td, D4[:], D4, reads=[D4], is_output=True)
                te = self.tap("t_eb", [128, 128], I32)
                S.dma(S.sp, te, IDXB[:], IDXB, reads=[IDXB], is_output=True)
            with S.scope():
                w1b = [S.sbuf(f"w1b{i}", [128, 8, 2 * D], BF16) for i in range(2)]
                w2b = [S.sbuf(f"w2b{i}", [128, 8, D], BF16) for i in range(2)]
                b1s = [S.sbuf(f"b1s{i}", [128, 16], F32) for i in range(2)]
                xts = [S.sbuf(f"xts{i}", [128, D], BF16) for i in range(4)]
                hTs = [S.sbuf(f"hTb{i}", [128, 8, BLK], BF16) for i in range(2)]
                actT = [S.sbuf(f"actT{j}", [128, BLK], BF16) for j in range(8)]
                g32 = [S.sbuf(f"g32{i}", [128, BLK], F32) for i in range(3)]
                sg = [S.sbuf(f"sg{i}", [128, BLK], F32) for i in range(3)]
                l32 = [S.sbuf(f"l32{i}", [128, BLK], F32) for i in range(3)]
                gs = [S.sbuf(f"gs{i}", [128, BLK], F32) for i in range(3)]
                yts = [S.sbuf(f"yts{i}", [128, D], F32) for i in range(2)]
                w1tab = I["exp_w1"].rearrange("l e r n -> (l e r) n")
                w2tab = I["exp_w2"].rearrange("l e r n -> (l e r) n")
                b1tab = I["exp_b1T"].rearrange("l r c -> (l r) c")

                def weight_jobs(b):
                    buf = b % 2
                    jobs = [lambda: S.idma(b1s[buf][:, :], b1tab, b1s[buf], IDXB[:, b:b + 1], False, None, reads=[IDXB], writes=[b1s[buf]])]
                    for k in range(8):
                        jobs.append(lambda k=k: S.idma(w1b[buf][:, k, :], w1tab, w1b[buf], IDX[:, b * 8 + k:b * 8 + k + 1], False, None,
                                                       reads=[IDX], writes=[w1b[buf]]))
                    for k in range(8):
                        jobs.append(lambda k=k: S.idma(w2b[buf][:, k, :], w2tab, w2b[buf], IDX[:, b * 8 + k:b * 8 + k + 1], False, None,
                                                       reads=[IDX], writes=[w2b[buf]]))
                    return jobs

                def load_rows(bb):
                    hT_ = hTs[bb % 2]
                    for c in range(4):
                        xt = xts[c]
                        S.dma(S.sp, xt[:], self.XS[bb * BLK + c * 128:bb * BLK + (c + 1) * 128, :], xt, reads=[self.XSb[bb], self.XSall], writes=[xt])
                        pt = self.pb[c % 2]

                        def trx(e, xt=xt, pt=pt):
                            for cc in range(8):
                                ins = e.transpose(pt[:, cc * 128:(cc + 1) * 128], xt[:, cc * 128:(cc + 1) * 128], self.idb[:])
                            return ins
                        S.op(S.pe, trx, reads=[xt, self.idb], writes=[pt])
                        S.op(S.act, lambda e, pt=pt, hT_=hT_, c=c: e.copy(hT_[:, :, c * 128:(c + 1) * 128], pt[:].rearrange("p (c t) -> p c t", c=8)),
                             reads=[pt], writes=[hT_])
                pending = weight_jobs(0)
                for jb in pending:
                    jb()
                pending = []
                na = 0
                for b in range(nblk):
                    buf = b % 2
                    hT = hTs[b % 2]
                    if b + 1 < nblk:
                        pending = weight_jobs(b + 1)
                    if b == 0:
                        load_rows(0)
                    for j in range(8):
                        a = na % 2
                        na += 1
                        pgl, pll = self.ps[2 * a], self.ps[2 * a + 1]

                        def mm1(e, j=j, pgl=pgl, pll=pll, hT=hT, buf=buf):
                            for k in range(8):
                                e.matmul(pgl[:], w1b[buf][:, k, j * 128:(j + 1) * 128], hT[:, k, :], start=(k == 0), stop=(k == 7))
                            for k in range(8):
                                ins = e.matmul(pll[:], w1b[buf][:, k, D + j * 128:D + (j + 1) * 128], hT[:, k, :], start=(k == 0), stop=(k == 7))
                            return ins
                        S.op(S.pe, mm1, reads=[w1b[buf], hT], writes=[pgl, pll])
                        a3 = (na - 1) % 3
                        g_, s_, l_, gs_ = g32[a3], sg[a3], l32[a3], gs[a3]
                        S.op(S.dve, lambda e, g_=g_, pgl=pgl, j=j, buf=buf: e.tensor_scalar(g_[:], pgl[:], b1s[buf][:, j:j + 1], 7.0, ALU.add, ALU.min),
                             reads=[pgl, b1s[buf]], writes=[g_])
                        S.op(S.act, lambda e, g_=g_, s_=s_: e.activation(s_[:], g_[:], AF.Sigmoid, scale=1.702), reads=[g_], writes=[s_])
                        S.op(S.dve, lambda e, l_=l_, pll=pll, j=j, buf=buf: e.tensor_scalar(l_[:], pll[:], b1s[buf][:, 8 + j:9 + j], 7.0, ALU.add, ALU.min),
                             reads=[pll, b1s[buf]], writes=[l_])
                        S.op(S.dve, lambda e, l_=l_: e.tensor_scalar(l_[:], l_[:], -7.0, 1.0, ALU.max, ALU.add), reads=[l_], writes=[l_])
                        S.op(S.dve, lambda e, g_=g_, s_=s_, gs_=gs_: e.tensor_tensor(gs_[:], g_[:], s_[:], ALU.mult), reads=[g_, s_], writes=[gs_])
                        S.op(S.dve, lambda e, gs_=gs_, l_=l_, j=j: e.tensor_tensor(actT[j][:], gs_[:], l_[:], ALU.mult), reads=[gs_, l_], writes=[actT[j]])
                        for _ in range(3):
                            if pending:
                                pending.pop(0)()
                    if b + 1 < nblk:
                        load_rows(b + 1)
                    n = 0
                    for c in range(4):
                        yt = yts[c % 2]
                        for hf in range(2):
                            py = self.ps[4 + n % 2]
                            n += 1

                            def mm2(e, py=py, c=c, hf=hf, buf=buf):
                                for j in range(8):
                                    ins = e.matmul(py[:], actT[j][:, c * 128:(c + 1) * 128], w2b[buf][:, j, hf * 512:(hf + 1) * 512],
                                                   start=(j == 0), stop=(j == 7))
                                return ins
                            S.op(S.pe, mm2, reads=actT + [w2b[buf]], writes=[py])
                            if hf == 0:
                                S.op(S.dve, lambda e, py=py, yt=yt: e.tensor_copy(yt[:, 0:512], py[:]), reads=[py], writes=[yt])
                            else:
                                S.op(S.act, lambda e, py=py, yt=yt: e.copy(yt[:, 512:1024], py[:]), reads=[py], writes=[yt])
                        S.dma(S.sp, self.YS[b * BLK + c * 128:b * BLK + (c + 1) * 128, :], yt[:], yt, reads=[yt], writes=[self.YSb[b]])
                    while pending:
                        pending.pop(0)()
            with S.scope():
                b2t = S.sbuf("b2t", [NE, D], F32)
                g2 = S.sbuf("g2", [128, D], F32)
                lng = S.sbuf("lng", [128, D], F32)
                lnb = S.sbuf("lnb", [128, D], F32)
                xb = [S.sbuf(f"xb{i}", [128, D], F32) for i in range(2)]
                zb = [S.sbuf(f"zb{i}", [128, D], F32) for i in range(2)]
                ob = [S.sbuf(f"ob{i}", [128, D], F32) for i in range(2)]
                yk = [[S.sbuf(f"yk{i}_{k}", [128, D], F32) for k in range(4)] for i in range(2)]
                sms = [S.sbuf(f"sml{i}", [128, 16], F32) for i in range(2)]
                gts_ = [S.sbuf(f"gtt{i}", [NE, 128], F32) for i in range(2)]
                S.dma(S.sp, b2t[:], I["exp_b2"][l], b2t, writes=[b2t])
                self.load_vec(lng, I["ln_g"][l, 1])
                self.load_vec(lnb, I["ln_b"][l, 1])
                kind = None
                for ti in range(ntile):
                    i = ti
                    k2 = 0 if i < NTL else 1
                    if k2 != kind:
                        kind = k2
                        self.load_mod(g2, l, kind, 5)
                    x, z, o, sm = xb[ti % 2], zb[ti % 2], ob[ti % 2], sms[ti % 2]
                    S.dma(S.sp, x[:], self.xsrc(i).t, x, reads=[self.xsrc(i)], writes=[x])
                    for k in range(4):
                        S.idma(yk[ti % 2][k][:, :], self.YS[:, :], yk[ti % 2][k], D4[:, ti * 4 + k:ti * 4 + k + 1], False, NBLK * BLK - 1,
                               reads=self.YSb[:nblk] + [D4], writes=[yk[ti % 2][k]])
                    pg = self.ps[3]
                    gt = gts_[ti % 2]
                    S.op(S.pe, lambda e, ti=ti, pg=pg: e.transpose(pg[0:NE, 0:128], G[:, ti, :], self.idf[:]), reads=[G, self.idf], writes=[pg])
                    S.op(S.act, lambda e, gt=gt, pg=pg: e.copy(gt[:], pg[0:NE, 0:128]), reads=[pg], writes=[gt])
                    for hf in range(2):
                        py = self.ps[hf]
                        S.op(S.pe, lambda e, py=py, gt=gt, hf=hf: e.matmul(py[:], gt[:], b2t[:, hf * 512:(hf + 1) * 512], start=True, stop=True),
                             reads=[gt, b2t], writes=[py])
                        S.op(S.act, lambda e, py=py, z=z, hf=hf: e.copy(z[:, hf * 512:(hf + 1) * 512], py[:]), reads=[py], writes=[z])
                    for k in range(4):
                        ykk = yk[ti % 2][k]
                        S.op(S.dve, lambda e, z=z, ykk=ykk, ti=ti, k=k: e.scalar_tensor_tensor(z[:], ykk[:], G4[:, ti, k:k + 1], z[:], ALU.mult, ALU.add),
                             reads=[ykk, G4, z], writes=[z])
                    S.op(S.dve, lambda e, z=z: e.tensor_tensor(z[:], z[:], g2[:], ALU.mult), reads=[z, g2], writes=[z])
                    S.op(S.dve, lambda e, z=z, x=x: e.scalar_tensor_tensor(z[:], x[:], ALPHA, z[:], ALU.mult, ALU.add), reads=[x, z], writes=[z])
                    self.layernorm(z, o, lng, lnb, sm)
                    if last and l == DEPTH - 1:
                        S.dma(S.sp, self.out[i * 128:(i + 1) * 128, :], o[:], o, reads=[o], writes=[self.OUTt[i]], is_output=True)
                    else:
                        S.dma(S.sp, self.Xt[i].t, o[:], o, reads=[o], writes=[self.Xt[i]])

    def ffn_group(self, l, last, t0, t1):
        S, I = self.S, self.I
        ng = t1 - t0
        ncols = ng * 128
        with S.scope():
            HT = S.sbuf("HT", [128, 8, ncols], BF16)
            yacc = S.sbuf("yacc", [128, ng, D], F32)
            yb = [[S.view(f"y{a}_{b}", yacc[:, a, b * 512:(b + 1) * 512]) for b in range(2)] for a in range(ng)]
            G = S.sbuf("G", [128, ng, NE], F32)
            GT = S.sbuf("GT", [NE, ncols], F32)
            with S.scope():
                s2 = S.sbuf("s2", [128, D], F32)
                h2 = S.sbuf("h2", [128, D], F32)
                rw = S.sbuf("rw", [128, 8, NE], F32)
                rb = S.sbuf("rb", [128, NE], F32)
                xb = [S.sbuf(f"xb{i}", [128, D], F32) for i in range(2)]
                h32 = [S.sbuf(f"h32{i}", [128, D], F32) for i in range(2)]
                hT32 = S.sbuf("hT32", [128, 8, 128], F32)
                sm = S.sbuf("smr", [128, 128], F32)
                S.dma(S.sp, rw[:], I["router_w"][l].rearrange("(c p) n -> p c n", p=128), rw, writes=[rw])
                self.load_vec(rb, I["router_b"][l])
                kind = None
                S.dma(S.sp, xb[0][:], self.xsrc(t0).t, xb[0], reads=[self.xsrc(t0)], writes=[xb[0]])
                for ti in range(ng):
                    i = t0 + ti
                    k2 = 0 if i < NTL else 1
                    if k2 != kind:
                        kind = k2
                        self.load_mod(s2, l, kind, 4)
                        self.load_mod(h2, l, kind, 3)
                    x = xb[ti % 2]
                    h = h32[ti % 2]
                    if ti + 1 < ng:
                        xn = xb[(ti + 1) % 2]
                        S.dma(S.sp, xn[:], self.xsrc(i + 1).t, xn, reads=[self.xsrc(i + 1)], writes=[xn])
                    S.op(S.dve, lambda e, h=h, x=x: e.tensor_tensor(h[:], x[:], s2[:], ALU.mult), reads=[x, s2], writes=[h])
                    S.op(S.dve, lambda e, h=h: e.tensor_tensor(h[:], h[:], h2[:], ALU.add), reads=[h, h2], writes=[h])
                    cols = slice(ti * 128, (ti + 1) * 128)
                    lvl = self.cfg.get('f1_level', 9)
                    if lvl <= 1:
                        continue
                    for half in range(2):
                        pp = self.ps[half]

                        def tr(e, pp=pp, half=half, h=h):
                            for c in range(4):
                                cc = half * 4 + c
                                ins = e.transpose(pp[:, c * 128:(c + 1) * 128], h[:, cc * 128:(cc + 1) * 128], self.idf[:])
                            return ins
                        S.op(S.pe, tr, reads=[h, self.idf], writes=[pp])
                        S.op(S.act, lambda e, pp=pp, half=half: e.copy(hT32[:, half * 4:(half + 1) * 4, :],
                                                                          pp[:].rearrange("p (c t) -> p c t", c=4)),
                             reads=[pp], writes=[hT32])
                        S.op(S.dve, lambda e, pp=pp, half=half, cols=cols: e.tensor_copy(
                            HT[:, half * 4:(half + 1) * 4, cols], pp[:].rearrange("p (c t) -> p c t", c=4)),
                            reads=[pp], writes=[HT])
                    if lvl <= 2:
                        continue
                    pr = self.ps[2]

                    def rmm(e):
                        for k in range(8):
                            ins = e.matmul(pr[:, 0:NE], hT32[:, k, :], rw[:, k, :], start=(k == 0), stop=(k == 7))
                        return ins
                    S.op(S.pe, rmm, reads=[hT32, rw], writes=[pr])
                    if lvl <= 3:
                        continue
                    lg, top8, nmx, ex, mk, ssum = sm[:, 0:32], sm[:, 32:40], sm[:, 40:41], sm[:, 48:80], sm[:, 80:112], sm[:, 112:113]
                    S.op(S.dve, lambda e: e.tensor_tensor(lg, pr[:, 0:NE], rb[:], ALU.add), reads=[pr, rb], writes=[sm])
                    S.op(S.dve, lambda e: e.max(top8, lg), reads=[sm], writes=[sm])
                    S.op(S.dve, lambda e: e.tensor_scalar_mul(nmx, top8[:, 0:1], -1.0), reads=[sm], writes=[sm])
                    S.op(S.dve, lambda e: e.tensor_scalar(mk, lg, top8[:, 3:4], None, ALU.is_ge), reads=[sm], writes=[sm])
                    S.op(S.act, lambda e: e.activation(ex, lg, AF.Exp, bias=nmx, scale=1.0), reads=[sm], writes=[sm])
                    S.op(S.dve, lambda e: e.tensor_tensor(ex, ex, mk, ALU.mult), reads=[sm], writes=[sm])
                    S.op(S.dve, lambda e: e.reduce_sum(ssum, ex, AX.X), reads=[sm], writes=[sm])
                    S.op(S.dve, lambda e: e.reciprocal(ssum, ssum), reads=[sm], writes=[sm])
                    S.op(S.dve, lambda e, ti=ti: e.tensor_scalar_mul(G[:, ti, :], ex, ssum), reads=[sm], writes=[G])
                    if lvl <= 4:
                        continue
                    pg = self.ps[3]
                    S.op(S.pe, lambda e, ti=ti: e.transpose(pg[0:NE, 0:128], G[:, ti, :], self.idf[:]), reads=[G, self.idf], writes=[pg])
                    S.op(S.act, lambda e, cols=cols: e.copy(GT[:, cols], pg[0:NE, 0:128]), reads=[pg], writes=[GT])
            if self.cfg.get("tap_gates") and t0 == 0:
                tg = self.tap("gates", [128, 12, NE])
                S.dma(S.sp, tg, G[:], G, reads=[G], is_output=True)
            if self.cfg.get('stop') == 'F1':
                return
            with S.scope():
                w1p = [S.sbuf(f"w1p{i}", [128, 8, 256], BF16) for i in range(4)]
                w2b = [S.sbuf(f"w2b{i}", [128, 8, D], BF16) for i in range(2)]
                b1t = S.sbuf("b1t", [128, NE, 16], F32)
                b2t = S.sbuf("b2t", [NE, D], F32)
                actT = [S.sbuf(f"actT{j}", [128, ncols], BF16) for j in range(8)]
                g32 = [S.sbuf(f"g32{i}", [128, 512], F32) for i in range(2)]
                sg = [S.sbuf(f"sg{i}", [128, 512], F32) for i in range(2)]
                l32 = [S.sbuf(f"l32{i}", [128, 512], F32) for i in range(2)]
                gs = [S.sbuf(f"gs{i}", [128, 512], F32) for i in range(2)]
                S.dma(S.sp, b1t[:], I["exp_b1L"][l], b1t, writes=[b1t])
                S.dma(S.sp, b2t[:], I["exp_b2"][l], b2t, writes=[b2t])
                n = 0
                for ti in range(ng):
                    for hf in range(2):
                        py = self.ps[4 + n % 2]
                        n += 1
                        S.op(S.pe, lambda e, py=py, ti=ti, hf=hf: e.matmul(py[:], GT[:, ti * 128:(ti + 1) * 128],
                                                                             b2t[:, hf * 512:(hf + 1) * 512], start=True, stop=True),
                             reads=[GT, b2t], writes=[py])
                        S.op(S.act, lambda e, py=py, ti=ti, hf=hf: e.copy(yb[ti][hf][:], py[:]), reads=[py], writes=[yb[ti][hf]])
                sts = []
                c = 0
                while c < ncols:
                    sts.append((c, min(512, ncols - c)))
                    c += 512
                experts = self.cfg.get("experts", range(NE))
                lw = 0 if "exp_w1" in (self.cfg.get("dev_slice") or {}) else l
                cnt = 0
                for ei, ex_ in enumerate(experts):
                    w2 = w2b[ei % 2]
                    w1v = I["exp_w1"][lw, ex_].rearrange("(c p) n -> p c n", p=128)
                    S.dma(S.pool, w2[:], I["exp_w2"][lw, ex_].rearrange("(c p) n -> p c n", p=128), w2, writes=[w2])
                    for j in range(8):
                        pc = w1p[(ei * 8 + j) % 4]
                        S.dma(S.pool, pc[:, :, 0:128], w1v[:, :, j * 128:(j + 1) * 128], pc, writes=[pc])
                        S.dma(S.pool, pc[:, :, 128:256], w1v[:, :, D + j * 128:D + (j + 1) * 128], pc, writes=[pc])
                        for (c0, n_) in sts:
                            a = cnt % 2
                            cnt += 1
                            pgl, pll = self.ps[2 * a], self.ps[2 * a + 1]
                            cs = slice(c0, c0 + n_)

                            def mm1(e, pc=pc, pgl=pgl, pll=pll, cs=cs, n_=n_):
                                for k in range(8):
                                    e.matmul(pgl[:, 0:n_], pc[:, k, 0:128], HT[:, k, cs], start=(k == 0), stop=(k == 7))
                                for k in range(8):
                                    ins = e.matmul(pll[:, 0:n_], pc[:, k, 128:256], HT[:, k, cs], start=(k == 0), stop=(k == 7))
                                return ins
                            S.op(S.pe, mm1, reads=[pc, HT], writes=[pgl, pll])
                            g_, s_, l_, gs_ = g32[a], sg[a], l32[a], gs[a]
                            S.op(S.dve, lambda e, g_=g_, pgl=pgl, n_=n_, j=j, ex_=ex_: e.tensor_scalar(
                                g_[:, 0:n_], pgl[:, 0:n_], b1t[:, ex_, j:j + 1], 7.0, ALU.add, ALU.min),
                                reads=[pgl, b1t], writes=[g_])
                            S.op(S.act, lambda e, g_=g_, s_=s_, n_=n_: e.activation(s_[:, 0:n_], g_[:, 0:n_], AF.Sigmoid, scale=1.702),
                                 reads=[g_], writes=[s_])
                            S.op(S.dve, lambda e, l_=l_, pll=pll, n_=n_, j=j, ex_=ex_: e.tensor_scalar(
                                l_[:, 0:n_], pll[:, 0:n_], b1t[:, ex_, 8 + j:9 + j], 7.0, ALU.add, ALU.min),
                                reads=[pll, b1t], writes=[l_])
                            S.op(S.pool, lambda e, l_=l_, n_=n_: e.tensor_scalar(l_[:, 0:n_], l_[:, 0:n_], -7.0, 1.0, ALU.max, ALU.add),
                                 reads=[l_], writes=[l_])
                            S.op(S.pool, lambda e, g_=g_, s_=s_, gs_=gs_, n_=n_: e.tensor_tensor(gs_[:, 0:n_], g_[:, 0:n_], s_[:, 0:n_], ALU.mult),
                                 reads=[g_, s_], writes=[gs_])
                            S.op(S.dve, lambda e, gs_=gs_, l_=l_, j=j, cs=cs, n_=n_: e.tensor_tensor(actT[j][:, cs], gs_[:, 0:n_], l_[:, 0:n_], ALU.mult),
                                 reads=[gs_, l_], writes=[actT[j]])
                    n = 0
                    for ti in range(ng):
                        for hf in range(2):
                            py = self.ps[4 + n % 2]
                            n += 1

                            def mm2(e, py=py, ti=ti, hf=hf, w2=w2):
                                for j in range(8):
                                    ins = e.matmul(py[:], actT[j][:, ti * 128:(ti + 1) * 128], w2[:, j, hf * 512:(hf + 1) * 512],
                                                   start=(j == 0), stop=(j == 7))
                                return ins
                            S.op(S.pe, mm2, reads=actT + [w2], writes=[py])
                            yy = yb[ti][hf]
                            S.op(S.dve, lambda e, py=py, ti=ti, yy=yy, ex_=ex_: e.scalar_tensor_tensor(
                                yy[:], py[:], G[:, ti, ex_:ex_ + 1], yy[:], ALU.mult, ALU.add),
                                reads=[py, G, yy], writes=[yy])
            if self.cfg.get('stop') == 'F2':
                return
            with S.scope():
                g2 = S.sbuf("g2", [128, D], F32)
                lng = S.sbuf("lng", [128, D], F32)
                lnb = S.sbuf("lnb", [128, D], F32)
                xb = [S.sbuf(f"xb{i}", [128, D], F32) for i in range(2)]
                zb = [S.sbuf(f"zb{i}", [128, D], F32) for i in range(2)]
                ob = [S.sbuf(f"ob{i}", [128, D], F32) for i in range(2)]
                sms = [S.sbuf(f"sml{i}", [128, 16], F32) for i in range(2)]
                self.load_vec(lng, I["ln_g"][l, 1])
                self.load_vec(lnb, I["ln_b"][l, 1])
                kind = None
                S.dma(S.sp, xb[0][:], self.xsrc(t0).t, xb[0], reads=[self.xsrc(t0)], writes=[xb[0]])
                for ti in range(ng):
                    i = t0 + ti
                    k2 = 0 if i < NTL else 1
                    if k2 != kind:
                        kind = k2
                        self.load_mod(g2, l, kind, 5)
                    x, z, o, sm = xb[ti % 2], zb[ti % 2], ob[ti % 2], sms[ti % 2]
                    if ti + 1 < ng:
                        xn = xb[(ti + 1) % 2]
                        S.dma(S.sp, xn[:], self.xsrc(i + 1).t, xn, reads=[self.xsrc(i + 1)], writes=[xn])
                    S.op(S.dve, lambda e, z=z, ti=ti: e.tensor_tensor(z[:], yacc[:, ti, :], g2[:], ALU.mult),
                         reads=[yb[ti][0], yb[ti][1], g2], writes=[z])
                    S.op(S.dve, lambda e, z=z, x=x: e.scalar_tensor_tensor(z[:], x[:], ALPHA, z[:], ALU.mult, ALU.add),
                         reads=[x, z], writes=[z])
                    self.layernorm(z, o, lng, lnb, sm)
                    if last and l == DEPTH - 1:
                        S.dma(S.sp, self.out[i * 128:(i + 1) * 128, :], o[:], o, reads=[o], writes=[self.OUTt[i]], is_output=True)
                    else:
                        S.dma(S.sp, self.Xt[i].t, o[:], o, reads=[o], writes=[self.Xt[i]])


    def st_range(self, lo, hi):
        return list(range(lo // 512, (hi - 1) // 512 + 1))

    def mixer_even(self, l, last):
        S, I = self.S, self.I
        j = l // 2
        QTv = self.QT.rearrange("(f p) t -> p f t", p=128)
        KTv = self.KT.rearrange("(f p) t -> p f t", p=128)
        with S.scope():
            win = S.sbuf("win", [128, 8, 2560], BF16)
            wv = I["ab_w_in"][j].rearrange("(c p) n -> p c n", p=128)
            for b5 in range(5):
                S.dma(S.pool, win[:, :, b5 * 512:(b5 + 1) * 512], wv[:, :, b5 * 512:(b5 + 1) * 512], win, writes=[win])
            s1 = S.sbuf("s1", [128, D], F32)
            h1 = S.sbuf("h1", [128, D], F32)
            xb = [S.sbuf(f"xb{i}", [128, D], F32) for i in range(2)]
            h32 = S.sbuf("h32", [128, D], F32)
            hb = [S.sbuf(f"hb{i}", [128, D], BF16) for i in range(2)]
            hTs = [S.sbuf(f"hT{i}", [128, 8, 512], BF16) for i in range(2)]
            qks = [S.sbuf(f"qk{i}", [128, 12, 512], BF16) for i in range(2)]
            uvs = [S.sbuf(f"uv{i}", [128, 1024], BF16) for i in range(2)]
            kind = None
            nx = 0
            for s_ in range(9):
                tiles = list(range(4 * s_, 4 * s_ + 4)) if s_ < 8 else [32, 33]
                N = 128 * len(tiles)
                c0 = tiles[0] * 128
                hT = hTs[s_ % 2]
                qk = qks[s_ % 2]
                k2 = 0 if s_ < 8 else 1
                if k2 != kind:
                    kind = k2
                    self.load_mod(s1, l, kind, 1)
                    self.load_mod(h1, l, kind, 0)
                for tt, i in enumerate(tiles):
                    x = xb[nx % 2]
                    hbb = hb[nx % 2]
                    nx += 1
                    S.dma(S.sp, x[:], self.xsrc(i).t, x, reads=[self.xsrc(i)], writes=[x])
                    S.op(S.dve, lambda e, x=x: e.tensor_tensor(h32[:], x[:], s1[:], ALU.mult), reads=[x, s1], writes=[h32])
                    S.op(S.dve, lambda e, hbb=hbb: e.tensor_tensor(hbb[:], h32[:], h1[:], ALU.add), reads=[h32, h1], writes=[hbb])
                    pt = self.pb[0]

                    def tr(e, hbb=hbb, pt=pt):
                        for c in range(8):
                            ins = e.transpose(pt[:, c * 128:(c + 1) * 128], hbb[:, c * 128:(c + 1) * 128], self.idb[:])
                        return ins
                    S.op(S.pe, tr, reads=[hbb, self.idb], writes=[pt])
                    S.op(S.act, lambda e, pt=pt, hT=hT, tt=tt: e.copy(hT[:, :, tt * 128:(tt + 1) * 128],
                                                                      pt[:].rearrange("p (c t) -> p c t", c=8)),
                         reads=[pt], writes=[hT])
                for tt, i in enumerate(tiles):
                    uv = uvs[i % 2]
                    pu, pv1, pv2 = self.ps[0], self.ps[1], self.ps[2]
                    tc_ = slice(tt * 128, (tt + 1) * 128)

                    def mmuv(e, tc_=tc_, hT=hT, pu=pu, pv1=pv1, pv2=pv2):
                        for k in range(8):
                            e.matmul(pu[:, 0:256], hT[:, k, tc_], win[:, k, 0:256], start=(k == 0), stop=(k == 7))
                        for k in range(8):
                            e.matmul(pv1[:], hT[:, k, tc_], win[:, k, 1792:2304], start=(k == 0), stop=(k == 7))
                        for k in range(8):
                            ins = e.matmul(pv2[:, 0:256], hT[:, k, tc_], win[:, k, 2304:2560], start=(k == 0), stop=(k == 7))
                        return ins
                    S.op(S.pe, mmuv, reads=[hT, win], writes=[pu, pv1, pv2])
                    S.op(S.dve, lambda e, uv=uv, pu=pu: e.tensor_copy(uv[:, 0:256], pu[:, 0:256]), reads=[pu], writes=[uv])
                    S.op(S.act, lambda e, uv=uv, pv1=pv1: e.copy(uv[:, 256:768], pv1[:]), reads=[pv1], writes=[uv])
                    S.op(S.dve, lambda e, uv=uv, pv2=pv2: e.tensor_copy(uv[:, 768:1024], pv2[:, 0:256]), reads=[pv2], writes=[uv])
                    S.dma(S.sp, self.U[i * 128:(i + 1) * 128, 0:256], uv[:, 0:256], uv, reads=[uv], writes=[self.Ut[i]])
                    S.dma(S.sp, self.V[i * 128:(i + 1) * 128, 0:768], uv[:, 256:1024], uv, reads=[uv], writes=[self.Vt[i]])
                for f in range(12):
                    pq = self.ps[3 + f % 2]

                    def mmq(e, f=f, pq=pq, hT=hT, N=N):
                        for k in range(8):
                            ins = e.matmul(pq[:, 0:N], win[:, k, 256 + f * 128:256 + (f + 1) * 128], hT[:, k, 0:N],
                                           start=(k == 0), stop=(k == 7))
                        return ins
                    S.op(S.pe, mmq, reads=[hT, win], writes=[pq])
                    if f < 6:
                        S.op(S.act, lambda e, f=f, pq=pq, qk=qk, N=N: e.mul(qk[:, f, 0:N], pq[:, 0:N], 0.125), reads=[pq], writes=[qk])
                    else:
                        S.op(S.dve, lambda e, f=f, pq=pq, qk=qk, N=N: e.tensor_copy(qk[:, f, 0:N], pq[:, 0:N]), reads=[pq], writes=[qk])
                S.dma(S.sp, QTv[:, :, c0:c0 + N], qk[:, 0:6, 0:N], qk, reads=[qk], writes=[self.QTs[s_]])
                S.dma(S.sp, KTv[:, :, c0:c0 + N], qk[:, 6:12, 0:N], qk, reads=[qk], writes=[self.KTs[s_]])
        with S.scope():
            bias = S.sbuf("nab", [128, 12, 832], F32)
            kTc = S.sbuf("kTc", [128, 6, 256], BF16)
            vc = S.sbuf("vc", [128, 2, 768], BF16)
            wout = S.sbuf("wout", [128, 8, D], BF16)
            Wbd = S.sbuf("Wbd", [128, 2, 256], BF16)
            pA = S.sbuf("pA", [128, 20, 128], BF16)
            psc = S.sbuf("psc", [128, 256], F32)
            g1 = S.sbuf("g1", [128, D], F32)
            lng = S.sbuf("lng", [128, D], F32)
            lnb = S.sbuf("lnb", [128, D], F32)
            kws = [S.sbuf(f"kw{i}", [128, 6, 576], BF16) for i in range(2)]
            vws = [S.sbuf(f"vw{i}", [128, 5, 768], BF16) for i in range(2)]
            qts = [S.sbuf(f"qt{i}", [128, 6, 128], BF16) for i in range(2)]
            uws = [S.sbuf(f"uw{i}", [128, 3, 256], BF16) for i in range(2)]
            xb = [S.sbuf(f"xb{i}", [128, D], F32) for i in range(2)]
            zb = [S.sbuf(f"zb{i}", [128, D], F32) for i in range(2)]
            ob = [S.sbuf(f"ob{i}", [128, D], F32) for i in range(2)]
            sms = [S.sbuf(f"sml{i}", [128, 16], F32) for i in range(2)]
            ssb = [S.sbuf(f"ssb{i}", [128, 832], F32) for i in range(3)]
            psb = [S.sbuf(f"psb{i}", [128, 832], BF16) for i in range(3)]
            pTs = [S.sbuf(f"pT{i}", [128, 7, 128], BF16) for i in range(3)]
            st4 = [S.sbuf(f"st4{i}", [128, 4], F32) for i in range(3)]
            rs12 = [S.sbuf(f"rs12{i}", [128, 24], F32) for i in range(2)]
            cats = [S.sbuf(f"cat{i}", [128, D], BF16) for i in range(2)]
            catT = S.sbuf("catT", [128, 8, 128], BF16)
            pooled = S.sbuf("pooled", [128, 256], BF16)
            pT2 = S.sbuf("pT2", [128, 2, 128], BF16)
            S.dma(S.sp, kTc[:], KTv[:, :, TL:T], kTc, reads=[self.KTs[8]], writes=[kTc])
            S.dma(S.sp, vc[:], self.V[TL:T, 0:768].rearrange("(c p) d -> p c d", p=128), vc, reads=[self.Vt[32], self.Vt[33]], writes=[vc])
            S.dma(S.pool, wout[:], I["ab_w_out"][j].rearrange("(c p) n -> p c n", p=128), wout, writes=[wout])
            S.dma(S.pool, pA[:], I["poolA"].rearrange("v s t -> s v t"), pA, writes=[pA])
            S.op(S.dve, lambda e: e.memset(Wbd[:], 0.0), writes=[Wbd])
            for g in range(4):
                S.dma(S.pool, Wbd[(g % 2) * 64:(g % 2) * 64 + 64, g // 2, g * 64:(g + 1) * 64], I["ab_pool_w"][j, g], Wbd, writes=[Wbd])
            self.load_vec(psc, I["ab_pool_scale"][j])
            self.load_vec(lng, I["ln_g"][l, 0])
            self.load_vec(lnb, I["ln_b"][l, 0])
            S.op(S.dve, lambda e: e.memset(bias[:, :, 0:256], 0.0), writes=[bias])
            ntile = NTL if last else NT
            kind = None
            cur_pat = None
            nh = 0
            for i in range(ntile):
                isctx = i >= NTL
                k2 = 1 if isctx else 0
                if k2 != kind:
                    kind = k2
                    self.load_mod(g1, l, kind, 2)
                x, z, o, sm = xb[i % 2], zb[i % 2], ob[i % 2], sms[i % 2]
                qt, kw, vw, uw, cat = qts[i % 2], kws[i % 2], vws[i % 2], uws[i % 2], cats[i % 2]
                S.dma(S.sp, x[:], self.xsrc(i).t, x, reads=[self.xsrc(i)], writes=[x])
                S.dma(S.sp, qt[:], QTv[:, :, i * 128:(i + 1) * 128], qt, reads=[self.QTs[i // 4 if not isctx else 8]], writes=[qt])
                base, nseq = (0, NTL) if not isctx else (NTL, 2)
                li = i - base
                blocks = []
                if li > 0:
                    blocks.append((0, 0))
                blocks.append((1, 3 if li == 0 else (4 if li == nseq - 1 else 1)))
                if li < nseq - 1:
                    blocks.append((2, 2))
                for (nb, var) in blocks:
                    ii = i + nb - 1
                    S.dma(S.sp, uw[:, nb, :], self.U[ii * 128:(ii + 1) * 128, 0:256], uw, reads=[self.Ut[ii]], writes=[uw])
                if not isctx:
                    pat = 0 if i == 0 else 1 if i == 1 else 3 if i == 30 else 4 if i == 31 else 2
                    if pat != cur_pat:
                        cur_pat = pat
                        S.dma(S.sp, bias[:, :, 256:832], I["nabias"][j, pat].rearrange("h q k -> q h k"), bias, writes=[bias])
                    kr0 = min(max(2 * i - 4, 0), 55)
                    k0 = kr0 * 64
                    S.dma(S.sp, kw[:], KTv[:, :, k0:k0 + 576], kw, reads=[self.KTs[a] for a in self.st_range(k0, k0 + 576)], writes=[kw])
                    vr = [self.Vt[a] for a in range(k0 // 128, (k0 + 575) // 128 + 1)]
                    S.dma(S.sp, vw[:, 0:4, :], self.V[k0:k0 + 512, 0:768].rearrange("(c p) d -> p c d", p=128), vw, reads=vr, writes=[vw])
                    S.dma(S.sp, vw[0:64, 4, :], self.V[k0 + 512:k0 + 576, 0:768], vw, reads=vr, writes=[vw])
                pp = self.ps[4]

                def mmpool(e, blocks=blocks, uw=uw, pp=pp):
                    for g in range(4):
                        for bi, (nb, var) in enumerate(blocks):
                            ins = e.matmul(pp[:, g * 64:(g + 1) * 64], pA[:, g * 5 + var, :], uw[:, nb, g * 64:(g + 1) * 64],
                                           start=(bi == 0), stop=(bi == len(blocks) - 1))
                    return ins
                S.op(S.pe, mmpool, reads=[pA, uw], writes=[pp])
                S.op(S.act, lambda e, pp=pp: e.copy(pooled[:], pp[:, 0:256]), reads=[pp], writes=[pooled])
                ptb = self.pb[1]

                def trp(e, ptb=ptb):
                    for c in range(2):
                        ins = e.transpose(ptb[:, c * 128:(c + 1) * 128], pooled[:, c * 128:(c + 1) * 128], self.idb[:])
                    return ins
                S.op(S.pe, trp, reads=[pooled, self.idb], writes=[ptb])
                S.op(S.dve, lambda e, ptb=ptb: e.tensor_copy(pT2[:], ptb[:, 0:256].rearrange("p (c t) -> p c t", c=2)), reads=[ptb], writes=[pT2])
                py = self.ps[5]

                def mmy(e, py=py):
                    for c in range(2):
                        ins = e.matmul(py[:, 0:256], pT2[:, c, :], Wbd[:, c, :], start=(c == 0), stop=(c == 1))
                    return ins
                S.op(S.pe, mmy, reads=[pT2, Wbd], writes=[py])
                S.op(S.dve, lambda e, py=py, cat=cat: e.tensor_tensor(cat[:, 0:256], py[:, 0:256], psc[:], ALU.mult), reads=[py, psc], writes=[cat])
                for h in range(12):
                    hp, hh = h // 2, h % 2
                    psl = slice(hh * 64, hh * 64 + 64)
                    ss, pb_, pT, st = ssb[nh % 3], psb[nh % 3], pTs[nh % 3], st4[nh % 3]
                    rs = rs12[i % 2]
                    rcol = (h % 2) * 6 + h // 2
                    nh += 1
                    pS1, pS2 = self.ps[2 * (h % 3)], self.ps[2 * (h % 3) + 1]
                    nk = 256 if isctx else 832
                    nblk = 2 if isctx else 7

                    def mms(e, qt=qt, kw=kw, hp=hp, psl=psl, pS1=pS1, pS2=pS2, isctx=isctx):
                        ins = e.matmul(pS1[:, 0:256], qt[psl, hp, :], kTc[psl, hp, :], start=True, stop=True)
                        if not isctx:
                            e.matmul(pS1[:, 256:512], qt[psl, hp, :], kw[psl, hp, 0:256], start=True, stop=True)
                            ins = e.matmul(pS2[:, 0:320], qt[psl, hp, :], kw[psl, hp, 256:576], start=True, stop=True)
                        return ins
                    S.op(S.pe, mms, reads=[qt, kTc] + ([] if isctx else [kw]), writes=[pS1] + ([] if isctx else [pS2]))
                    if isctx:
                        S.op(S.act, lambda e, ss=ss, pS1=pS1: e.copy(ss[:, 0:256], pS1[:, 0:256]), reads=[pS1], writes=[ss])
                    else:
                        S.op(S.dve, lambda e, ss=ss, pS1=pS1, h=h: e.tensor_tensor(ss[:, 0:512], pS1[:], bias[:, h, 0:512], ALU.add),
                             reads=[pS1, bias], writes=[ss])
                        S.op(S.dve, lambda e, ss=ss, pS2=pS2, h=h: e.tensor_tensor(ss[:, 512:832], pS2[:, 0:320], bias[:, h, 512:832], ALU.add),
                             reads=[pS2, bias], writes=[ss])
                    S.op(S.dve, lambda e, ss=ss, st=st, nk=nk: e.reduce_max(st[:, 0:1], ss[:, 0:nk], AX.X), reads=[ss], writes=[st])
                    S.op(S.dve, lambda e, st=st: e.tensor_scalar_mul(st[:, 1:2], st[:, 0:1], -1.0), reads=[st], writes=[st])
                    S.op(S.act, lambda e, ss=ss, pb_=pb_, st=st, nk=nk: e.activation(pb_[:, 0:nk], ss[:, 0:nk], AF.Exp, bias=st[:, 1:2], scale=1.0,
                                                                               accum_out=rs[:, rcol:rcol + 1]),
                         reads=[ss, st], writes=[pb_, rs])
                    ptb = pS1
                    ptv = pS1.t[:].bitcast(BF16)

                    def trs(e, pb_=pb_, ptb=ptv, nblk=nblk):
                        for c in range(nblk):
                            if c < 6:
                                ins = e.transpose(ptb[:, c * 128:(c + 1) * 128], pb_[:, c * 128:(c + 1) * 128], self.idb[:])
                            else:
                                ins = e.transpose(ptb[0:64, 768:896], pb_[:, 768:832], self.idb[:])
                        return ins
                    S.op(S.pe, trs, reads=[pb_, self.idb], writes=[ptb])
                    cp_eng = S.act if h % 2 == 0 else S.dve
                    if cp_eng is S.act:
                        S.op(S.act, lambda e, pT=pT, ptv=ptv, nblk=nblk: e.copy(pT[:, 0:nblk, :], ptv[:, 0:nblk * 128].rearrange("p (c t) -> p c t", c=nblk)),
                             reads=[ptb], writes=[pT])
                    else:
                        S.op(S.dve, lambda e, pT=pT, ptv=ptv, nblk=nblk: e.tensor_copy(pT[:, 0:nblk, :], ptv[:, 0:nblk * 128].rearrange("p (c t) -> p c t", c=nblk)),
                             reads=[ptb], writes=[pT])
                    po = self.pb[h % 2]
                    pov = po.t[:].bitcast(F32)
                    oc = slice((h // 2) * 64, (h // 2) * 64 + 64)

                    def mmpv(e, pT=pT, vw=vw, po=pov, oc=oc, h=h, nblk=nblk):
                        for c in range(nblk):
                            if c < 2:
                                ins = e.matmul(po[:, oc], pT[:, c, :], vc[:, c, h * 64:(h + 1) * 64], start=(c == 0), stop=(c == nblk - 1))
                            elif c < 6:
                                ins = e.matmul(po[:, oc], pT[:, c, :], vw[:, c - 2, h * 64:(h + 1) * 64], start=False, stop=False)
                            else:
                                ins = e.matmul(po[:, oc], pT[0:64, 6, :], vw[0:64, 4, h * 64:(h + 1) * 64], start=False, stop=True)
                        return ins
                    S.op(S.pe, mmpv, reads=[pT, vc] + ([] if isctx else [vw]), writes=[po])
                rs = rs12[i % 2]
                S.op(S.dve, lambda e, rs=rs: e.reciprocal(rs[:, 12:24], rs[:, 0:12]), reads=[rs], writes=[rs])
                cat4 = cat[:, 256:1024].rearrange("p (a two d) -> p a two d", two=2, d=64)
                for par in range(2):
                    po = self.pb[par]
                    pov2 = po.t[:].bitcast(F32)
                    S.op(S.dve, lambda e, po=pov2, par=par, rs=rs, cat4=cat4: e.tensor_tensor(
                        cat4[:, :, par, :], po[:, 0:384].rearrange("p (a d) -> p a d", d=64),
                        rs[:, 12 + par * 6:18 + par * 6].rearrange("p (a o) -> p a o", o=1).to_broadcast([128, 6, 64]), ALU.mult),
                        reads=[po, rs], writes=[cat])
                self.out_proj_ln(l, i, cat, catT, wout, g1, x, z, o, sm, lng, lnb)
        self.src_is_input = False

    def out_proj_ln(self, l, i, cat, catT, wout, g1, x, z, o, sm, lng, lnb):
        S = self.S
        ptb = self.pb[1]

        def trc(e):
            for c in range(8):
                ins = e.transpose(ptb[:, c * 128:(c + 1) * 128], cat[:, c * 128:(c + 1) * 128], self.idb[:])
            return ins
        S.op(S.pe, trc, reads=[cat, self.idb], writes=[ptb])
        S.op(S.act, lambda e: e.copy(catT[:], ptb[:].rearrange("p (c t) -> p c t", c=8)), reads=[ptb], writes=[catT])
        for hf in range(2):
            po2 = self.ps[hf]

            def mmo(e, po2=po2, hf=hf):
                for k in range(8):
                    ins = e.matmul(po2[:], catT[:, k, :], wout[:, k, hf * 512:(hf + 1) * 512], start=(k == 0), stop=(k == 7))
                return ins
            S.op(S.pe, mmo, reads=[catT, wout], writes=[po2])
            S.op(S.dve, lambda e, po2=po2, hf=hf: e.tensor_tensor(z[:, hf * 512:(hf + 1) * 512], po2[:], g1[:, hf * 512:(hf + 1) * 512], ALU.mult),
                 reads=[po2, g1], writes=[z])
        if self.cfg.get("tap_ox"):
            if not hasattr(self, "_tox"):
                self._tox = self.tap("oxg", [T, D])
            S.dma(S.sp, self._tox[i * 128:(i + 1) * 128, :], z[:], z, reads=[z], is_output=True)
        S.op(S.dve, lambda e: e.scalar_tensor_tensor(z[:], x[:], ALPHA, z[:], ALU.mult, ALU.add), reads=[x, z], writes=[z])
        self.layernorm(z, o, lng, lnb, sm)
        S.dma(S.sp, self.Xt[i].t, o[:], o, reads=[o], writes=[self.Xt[i]])


    def mixer_odd(self, l, last):
        S, I = self.S, self.I
        j = l // 2
        QTv = self.QT[0:512, :].rearrange("(f p) t -> p f t", p=128)
        KTv = self.KT[0:512, :].rearrange("(f p) t -> p f t", p=128)
        QSCALE = 128.0 ** -0.5
        with S.scope():
            win = S.sbuf("gwin", [128, 8, 3104], BF16)
            wv = I["gla_w_in"][j].rearrange("(c p) n -> p c n", p=128)
            for b5 in range(6):
                S.dma(S.pool, win[:, :, b5 * 512:(b5 + 1) * 512], wv[:, :, b5 * 512:(b5 + 1) * 512], win, writes=[win])
            S.dma(S.pool, win[:, :, 3072:3104], wv[:, :, 3072:3104], win, writes=[win])
            s1 = S.sbuf("s1", [128, D], F32)
            h1 = S.sbuf("h1", [128, D], F32)
            xb = [S.sbuf(f"xb{i}", [128, D], F32) for i in range(2)]
            h32 = S.sbuf("h32", [128, D], F32)
            hb = [S.sbuf(f"hb{i}", [128, D], BF16) for i in range(2)]
            hTs = [S.sbuf(f"hT{i}", [128, 8, 512], BF16) for i in range(2)]
            qk32 = S.sbuf("qk32", [128, D], F32)
            tA = S.sbuf("tA", [128, 512], F32)
            tB = S.sbuf("tB", [128, 512], F32)
            qkb = [S.sbuf(f"qkb{i}", [128, D], BF16) for i in range(2)]
            qkT = [S.sbuf(f"qkT{i}", [128, 8, 128], BF16) for i in range(2)]
            vrs = [S.sbuf(f"vr{i}", [128, 2 * D], BF16) for i in range(2)]
            css = [S.sbuf(f"cs{i}", [128, 2, 64], F32) for i in range(2)]
            gsb = [S.sbuf(f"gsb{i}", [16, 2, 512], F32) for i in range(2)]
            kind = None
            nx = 0
            for s_ in range(9):
                tiles = list(range(4 * s_, 4 * s_ + 4)) if s_ < 8 else [32, 33]
                N = 128 * len(tiles)
                c0 = tiles[0] * 128
                hT = hTs[s_ % 2]
                k2 = 0 if s_ < 8 else 1
                if k2 != kind:
                    kind = k2
                    self.load_mod(s1, l, kind, 1)
                    self.load_mod(h1, l, kind, 0)
                for tt, i in enumerate(tiles):
                    x = xb[nx % 2]
                    hbb = hb[nx % 2]
                    nx += 1
                    S.dma(S.sp, x[:], self.xsrc(i).t, x, reads=[self.xsrc(i)], writes=[x])
                    S.op(S.dve, lambda e, x=x: e.tensor_tensor(h32[:], x[:], s1[:], ALU.mult), reads=[x, s1], writes=[h32])
                    S.op(S.dve, lambda e, hbb=hbb: e.tensor_tensor(hbb[:], h32[:], h1[:], ALU.add), reads=[h32, h1], writes=[hbb])
                    pt = self.pb[0]

                    def tr(e, hbb=hbb, pt=pt):
                        for c in range(8):
                            ins = e.transpose(pt[:, c * 128:(c + 1) * 128], hbb[:, c * 128:(c + 1) * 128], self.idb[:])
                        return ins
                    S.op(S.pe, tr, reads=[hbb, self.idb], writes=[pt])
                    S.op(S.act, lambda e, pt=pt, hT=hT, tt=tt: e.copy(hT[:, :, tt * 128:(tt + 1) * 128],
                                                                      pt[:].rearrange("p (c t) -> p c t", c=8)),
                         reads=[pt], writes=[hT])
                gs_ = gsb[s_ % 2]
                for z in range(2):
                    pgz = self.ps[4 + z]

                    def mmg(e, z=z, pgz=pgz, hT=hT, N=N):
                        for k in range(8):
                            ins = e.matmul(pgz[0:16, 0:N], win[:, k, 3072 + 16 * z:3088 + 16 * z], hT[:, k, 0:N], start=(k == 0), stop=(k == 7))
                        return ins
                    S.op(S.pe, mmg, reads=[hT, win], writes=[pgz])
                    S.op(S.act, lambda e, z=z, pgz=pgz, gs_=gs_, N=N: e.copy(gs_[:, z, 0:N], pgz[0:16, 0:N]), reads=[pgz], writes=[gs_])
                S.dma(S.sp, self.GS[:, :, c0:c0 + N].rearrange("z r t -> r z t"), gs_[:, :, 0:N], gs_, reads=[gs_], writes=[self.GSs[s_]])
                for tt, i in enumerate(tiles):
                    isctx = i >= NTL
                    tc_ = slice(tt * 128, (tt + 1) * 128)
                    vr = vrs[i % 2]
                    qb = qkb[i % 2]
                    qT_ = qkT[i % 2]
                    for blk in range(6):
                        pz = self.ps[blk % 4]

                        def mmt(e, blk=blk, pz=pz, hT=hT, tc_=tc_):
                            for k in range(8):
                                ins = e.matmul(pz[:], hT[:, k, tc_], win[:, k, blk * 512:(blk + 1) * 512], start=(k == 0), stop=(k == 7))
                            return ins
                        S.op(S.pe, mmt, reads=[hT, win], writes=[pz])
                        if blk == 0:
                            S.op(S.act, lambda e, pz=pz: e.mul(qk32[:, 0:512], pz[:], QSCALE), reads=[pz], writes=[qk32])
                        elif blk == 1:
                            S.op(S.act, lambda e, pz=pz: e.copy(qk32[:, 512:1024], pz[:]), reads=[pz], writes=[qk32])
                        elif blk % 2 == 0:
                            S.op(S.dve, lambda e, pz=pz, blk=blk, vr=vr: e.tensor_copy(vr[:, (blk - 2) * 512:(blk - 1) * 512], pz[:]), reads=[pz], writes=[vr])
                        else:
                            S.op(S.act, lambda e, pz=pz, blk=blk, vr=vr: e.copy(vr[:, (blk - 2) * 512:(blk - 1) * 512], pz[:]), reads=[pz], writes=[vr])
                    S.dma(S.sp, self.V[i * 128:(i + 1) * 128, :], vr[:, 0:D], vr, reads=[vr], writes=[self.Vt[i]])
                    S.dma(S.sp, self.U[i * 128:(i + 1) * 128, :], vr[:, D:2 * D], vr, reads=[vr], writes=[self.Ut[i]])
                    if isctx:
                        S.op(S.dve, lambda e, qb=qb: e.tensor_copy(qb[:], qk32[:]), reads=[qk32], writes=[qb])
                    else:
                        cs = css[i % 2]
                        S.dma(S.sp, cs[:], I["ropecs"][i * 128:(i + 1) * 128], cs, writes=[cs])
                        q4 = qk32[:].rearrange("p (h two d) -> p h two d", two=2, d=64)
                        o4 = qb[:].rearrange("p (h two d) -> p h two d", two=2, d=64)
                        x1, x2 = q4[:, :, 0, :], q4[:, :, 1, :]
                        cosb = cs[:, 0:1, :].to_broadcast([128, 8, 64])
                        sinb = cs[:, 1:2, :].to_broadcast([128, 8, 64])
                        a3 = tA[:].rearrange("p (h d) -> p h d", d=64)
                        b3 = tB[:].rearrange("p (h d) -> p h d", d=64)
                        S.op(S.dve, lambda e: e.tensor_tensor(a3, x1, cosb, ALU.mult), reads=[qk32, cs], writes=[tA])
                        S.op(S.pool, lambda e: e.tensor_tensor(b3, x2, sinb, ALU.mult), reads=[qk32, cs], writes=[tB])
                        S.op(S.dve, lambda e, o4=o4: e.tensor_tensor(o4[:, :, 0, :], a3, b3, ALU.subtract), reads=[tA, tB], writes=[qb])
                        S.op(S.dve, lambda e: e.tensor_tensor(a3, x1, sinb, ALU.mult), reads=[qk32, cs], writes=[tA])
                        S.op(S.pool, lambda e: e.tensor_tensor(b3, x2, cosb, ALU.mult), reads=[qk32, cs], writes=[tB])
                        S.op(S.dve, lambda e, o4=o4: e.tensor_tensor(o4[:, :, 1, :], a3, b3, ALU.add), reads=[tA, tB], writes=[qb])
                    pt = self.pb[1]

                    def trq(e, qb=qb, pt=pt):
                        for c in range(8):
                            ins = e.transpose(pt[:, c * 128:(c + 1) * 128], qb[:, c * 128:(c + 1) * 128], self.idb[:])
                        return ins
                    S.op(S.pe, trq, reads=[qb, self.idb], writes=[pt])
                    S.op(S.dve, lambda e, pt=pt, qT_=qT_: e.tensor_copy(qT_[:], pt[:].rearrange("p (c t) -> p c t", c=8)), reads=[pt], writes=[qT_])
                    S.dma(S.sp, QTv[:, :, i * 128:(i + 1) * 128], qT_[:, 0:4, :], qT_, reads=[qT_], writes=[self.QTs[s_]])
                    S.dma(S.sp, KTv[:, :, i * 128:(i + 1) * 128], qT_[:, 4:8, :], qT_, reads=[qT_], writes=[self.KTs[s_]])
        with S.scope():
            wg = S.sbuf("wg", [16, 2, 512], F32)
            negb = S.sbuf("negb", [128, 2, 4], F32)
            msk = S.sbuf("msk", [128, 2, 128], F32)
            ones = S.sbuf("ones", [128, 128], F32)
            S.dma(S.sp, wg[:], I["gla_w_gate"][j].rearrange("z r e -> r z e"), wg, writes=[wg])
            S.dma(S.sp, negb[:], I["gla_bgL"][j], negb, writes=[negb])
            S.op(S.dve, lambda e: e.tensor_scalar_mul(negb[:], negb[:], -1.0), reads=[negb], writes=[negb])
            S.dma(S.sp, msk[:], I["trimask"][0:2].rearrange("z s t -> s z t"), msk, writes=[msk])
            S.op(S.dve, lambda e: e.memset(ones[:], 1.0), writes=[ones])
            S32 = [[S.sbuf(f"S32_{z}{h}", [128, 256], F32) for h in range(4)] for z in range(2)]
            Sb = [[S.sbuf(f"Sb_{z}{h}", [128, 256], BF16) for h in range(4)] for z in range(2)]
            for z in range(2):
                for h in range(4):
                    S.op(S.dve, lambda e, z=z, h=h: e.memset(S32[z][h][:], 0.0), writes=[S32[z][h]])
                    S.op(S.pool, lambda e, z=z, h=h: e.memset(Sb[z][h][:], 0.0), writes=[Sb[z][h]])
            qts = [[S.sbuf(f"gq{z}{i}", [128, 4, 128], BF16) for i in range(2)] for z in range(2)]
            kts = [[S.sbuf(f"gk{z}{i}", [128, 4, 128], BF16) for i in range(2)] for z in range(2)]
            vts = [[S.sbuf(f"gv{z}{i}", [128, D], BF16) for i in range(2)] for z in range(2)]
            gts = [[S.sbuf(f"gg{z}{i}", [16, 128], F32) for i in range(2)] for z in range(2)]
            osb = [[S.sbuf(f"go{z}{i}", [128, D], F32) for i in range(2)] for z in range(2)]
            W = {}
            for a in range(4):
                for nm, sh, dt in (("e1", [128, 128], F32), ("sp", [128, 128], F32), ("cum", [128, 128], F32),
                                   ("Eq", [128, 128], F32), ("Ek", [128, 128], F32), ("El", [128, 128], F32),
                                   ("sm", [128, 4], F32), ("qe", [128, 128], BF16), ("ke", [128, 128], BF16),
                                   ("kl", [128, 128], BF16), ("klT", [128, 128], BF16), ("attm", [128, 128], BF16)):
                    W[(nm, a)] = S.sbuf(f"g{nm}{a}", sh, dt)
            order = [[32, 33] + list(range(32)), [33, 32] + list(range(31, -1, -1))]
            na = 0
            for step in range(NT):
                for z in range(2):
                    i = order[z][step]
                    sl = step % 2
                    st_i = i // 4 if i < NTL else 8
                    cs_ = slice(i * 128, (i + 1) * 128)
                    S.dma(S.sp, qts[z][sl][:], QTv[:, :, cs_], qts[z][sl], reads=[self.QTs[st_i]], writes=[qts[z][sl]])
                    S.dma(S.sp, kts[z][sl][:], KTv[:, :, cs_], kts[z][sl], reads=[self.KTs[st_i]], writes=[kts[z][sl]])
                    S.dma(S.sp, vts[z][sl][:], self.V[cs_, :], vts[z][sl], reads=[self.Vt[i]], writes=[vts[z][sl]])
                    S.dma(S.sp, gts[z][sl][:], self.GS[z, :, cs_], gts[z][sl], reads=[self.GSs[st_i]], writes=[gts[z][sl]])
                for h in range(4):
                    for z in range(2):
                        i = order[z][step]
                        sl = step % 2
                        a = na % 4
                        na += 1
                        qt, kt, vt, gt, ot = qts[z][sl], kts[z][sl], vts[z][sl], gts[z][sl], osb[z][sl]
                        e1, sp, cum, Eq, Ek, El, sm = (W[(n_, a)] for n_ in ("e1", "sp", "cum", "Eq", "Ek", "El", "sm"))
                        qe, ke, kl, klT, attm = (W[(n_, a)] for n_ in ("qe", "ke", "kl", "klT", "attm"))
                        if a < 3:
                            bA, bB = self.ps[a * 2], self.ps[a * 2 + 1]
                            vA, vB, vT = bA.t[:], bB.t[:], bA.t[:].bitcast(BF16)
                        else:
                            bA, bB = self.pb[0], self.pb[1]
                            vA, vB, vT = bA.t[:].bitcast(F32), bB.t[:].bitcast(F32), bA.t[:]
                        pla = patt = ptb = bA
                        pos = bB
                        s32, sb_ = S32[z][h], Sb[z][h]
                        S.op(S.pe, lambda e, z=z, h=h, gt=gt, vA=vA: e.matmul(vA[:, 0:128], wg[:, z, h * 128:(h + 1) * 128], gt[:], start=True, stop=True),
                             reads=[wg, gt], writes=[pla])
                        S.op(S.act, lambda e, e1=e1, vA=vA, z=z, h=h: e.activation(e1[:], vA[:, 0:128], AF.Exp, bias=negb[:, z, h:h + 1], scale=-1.0),
                             reads=[pla, negb], writes=[e1])
                        S.op(S.act, lambda e, e1=e1, sp=sp: e.activation(sp[:], e1[:], AF.Ln, bias=1.0, scale=1.0), reads=[e1], writes=[sp])
                        S.op(S.dve, lambda e, cum=cum, sp=sp: e.tensor_tensor_scan(cum[:], ones[:], sp[:], 0.0, ALU.mult, ALU.add), reads=[ones, sp], writes=[cum])
                        S.op(S.dve, lambda e, sm=sm, cum=cum: e.tensor_copy(sm[:, 0:1], cum[:, 127:128]), reads=[cum], writes=[sm])
                        S.op(S.dve, lambda e, sm=sm: e.tensor_scalar_mul(sm[:, 1:2], sm[:, 0:1], -1.0 / 16.0), reads=[sm], writes=[sm])
                        if z == 1:
                            S.op(S.dve, lambda e, cum=cum, sm=sm: e.tensor_scalar(cum[:], cum[:], -1.0, sm[:, 0:1], ALU.mult, ALU.add), reads=[cum, sm], writes=[cum])
                            S.op(S.dve, lambda e, cum=cum, sp=sp: e.tensor_tensor(cum[:], cum[:], sp[:], ALU.add), reads=[cum, sp], writes=[cum])
                        S.op(S.act, lambda e, Eq=Eq, cum=cum: e.activation(Eq[:], cum[:], AF.Exp, scale=-1.0 / 16.0), reads=[cum], writes=[Eq])
                        S.op(S.act, lambda e, Ek=Ek, cum=cum: e.activation(Ek[:], cum[:], AF.Exp, scale=1.0 / 16.0), reads=[cum], writes=[Ek])
                        S.op(S.act, lambda e, El=El, cum=cum, sm=sm: e.activation(El[:], cum[:], AF.Exp, bias=sm[:, 1:2], scale=1.0 / 16.0), reads=[cum, sm], writes=[El])
                        S.op(S.act, lambda e, sm=sm: e.activation(sm[:, 2:3], sm[:, 0:1], AF.Exp, scale=-1.0 / 16.0), reads=[sm], writes=[sm])
                        S.op(S.dve, lambda e, qe=qe, qt=qt, Eq=Eq, h=h: e.tensor_tensor(qe[:], qt[:, h, :], Eq[:], ALU.mult), reads=[qt, Eq], writes=[qe])
                        S.op(S.pool, lambda e, ke=ke, kt=kt, Ek=Ek, h=h: e.tensor_tensor(ke[:], kt[:, h, :], Ek[:], ALU.mult), reads=[kt, Ek], writes=[ke])
                        S.op(S.pool, lambda e, kl=kl, kt=kt, El=El, h=h: e.tensor_tensor(kl[:], kt[:, h, :], El[:], ALU.mult), reads=[kt, El], writes=[kl])
                        S.op(S.pe, lambda e, kl=kl, vT=vT: e.transpose(vT[:, 512:640], kl[:], self.idb[:]), reads=[kl, self.idb], writes=[ptb])
                        S.op(S.act, lambda e, klT=klT, vT=vT: e.copy(klT[:], vT[:, 512:640]), reads=[ptb], writes=[klT])
                        S.op(S.pe, lambda e, ke=ke, qe=qe, vA=vA: e.matmul(vA[:, 128:256], ke[:], qe[:], start=True, stop=True), reads=[ke, qe], writes=[patt])
                        S.op(S.dve, lambda e, attm=attm, vA=vA, z=z: e.tensor_tensor(attm[:], vA[:, 128:256], msk[:, z, :], ALU.mult), reads=[patt, msk], writes=[attm])
                        vh = slice(h * 256, (h + 1) * 256)

                        def mmo(e, attm=attm, vt=vt, qe=qe, sb_=sb_, vB=vB, vh=vh):
                            e.matmul(vB[:, 0:256], attm[:], vt[:, vh], start=True, stop=False)
                            return e.matmul(vB[:, 0:256], qe[:], sb_[:], start=False, stop=True)
                        S.op(S.pe, mmo, reads=[attm, vt, qe, sb_], writes=[pos])
                        S.op(S.act, lambda e, ot=ot, vB=vB, vh=vh: e.copy(ot[:, vh], vB[:, 0:256]), reads=[pos], writes=[ot])
                        S.op(S.pe, lambda e, klT=klT, vt=vt, vB=vB, vh=vh: e.matmul(vB[:, 256:512], klT[:], vt[:, vh], start=True, stop=True),
                             reads=[klT, vt], writes=[pos])
                        S.op(S.dve, lambda e, s32=s32, sm=sm, vB=vB: e.scalar_tensor_tensor(s32[:], s32[:], sm[:, 2:3], vB[:, 256:512], ALU.mult, ALU.add),
                             reads=[s32, sm, pos], writes=[s32])
                        S.op(S.act, lambda e, sb_=sb_, s32=s32: e.copy(sb_[:], s32[:]), reads=[s32], writes=[sb_])
                for z in range(2):
                    i = order[z][step]
                    ot = osb[z][step % 2]
                    S.dma(S.sp, self.OF[z, i * 128:(i + 1) * 128, :], ot[:], ot, reads=[ot], writes=[self.OFt[z][i]])
        with S.scope():
            wout = S.sbuf("gwout", [128, 8, D], BF16)
            S.dma(S.pool, wout[:], I["gla_w_out"][j].rearrange("(c p) n -> p c n", p=128), wout, writes=[wout])
            ngb = S.sbuf("ngb", [128, 256], F32)
            g1 = S.sbuf("g1", [128, D], F32)
            lng = S.sbuf("lng", [128, D], F32)
            lnb = S.sbuf("lnb", [128, D], F32)
            self.load_vec(ngb, I["gla_norm_g"][j])
            self.load_vec(lng, I["ln_g"][l, 0])
            self.load_vec(lnb, I["ln_b"][l, 0])
            xb = [S.sbuf(f"xb{i}", [128, D], F32) for i in range(2)]
            zb = [S.sbuf(f"zb{i}", [128, D], F32) for i in range(2)]
            ob = [S.sbuf(f"ob{i}", [128, D], F32) for i in range(2)]
            ofb = [S.sbuf(f"ofb{i}", [128, D], F32) for i in range(2)]
            obb = [S.sbuf(f"obb{i}", [128, D], F32) for i in range(2)]
            rb = [S.sbuf(f"rb{i}", [128, D], BF16) for i in range(2)]
            sr = S.sbuf("sr", [128, D], F32)
            junk = S.sbuf("junk", [128, 256], F32)
            sms = [S.sbuf(f"sml{i}", [128, 16], F32) for i in range(2)]
            st8 = [S.sbuf(f"st8{i}", [128, 8], F32) for i in range(2)]
            cats = [S.sbuf(f"cat{i}", [128, D], BF16) for i in range(2)]
            catT = S.sbuf("catT", [128, 8, 128], BF16)
            ntile = NTL if last else NT
            kind = None
            for i in range(ntile):
                k2 = 1 if i >= NTL else 0
                if k2 != kind:
                    kind = k2
                    self.load_mod(g1, l, kind, 2)
                x, z_, o, sm = xb[i % 2], zb[i % 2], ob[i % 2], sms[i % 2]
                of_, ob_, r_, st, cat = ofb[i % 2], obb[i % 2], rb[i % 2], st8[i % 2], cats[i % 2]
                S.dma(S.sp, x[:], self.xsrc(i).t, x, reads=[self.xsrc(i)], writes=[x])
                S.dma(S.sp, of_[:], self.OF[0, i * 128:(i + 1) * 128, :], of_, reads=[self.OFt[0][i]], writes=[of_])
                S.dma(S.sp, ob_[:], self.OF[1, i * 128:(i + 1) * 128, :], ob_, reads=[self.OFt[1][i]], writes=[ob_])
                S.dma(S.sp, r_[:], self.U[i * 128:(i + 1) * 128, :], r_, reads=[self.Ut[i]], writes=[r_])
                S.op(S.dve, lambda e, of_=of_, ob_=ob_: e.tensor_tensor(of_[:], of_[:], ob_[:], ALU.add), reads=[of_, ob_], writes=[of_])
                for h in range(4):
                    S.op(S.act, lambda e, of_=of_, st=st, h=h: e.activation(junk[:], of_[:, h * 256:(h + 1) * 256], AF.Square, accum_out=st[:, h:h + 1]),
                         reads=[of_], writes=[junk, st])
                S.op(S.dve, lambda e, st=st: e.tensor_scalar(st[:, 4:8], st[:, 0:4], 1.0 / 256.0, 1e-6, ALU.mult, ALU.add), reads=[st], writes=[st])
                S.op(S.act, lambda e, st=st: e.sqrt(st[:, 4:8], st[:, 4:8]), reads=[st], writes=[st])
                S.op(S.dve, lambda e, st=st: e.reciprocal(st[:, 4:8], st[:, 4:8]), reads=[st], writes=[st])
                S.op(S.act, lambda e, r_=r_: e.activation(sr[:], r_[:], AF.Silu), reads=[r_], writes=[sr])
                for h in range(4):
                    hs = slice(h * 256, (h + 1) * 256)
                    S.op(S.dve, lambda e, of_=of_, st=st, h=h, hs=hs: e.scalar_tensor_tensor(of_[:, hs], of_[:, hs], st[:, 4 + h:5 + h], ngb[:], ALU.mult, ALU.mult),
                         reads=[of_, st, ngb], writes=[of_])
                S.op(S.dve, lambda e, of_=of_, cat=cat: e.tensor_tensor(cat[:], of_[:], sr[:], ALU.mult), reads=[of_, sr], writes=[cat])
                self.out_proj_ln(l, i, cat, catT, wout, g1, x, z_, o, sm, lng, lnb)
        self.src_is_input = False

    def build(self):
        cfg = self.cfg
        layers = cfg.get("layers", list(range(DEPTH)))
        self.adaln(layers)
        for l in layers:
            last = l == DEPTH - 1
            if cfg.get("mixer", True):
                if l % 2 == 0:
                    self.mixer_even(l, last)
                else:
                    self.mixer_odd(l, last)
            if cfg.get("ffn", True):
                self.ffn(l, last)
        if cfg.get("dump_mod"):
            S = self.S
            tm = self.tap("moddump", [DEPTH, 2, 6 * D])
            with S.scope():
                mb = S.sbuf("mdump", [2, 6 * D], F32)
                for l in layers:
                    S.dma(S.sp, mb[:], self.MOD[l], mb, reads=[self.MODb[l]], writes=[mb])
                    S.dma(S.sp, tm[l], mb[:], mb, reads=[mb], is_output=True)
        if cfg.get("dump_x"):
            S = self.S
            tx = self.tap("xdump", [T, D])
            with S.scope():
                xb = [S.sbuf(f"dx{i}", [128, D], F32) for i in range(2)]
                for i in range(NT):
                    b = xb[i % 2]
                    S.dma(S.sp, b[:], self.Xt[i].t, b, reads=[self.Xt[i]], writes=[b])
                    S.dma(S.sp, tx[i * 128:(i + 1) * 128, :], b[:], b, reads=[b], is_output=True)
        self.S.finish()
        return self.nc


def na_bias_tables(rpb):
    out = np.full((rpb.shape[0], 5, 12, 128, 576), -30000.0, np.float32)
    qi = np.arange(128)
    ki = np.arange(576)
    for p, i in enumerate((0, 1, 2, 30, 31)):
        r0 = 2 * i
        kr0 = min(max(2 * i - 4, 0), 55)
        r = r0 + qi // 64
        col = qi % 64
        krow = kr0 + ki // 64
        kcol = ki % 64
        rs = np.clip(r - 4, 0, 56)
        ws = np.clip(col - 8, 0, 48)
        ok = ((krow[None, :] >= rs[:, None]) & (krow[None, :] < rs[:, None] + 8) &
              (kcol[None, :] >= ws[:, None]) & (kcol[None, :] < ws[:, None] + 16))
        drow = np.clip(krow[None, :] - r[:, None] + 7, 0, 14)
        dcol = np.clip(kcol[None, :] - col[:, None] + 15, 0, 30)
        g = rpb[:, :, drow, dcol]
        out[:, p] = np.where(ok[None, None], g, np.float32(-30000.0))
    return out


def pool_matrices():
    A = np.zeros((4, 5, 128, 128), np.float64)
    Tseq = 128 * 4
    for g, w in enumerate((2, 4, 8, 16)):
        for var, (ti, nb) in enumerate(((1, -1), (1, 0), (1, 1), (0, 0), (3, 0))):
            for tl in range(128):
                t = ti * 128 + tl
                lo = min(max(t - w // 2, 0), Tseq)
                hi = min(max(t - w // 2 + w, 0), Tseq)
                for sg in range(lo, hi):
                    sl = sg - (ti + nb) * 128
                    if 0 <= sl < 128:
                        A[g, var, sl, tl] += 1.0 / (hi - lo)
                if nb == 0:
                    A[g, var, tl, tl] -= 1.0
    return np.ascontiguousarray(A.reshape(20, 128, 128).astype(np.float32))


def rope_tables():
    t = np.arange(TL)
    row = (t // GW).astype(np.float32)
    col = (t % GW).astype(np.float32)
    nf = 32
    freqs = (np.float32(10000.0) ** (-np.arange(nf, dtype=np.float32) / nf)).astype(np.float32)
    ang = np.concatenate([row[:, None] * freqs, col[:, None] * freqs], axis=-1).astype(np.float32)
    return np.ascontiguousarray(np.stack([np.cos(ang), np.sin(ang)], axis=1).astype(np.float32))


def tri_masks():
    s_ = np.arange(128)[:, None]
    t_ = np.arange(128)[None, :]
    return np.ascontiguousarray(np.stack([(s_ <= t_), (s_ >= t_), (s_ < t_)], 0).astype(np.float32))


def host_inputs(inputs, cores):
    f = lambda a: np.ascontiguousarray(np.asarray(a, dtype=np.float32))
    shared = {
        "ada_w": f(inputs["ada_w"]), "ada_b": f(inputs["ada_b"]),
        "ln_g": f(inputs["ln_g"]), "ln_b": f(inputs["ln_b"]),
        "router_w": f(inputs["router_w"]), "router_b": f(inputs["router_b"]),
        "exp_w1": f(inputs["exp_w1"]), "exp_w2": f(inputs["exp_w2"]), "exp_b2": f(inputs["exp_b2"]),
        "exp_b1L": f(np.asarray(inputs["exp_b1"]).reshape(DEPTH, NE, 16, 128).transpose(0, 3, 1, 2)),
        "ident": np.eye(128, dtype=np.float32),
        "ab_w_in": f(inputs["ab_w_in"]), "ab_pool_w": f(inputs["ab_pool_w"]), "ab_pool_scale": f(inputs["ab_pool_scale"]),
        "ab_w_out": f(inputs["ab_w_out"]), "nabias": na_bias_tables(np.asarray(inputs["ab_rpb"], dtype=np.float32)),
        "poolA": pool_matrices(),
        "gla_w_in": f(inputs["gla_w_in"]), "gla_w_gate": f(inputs["gla_w_gate"]),
        "gla_bgL": f(np.asarray(inputs["gla_b_gate"]).reshape(2, 2, 4, 128).transpose(0, 3, 1, 2)),
        "gla_norm_g": f(inputs["gla_norm_g"]), "gla_w_out": f(inputs["gla_w_out"]),
        "ropecs": rope_tables(), "trimask": tri_masks(),
        "blkstart": (np.arange(128, dtype=np.float32) * BLK).reshape(128, 1),
        "rowbase": np.ascontiguousarray(np.concatenate([np.arange(8)[None, :] * 128 + np.arange(128)[:, None],
                                                        np.arange(128)[:, None]], axis=1).astype(np.float32)),
        "exp_b1T": f(np.asarray(inputs["exp_b1"]).reshape(DEPTH, NE, 16, 128).transpose(0, 1, 3, 2).reshape(DEPTH, NE * 128, 16)),
    }
    maps = []
    cc = np.asarray(inputs["c_ctx"], dtype=np.float32).reshape(8, 128).T
    for b in cores:
        m = dict(shared)
        m["xc"] = f(np.concatenate([inputs["x"][b], inputs["ctx"][b]], axis=0))
        cb = np.asarray(inputs["c"][b], dtype=np.float32).reshape(8, 128).T
        m["cvec"] = f(np.stack([cb, cc], axis=-1))
        maps.append(m)
    return maps


def kernel(**inputs):
    prog = Prog({})
    nc = prog.build()
    maps = host_inputs(inputs, list(range(8)))
    res = run_bass_kernel_spmd(nc, maps, core_ids=list(range(8)))
    out = np.stack([np.asarray(r["out"]) for r in res.results], axis=0)
    return out.astype(np.float32)
```

```python
import contextlib
import numpy as np
import concourse.bass as bass
import concourse.mybir as mybir
from concourse.bass_utils import run_bass_kernel_spmd

F32 = mybir.dt.float32
BF16 = mybir.dt.bfloat16
AF = mybir.ActivationFunctionType
ALU = mybir.AluOpType
AX = mybir.AxisListType

D = 1024
TL = 4096
NCTX = 256
T = TL + NCTX
NT = T // 128
NTL = TL // 128
DEPTH = 4
ALPHA = (2.0 * DEPTH) ** 0.25
NE = 32
GW = 64
BLK = 512
NBLK = (T * 4 + NE * (BLK - 1)) // BLK + 1
I32 = mybir.dt.int32

SAME_ENGINE_SYNC = True


class Buf:
    __slots__ = ("name", "t", "w", "r", "dsem", "dcnt", "excl")

    def __init__(self, name, t):
        self.excl = False
        self.name = name
        self.t = t
        self.w = None
        self.r = {}
        self.dsem = None
        self.dcnt = 0

    def __getitem__(self, k):
        return self.t[k]


class Eng:
    def __init__(self, name, h, sem):
        self.name = name
        self.h = h
        self.sem = sem
        self.cnt = 0
        self.seen = {}


class Sched:
    def __init__(self, nc):
        self.nc = nc
        self.es = contextlib.ExitStack()
        self.nsem = 0
        self.dma_latest = {}
        self.free_dsems = []
        self.free_dsems_sw = []
        self.sw_sems = set()
        self.scope_bufs = [[]]
        self.pe = self._eng("pe", nc.tensor)
        self.act = self._eng("act", nc.scalar)
        self.dve = self._eng("dve", nc.vector)
        self.pool = self._eng("pool", nc.gpsimd)
        self.sp = self._eng("sp", nc.sync)
        self.engs = [self.pe, self.act, self.dve, self.pool, self.sp]
        self.out_toks = []
        self.n_ins = 0

    def new_sem(self, name):
        self.nsem += 1
        return self.es.enter_context(self.nc.semaphore(f"{name}_{self.nsem}"))

    def _eng(self, name, h):
        return Eng(name, h, self.new_sem("e" + name))

    def sbuf(self, name, shape, dtype):
        st = self.scope_stacks[-1] if getattr(self, "scope_stacks", None) else self.es
        self.nbuf = getattr(self, "nbuf", 0) + 1
        t = st.enter_context(self.nc.sbuf_tensor(f"{name}_{self.nbuf}", list(shape), dtype))
        b = Buf(name, t)
        self.scope_bufs[-1].append(b)
        return b

    def psum(self, name, shape, dtype):
        t = self.es.enter_context(self.nc.psum_tensor(name, list(shape), dtype))
        b = Buf(name, t)
        b.excl = True
        return b

    def dram(self, name, shape, dtype):
        t = self.nc.dram_tensor(name, list(shape), dtype, kind="Internal")
        return t.ap()

    def view(self, name, ap):
        return Buf(name, ap)

    @contextlib.contextmanager
    def scope(self):
        if not getattr(self, "scope_stacks", None):
            self.scope_stacks = []
        st = contextlib.ExitStack()
        self.scope_stacks.append(st)
        self.scope_bufs.append([])
        try:
            yield
        finally:
            self.barrier()
            for b in self.scope_bufs.pop():
                if b.dsem is not None:
                    for sw, ent in b.dsem.items():
                        (self.free_dsems_sw if sw else self.free_dsems).append((ent[0], ent[1]))
                    b.dsem = None
            self.scope_stacks.pop()
            st.close()

    def _wait(self, eng, sem, val):
        if sem in self.dma_latest:
            val = max(val, self.dma_latest[sem])
        if sem is eng.sem and not SAME_ENGINE_SYNC:
            return
        if eng.seen.get(sem, 0) >= val:
            return
        eng.h.wait_ge(sem, val)
        eng.seen[sem] = val
        self.n_ins += 1

    def _deps(self, eng, reads, writes, skip_sem=None):
        for b in reads:
            if b.w is not None:
                self._wait(eng, *b.w)
        for b in writes:
            if b.w is not None and b.w[0] is not skip_sem:
                self._wait(eng, *b.w)
            for s, v in b.r.items():
                self._wait(eng, s, v)

    def _commit(self, tok, reads, writes):
        for b in writes:
            b.w = tok
            b.r = {}
        s, v = tok
        for b in reads:
            if b in writes:
                continue
            if b.r.get(s, 0) < v:
                b.r[s] = v

    def op(self, eng, fn, reads=(), writes=()):
        ex = [b for b in reads if b.excl and b not in writes]
        if ex:
            writes = list(writes) + ex
            reads = [b for b in reads if not b.excl]
        self._deps(eng, reads, writes)
        ins = fn(eng.h)
        eng.cnt += 1
        ins.then_inc(eng.sem, 1)
        self.n_ins += 1
        self._commit((eng.sem, eng.cnt), reads, writes)

    def dma(self, q, out_ap, in_ap, sb, reads=(), writes=(), is_output=False, **kw):
        sw = q is self.pool
        cur = sb.dsem[sw][0] if (sb.dsem and sw in sb.dsem) else None
        self._deps(q, reads, writes, skip_sem=cur if sb in writes else None)
        sem, cnt = self.dma_sem(sb, sw)
        q.h.dma_start(out=out_ap, in_=in_ap, **kw).then_inc(sem, 16)
        self.n_ins += 1
        tok = (sem, cnt)
        self._commit(tok, reads, writes)
        if is_output:
            self.out_toks.append(tok)

    def dma_sem(self, sb, sw):
        if sb.dsem is None:
            sb.dsem = {}
        if sw not in sb.dsem:
            fl = self.free_dsems_sw if sw else self.free_dsems
            if fl:
                sb.dsem[sw] = list(fl.pop())
            else:
                sb.dsem[sw] = [self.new_sem("dsw" if sw else "d"), 0]
                if sw:
                    self.sw_sems.add(sb.dsem[sw][0])
        ent = sb.dsem[sw]
        ent[1] += 16
        self.dma_latest[ent[0]] = ent[1]
        return ent[0], ent[1]

    def idma(self, out_ap, in_ap, sb, idx_ap, scatter, bound, reads=(), writes=()):
        q = self.pool
        cur = sb.dsem[True][0] if (sb.dsem and True in sb.dsem) else None
        self._deps(q, reads, writes, skip_sem=cur if sb in writes else None)
        sem, cnt = self.dma_sem(sb, True)
        off = bass.IndirectOffsetOnAxis(ap=idx_ap, axis=0)
        q.h.indirect_dma_start(out=out_ap, out_offset=off if scatter else None, in_=in_ap,
                               in_offset=None if scatter else off).then_inc(sem, 16)
        self.n_ins += 1
        self._commit((sem, cnt), reads, writes)

    def barrier(self):
        for e in self.engs:
            for f in self.engs:
                if f is not e and f.cnt > 0:
                    self._wait(e, f.sem, f.cnt)
            for s, v in self.dma_latest.items():
                self._wait(e, s, v)

    def finish(self):
        self.barrier()

    def close(self):
        self.es.close()


class LazyInputs(dict):
    def __init__(self, prog):
        super().__init__()
        self.prog = prog

    def __missing__(self, name):
        shape = list(self.prog.shapes[name])
        dev = self.prog.cfg.get("dev_slice")
        if dev and name in dev:
            shape = list(dev[name])
        ap = self.prog.nc.dram_tensor(name, shape, F32, kind="ExternalInput").ap()
        self[name] = ap
        return ap


class Prog:
    def __init__(self, cfg):
        self.cfg = cfg
        nc = self.nc = bass.Bass("TRN2", target_bir_lowering=False)
        S = self.S = Sched(nc)
        self.taps = {}

        self.shapes = {
            "xc": [T, D], "cvec": [128, 8, 2], "ada_w": [DEPTH, D, 6 * D], "ada_b": [DEPTH, 6 * D],
            "ln_g": [DEPTH, 2, D], "ln_b": [DEPTH, 2, D], "router_w": [DEPTH, D, NE], "router_b": [DEPTH, NE],
            "exp_w1": [DEPTH, NE, D, 2 * D], "exp_b1L": [DEPTH, 128, NE, 16], "exp_w2": [DEPTH, NE, D, D],
            "exp_b2": [DEPTH, NE, D], "ident": [128, 128],
            "ab_w_in": [2, D, 2560], "ab_pool_w": [2, 4, 64, 64], "ab_pool_scale": [2, 256],
            "ab_w_out": [2, D, D], "nabias": [2, 5, 12, 128, 576], "poolA": [20, 128, 128],
            "gla_w_in": [2, D, 3104], "gla_w_gate": [2, 2, 16, 512], "gla_bgL": [2, 128, 2, 4],
            "gla_norm_g": [2, 256], "gla_w_out": [2, D, D], "ropecs": [TL, 2, 64], "trimask": [3, 128, 128], "blkstart": [128, 1], "rowbase": [128, 9], "exp_b1T": [DEPTH, NE * 128, 16],
        }
        self.I = LazyInputs(self)
        self.out = nc.dram_tensor("out", [TL, D], F32, kind="ExternalOutput").ap()

        self.X = S.dram("X", [T, D], F32)
        self.Xt = [S.view(f"X{i}", self.X[i * 128:(i + 1) * 128, :]) for i in range(NT)]
        self.XCt = [S.view(f"XC{i}", self.I["xc"][i * 128:(i + 1) * 128, :]) for i in range(NT)]
        self.OUTt = [S.view(f"O{i}", self.out[i * 128:(i + 1) * 128, :]) for i in range(NTL)]
        self.MOD = S.dram("MOD", [DEPTH, 2, 6 * D], F32)
        self.MODb = [S.view(f"MOD{l}", self.MOD[l]) for l in range(DEPTH)]
        self.src_is_input = True
        self.QT = S.dram("QT", [768, T], BF16)
        self.KT = S.dram("KT", [768, T], BF16)
        self.V = S.dram("V", [T, 1024], BF16)
        self.U = S.dram("U", [T, 1024], BF16)
        self.XS = S.dram("XS", [NBLK * BLK, D], BF16)
        self.XSb = [S.view(f"XS{b}", self.XS) for b in range(NBLK)]
        self.XSall = S.view("XSall", self.XS)
        self.YS = S.dram("YS", [NBLK * BLK, D], F32)
        self.YSb = [S.view(f"YS{b}", self.YS) for b in range(NBLK)]
        self.HB = S.dram("HB", [T, D], BF16)
        self.HBt = [S.view(f"HB{i}", self.HB) for i in range(NT)]
        self.EB = S.dram("EB", [128, 1], I32)
        self.EBb = S.view("EBb", self.EB)
        self.xs_zeroed = False
        self.OF = S.dram("OF", [2, T, D], F32)
        self.OFt = [[S.view(f"OF{z}_{i}", self.OF) for i in range(NT)] for z in range(2)]
        self.GS = S.dram("GS", [2, 16, T], F32)
        self.GSs = [S.view(f"GS{i}", self.GS) for i in range(9)]
        self.QTs = [S.view(f"QT{i}", self.QT) for i in range(9)]
        self.KTs = [S.view(f"KT{i}", self.KT) for i in range(9)]
        self.Vt = [S.view(f"V{i}", self.V) for i in range(NT)]
        self.Ut = [S.view(f"U{i}", self.U) for i in range(NT)]

        self.idf = S.sbuf("idf", [128, 128], F32)
        self.idb = S.sbuf("idb", [128, 128], BF16)
        S.dma(S.sp, self.idf[:], self.I["ident"], self.idf, writes=[self.idf])
        S.op(S.dve, lambda e: e.tensor_copy(self.idb[:], self.idf[:]), reads=[self.idf], writes=[self.idb])
        self.ps = [S.psum(f"ps{i}", [128, 512], F32) for i in range(6)]
        self.pb = [S.psum(f"pb{i}", [128, 1024], BF16) for i in range(2)]

    def tap(self, name, shape, dt=F32):
        ap = self.nc.dram_tensor(name, list(shape), dt, kind="ExternalOutput").ap()
        self.taps[name] = ap
        return ap

    def xsrc(self, i):
        return self.XCt[i] if self.src_is_input else self.Xt[i]

    def adaln(self, layers):
        S, I = self.S, self.I
        with S.scope():
            cv = S.sbuf("cv", [128, 8, 2], F32)
            sc = S.sbuf("sc", [128, 8, 2], F32)
            S.dma(S.sp, cv[:], I["cvec"], cv, writes=[cv])
            S.op(S.act, lambda e: e.activation(sc[:], cv[:], AF.Silu), reads=[cv], writes=[sc])
            wts = [S.sbuf(f"adw{i}", [128, 8, 512], F32) for i in range(2)]
            bts = [S.sbuf(f"adb{i}", [2, 512], F32) for i in range(2)]
            msb = S.sbuf("msb", [2, 6 * D], F32)
            n = 0
            for l in layers:
                wv = I["ada_w"][l].rearrange("(c p) n -> p c n", p=128)
                for blk in range(12):
                    wt, bt = wts[n % 2], bts[n % 2]
                    ps = self.ps[n % 2]
                    n += 1
                    cs = slice(blk * 512, (blk + 1) * 512)
                    S.dma(S.sp, wt[:], wv[:, :, cs], wt, writes=[wt])
                    S.dma(S.sp, bt[:], I["ada_b"][l, cs].partition_broadcast(2), bt, writes=[bt])

                    def mm(e, wt=wt, ps=ps):
                        for k in range(8):
                            ins = e.matmul(ps[0:2, :], sc[:, k, :], wt[:, k, :], start=(k == 0), stop=(k == 7))
                        return ins
                    S.op(S.pe, mm, reads=[sc, wt], writes=[ps])
                    S.op(S.dve, lambda e, ps=ps, bt=bt, cs=cs: e.tensor_tensor(msb[:, cs], ps[0:2, :], bt[:], ALU.add),
                         reads=[ps, bt], writes=[msb])
                for j in (1, 4):
                    S.op(S.dve, lambda e, j=j: e.tensor_scalar_add(msb[:, j * D:(j + 1) * D], msb[:, j * D:(j + 1) * D], 1.0),
                         reads=[msb], writes=[msb])
                S.dma(S.sp, self.MOD[l], msb[:], msb, reads=[msb], writes=[self.MODb[l]])

    def load_mod(self, dst, l, which, j):
        S = self.S
        S.dma(S.sp, dst[:], self.MOD[l, which, j * D:(j + 1) * D].partition_broadcast(128), dst,
              reads=[self.MODb[l]], writes=[dst])

    def load_vec(self, dst, ap1d):
        S = self.S
        S.dma(S.sp, dst[:], ap1d.partition_broadcast(128), dst, writes=[dst])

    def layernorm(self, z, o, lng, lnb, sm):
        S = self.S

        S.op(S.dve, lambda e: e.bn_stats(sm[:, 0:6], z[:, 0:512]), reads=[z], writes=[sm])
        S.op(S.dve, lambda e: e.bn_stats(sm[:, 6:12], z[:, 512:1024]), reads=[z], writes=[sm])
        S.op(S.dve, lambda e: e.bn_aggr(sm[:, 12:14], sm[:, 0:12]), reads=[sm], writes=[sm])
        S.op(S.dve, lambda e: e.tensor_scalar_add(sm[:, 14:15], sm[:, 13:14], 1e-5), reads=[sm], writes=[sm])
        S.op(S.act, lambda e: e.sqrt(sm[:, 14:15], sm[:, 14:15]), reads=[sm], writes=[sm])
        S.op(S.dve, lambda e: e.reciprocal(sm[:, 14:15], sm[:, 14:15]), reads=[sm], writes=[sm])
        S.op(S.dve, lambda e: e.tensor_scalar(sm[:, 15:16], sm[:, 12:13], sm[:, 14:15], -1.0, ALU.mult, ALU.mult),
             reads=[sm], writes=[sm])
        S.op(S.act, lambda e: e.activation(o[:], z[:], AF.Identity, bias=sm[:, 15:16], scale=sm[:, 14:15]),
             reads=[z, sm], writes=[o])
        S.op(S.dve, lambda e: e.tensor_tensor(o[:], o[:], lng[:], ALU.mult), reads=[o, lng], writes=[o])
        S.op(S.dve, lambda e: e.tensor_tensor(o[:], o[:], lnb[:], ALU.add), reads=[o, lnb], writes=[o])

    def ffn(self, l, last):
        S, I = self.S, self.I
        if self.cfg.get("moe", "sparse") == "sparse":
            self.ffn_sparse(l, last)
            self.src_is_input = False
            return
        ntiles = NTL if last else NT
        groups = []
        t = 0
        while t < ntiles:
            groups.append((t, min(t + 12, ntiles)))
            t += 12
        for (t0, t1) in groups:
            self.ffn_group(l, last, t0, t1)
        self.src_is_input = False

    def ffn_sparse(self, l, last):
        S, I = self.S, self.I
        ntile = NTL if last else NT
        nblk = (ntile * 128 * 4 + NE * (BLK - 1)) // BLK + 1
        lw = 0 if "exp_w1" in (self.cfg.get("dev_slice") or {}) else l
        with S.scope():
            G = S.sbuf("G", [128, ntile, NE], F32)
            G4 = S.sbuf("G4", [128, ntile, 4], F32)
            D4 = S.sbuf("D4", [128, ntile * 4], I32)
            IDX = S.sbuf("IDX", [128, 128 * 8], I32)
            IDXB = S.sbuf("IDXB", [128, 128], I32)
            if not self.xs_zeroed:
                self.xs_zeroed = True
                with S.scope():
                    zt = S.sbuf("zt", [128, 4, D], BF16)
                    S.op(S.dve, lambda e: e.memset(zt[:], 0.0), writes=[zt])
                    for b in range(NBLK):
                        S.dma(S.sp, self.XS[b * BLK:(b + 1) * BLK, :].rearrange("(c p) d -> p c d", p=128), zt[:], zt, reads=[zt], writes=[self.XSb[b]])
            with S.scope():
                MK = S.sbuf("MK", [128, ntile, NE], F32)
                RK = S.sbuf("RK", [128, ntile, NE], F32)
                s2 = S.sbuf("s2", [128, D], F32)
                h2 = S.sbuf("h2", [128, D], F32)
                rw = S.sbuf("rw", [128, 8, NE], F32)
                rb = S.sbuf("rb", [128, NE], F32)
                tri = S.sbuf("tri", [128, 128], F32)
                ustr = S.sbuf("ustr", [128, 128], BF16)
                onesb = S.sbuf("onesb", [128, 128], BF16)
                onesf = S.sbuf("onesf", [128, NE], F32)
                blk = S.sbuf("blk", [128, 1], F32)
                cnt = S.sbuf("cnt", [128, NE], F32)
                xb = [S.sbuf(f"xb{i}", [128, D], F32) for i in range(2)]
                h32 = [S.sbuf(f"h32{i}", [128, D], F32) for i in range(2)]
                hbs = [S.sbuf(f"hbs{i}", [128, D], BF16) for i in range(2)]
                hT32s = [S.sbuf(f"hT32{i}", [128, 8, 128], F32) for i in range(2)]
                sms_ = [S.sbuf(f"smr{i}", [128, 160], F32) for i in range(2)]
                mkbs = [S.sbuf(f"mkb{i}", [128, NE], BF16) for i in range(2)]
                sm = sms_[0]
                S.dma(S.sp, rw[:], I["router_w"][l].rearrange("(c p) n -> p c n", p=128), rw, writes=[rw])
                self.load_vec(rb, I["router_b"][l])
                S.dma(S.sp, tri[:], I["trimask"][2], tri, writes=[tri])
                S.dma(S.sp, blk[:], I["blkstart"], blk, writes=[blk])
                S.op(S.dve, lambda e: e.tensor_copy(ustr[:], tri[:]), reads=[tri], writes=[ustr])
                S.op(S.dve, lambda e: e.memset(onesb[:], 1.0), writes=[onesb])
                S.op(S.dve, lambda e: e.memset(onesf[:], 1.0), writes=[onesf])
                S.op(S.dve, lambda e: e.memset(cnt[:], 0.0), writes=[cnt])
                kind = None
                for ti in range(ntile):
                    i = ti
                    k2 = 0 if i < NTL else 1
                    if k2 != kind:
                        kind = k2
                        self.load_mod(s2, l, kind, 4)
                        self.load_mod(h2, l, kind, 3)
                    x, h, hb_ = xb[ti % 2], h32[ti % 2], hbs[ti % 2]
                    hT32, sm, mkb = hT32s[ti % 2], sms_[ti % 2], mkbs[ti % 2]
                    S.dma(S.sp, x[:], self.xsrc(i).t, x, reads=[self.xsrc(i)], writes=[x])
                    S.op(S.dve, lambda e, h=h, x=x: e.tensor_tensor(h[:], x[:], s2[:], ALU.mult), reads=[x, s2], writes=[h])
                    S.op(S.dve, lambda e, h=h: e.tensor_tensor(h[:], h[:], h2[:], ALU.add), reads=[h, h2], writes=[h])
                    S.op(S.act, lambda e, h=h, hb_=hb_: e.copy(hb_[:], h[:]), reads=[h], writes=[hb_])
                    S.dma(S.pool, self.HB[i * 128:(i + 1) * 128, :], hb_[:], hb_, reads=[hb_], writes=[self.HBt[i]])
                    cols = slice(ti * 128, (ti + 1) * 128)
                    for half in range(2):
                        pp = self.ps[half + 2 * (ti % 2)]

                        def tr(e, pp=pp, half=half, h=h):
                            for c in range(4):
                                cc = half * 4 + c
                                ins = e.transpose(pp[:, c * 128:(c + 1) * 128], h[:, cc * 128:(cc + 1) * 128], self.idf[:])
                            return ins
                        S.op(S.pe, tr, reads=[h, self.idf], writes=[pp])
                        S.op(S.act, lambda e, pp=pp, half=half, hT32=hT32: e.copy(hT32[:, half * 4:(half + 1) * 4, :],
                                                                          pp[:].rearrange("p (c t) -> p c t", c=4)),
                             reads=[pp], writes=[hT32])
                    pr = self.ps[4 + ti % 2]

                    def rmm(e, pr=pr, hT32=hT32):
                        for k in range(8):
                            ins = e.matmul(pr[:, 0:NE], hT32[:, k, :], rw[:, k, :], start=(k == 0), stop=(k == 7))
                        return ins
                    S.op(S.pe, rmm, reads=[hT32, rw], writes=[pr])
                    lg, top8, nmx, ex, ssum = sm[:, 0:32], sm[:, 32:40], sm[:, 40:41], sm[:, 48:80], sm[:, 112:113]
                    smB = sm
                    mk = MK[:, ti, :]
                    S.op(S.dve, lambda e: e.tensor_tensor(lg, pr[:, 0:NE], rb[:], ALU.add), reads=[pr, rb], writes=[sm])
                    S.op(S.dve, lambda e: e.max(top8, lg), reads=[sm], writes=[sm])
                    S.op(S.dve, lambda e: e.tensor_scalar_mul(nmx, top8[:, 0:1], -1.0), reads=[sm], writes=[sm])
                    S.op(S.dve, lambda e, mk=mk: e.tensor_scalar(mk, lg, top8[:, 3:4], None, ALU.is_ge), reads=[sm], writes=[MK])
                    S.op(S.act, lambda e: e.activation(ex, lg, AF.Exp, bias=nmx, scale=1.0), reads=[sm], writes=[sm])
                    S.op(S.dve, lambda e, mk=mk: e.tensor_tensor(ex, ex, mk, ALU.mult), reads=[sm, MK], writes=[sm])
                    S.op(S.dve, lambda e: e.reduce_sum(ssum, ex, AX.X), reads=[sm], writes=[sm])
                    S.op(S.dve, lambda e: e.reciprocal(ssum, ssum), reads=[sm], writes=[sm])
                    S.op(S.dve, lambda e, ti=ti: e.tensor_scalar_mul(G[:, ti, :], ex, ssum), reads=[sm], writes=[G])
                    S.op(S.dve, lambda e, mk=mk: e.tensor_copy(mkb[:], mk), reads=[MK], writes=[mkb])
                    pk = self.ps[4]

                    def rkmm(e):
                        e.matmul(pk[:, 0:NE], ustr[:], mkb[:], start=True, stop=True)
                        return e.matmul(pk[:, NE:2 * NE], onesb[:], mkb[:], start=True, stop=True)
                    S.op(S.pe, rkmm, reads=[ustr, onesb, mkb], writes=[pk])
                    S.op(S.dve, lambda e, ti=ti: e.tensor_tensor(RK[:, ti, :], pk[:, 0:NE], cnt[:], ALU.add), reads=[pk, cnt], writes=[RK])
                    S.op(S.dve, lambda e: e.tensor_tensor(cnt[:], cnt[:], pk[:, NE:2 * NE], ALU.add), reads=[pk, cnt], writes=[cnt])
                pad, pend, ps1, tmp = sm[:, 0:32], sm[:, 32:64], sm[:, 64:96], sm[:, 96:128]
                S.op(S.dve, lambda e: e.tensor_single_scalar(tmp, cnt[:], 0.0, ALU.is_gt), reads=[cnt], writes=[sm])
                for m in range(1, (T + BLK - 1) // BLK):
                    S.op(S.dve, lambda e, m=m: e.scalar_tensor_tensor(tmp, cnt[:], float(m * BLK), tmp, ALU.is_gt, ALU.add), reads=[cnt, sm], writes=[sm])
                S.op(S.dve, lambda e: e.tensor_scalar_mul(pad, tmp, float(BLK)), reads=[sm], writes=[sm])
                S.op(S.dve, lambda e: e.tensor_tensor_scan(pend, onesf[:], pad, 0.0, ALU.mult, ALU.add), reads=[sm, onesf], writes=[sm])
                S.op(S.dve, lambda e: e.tensor_tensor(ps1, pend, pad, ALU.subtract), reads=[sm], writes=[sm])
                S.op(S.dve, lambda e: e.tensor_scalar_add(ps1, ps1, 1.0), reads=[sm], writes=[sm])
                S.op(S.dve, lambda e: e.tensor_scalar(tmp, pend, blk[:, 0:1], None, ALU.is_le), reads=[sm, blk], writes=[sm])
                S.op(S.dve, lambda e: e.reduce_sum(sm[:, 128:129], tmp, AX.X), reads=[sm], writes=[sm])
                S.op(S.dve, lambda e: e.tensor_scalar_min(sm[:, 128:129], sm[:, 128:129], float(NE - 1)), reads=[sm], writes=[sm])
                ebi = S.sbuf("ebi", [128, 1], I32)
                S.op(S.dve, lambda e: e.tensor_copy(ebi[:], sm[:, 128:129]), reads=[sm], writes=[ebi])
                S.dma(S.sp, self.EB, ebi[:], ebi, reads=[ebi], writes=[self.EBb])
                ebB = S.sbuf("ebB", [128, 128], I32)
                ebF = S.sbuf("ebF", [128, 128], F32)
                rbase = S.sbuf("rbase", [128, 9], F32)
                idxf = S.sbuf("idxf", [128, 128 * 8], F32)
                S.dma(S.sp, ebB[:], self.EB.rearrange("p o -> (p o)").partition_broadcast(128), ebB, reads=[self.EBb], writes=[ebB])
                S.dma(S.sp, rbase[:], I["rowbase"], rbase, writes=[rbase])
                S.op(S.dve, lambda e: e.tensor_copy(ebF[:], ebB[:]), reads=[ebB], writes=[ebF])
                S.op(S.dve, lambda e: e.scalar_tensor_tensor(idxf[:].rearrange("p (b k) -> p b k", k=8),
                                                             ebF[:].rearrange("p (b o) -> p b o", o=1).to_broadcast([128, 128, 8]), 1024.0,
                                                             rbase[:, 0:8].rearrange("p (o k) -> p o k", o=1).to_broadcast([128, 128, 8]),
                                                             ALU.mult, ALU.add), reads=[ebF, rbase], writes=[idxf])
                if lw:
                    S.op(S.dve, lambda e: e.tensor_scalar_add(idxf[:], idxf[:], float(lw * NE * D)), reads=[idxf], writes=[idxf])
                S.op(S.dve, lambda e: e.tensor_copy(IDX[:], idxf[:]), reads=[idxf], writes=[IDX])
                S.op(S.dve, lambda e: e.tensor_scalar(idxf[:, 0:128], ebF[:], 128.0, rbase[:, 8:9], ALU.mult, ALU.add), reads=[ebF, rbase, idxf], writes=[idxf])
                if l:
                    S.op(S.dve, lambda e: e.tensor_scalar_add(idxf[:, 0:128], idxf[:, 0:128], float(l * NE * 128)), reads=[idxf], writes=[idxf])
                S.op(S.dve, lambda e: e.tensor_copy(IDXB[:], idxf[:, 0:128]), reads=[idxf], writes=[IDXB])
                key, t8, eq, d4f = sm[:, 0:32], sm[:, 96:104], sm[:, 104:136], sm[:, 136:140]
                for ti in range(ntile):
                    hb_ = hbs[ti % 2]
                    S.dma(S.sp, hb_[:], self.HB[ti * 128:(ti + 1) * 128, :], hb_, reads=[self.HBt[ti]], writes=[hb_])
                    S.op(S.dve, lambda e, ti=ti: e.tensor_tensor(key, RK[:, ti, :], ps1, ALU.add), reads=[RK, sm], writes=[sm])
                    S.op(S.dve, lambda e, ti=ti: e.tensor_tensor(key, key, MK[:, ti, :], ALU.mult), reads=[MK, sm], writes=[sm])
                    S.op(S.dve, lambda e: e.max(t8, key), reads=[sm], writes=[sm])
                    S.op(S.dve, lambda e: e.tensor_scalar_add(d4f, t8[:, 0:4], -1.0), reads=[sm], writes=[sm])
                    S.op(S.dve, lambda e, ti=ti: e.tensor_copy(D4[:, ti * 4:ti * 4 + 4], d4f), reads=[sm], writes=[D4])
                    for k in range(4):
                        S.op(S.dve, lambda e, k=k: e.tensor_scalar(eq, key, t8[:, k:k + 1], None, ALU.is_equal), reads=[sm], writes=[sm])
                        S.op(S.dve, lambda e, ti=ti: e.tensor_tensor(eq, eq, G[:, ti, :], ALU.mult), reads=[sm, G], writes=[sm])
                        S.op(S.dve, lambda e, ti=ti, k=k: e.reduce_sum(G4[:, ti, k:k + 1], eq, AX.X), reads=[sm], writes=[G4])
                    for k in range(4):
                        S.idma(self.XS[:, :], hb_[:, :], hb_, D4[:, ti * 4 + k:ti * 4 + k + 1], True, NBLK * BLK - 1,
                               reads=[hb_, D4], writes=[self.XSall] + self.XSb)
            if self.cfg.get("tap_route"):
                tg = self.tap("t_g4", [128, ntile, 4])
                S.dma(S.sp, tg, G4[:], G4, reads=[G4], is_output=True)
                td = self.tap("t_d4", [128, ntile * 4], I32)
                S.dma(S.sp, td, D4[:], D4, reads=[D4], is_output=True)
                te = self.tap("t_eb", [128, 128], I32)
                S.dma(S.sp, te, IDXB[:], IDXB, reads=[IDXB], is_output=True)
            with S.scope():
                w1b = [S.sbuf(f"w1b{i}", [128, 8, 2 * D], BF16) for i in range(2)]
                w2b = [S.sbuf(f"w2b{i}", [128, 8, D], BF16) for i in range(2)]
                b1s = [S.sbuf(f"b1s{i}", [128, 16], F32) for i in range(2)]
                xts = [S.sbuf(f"xts{i}", [128, D], BF16) for i in range(4)]
                hTs = [S.sbuf(f"hTb{i}", [128, 8, BLK], BF16) for i in range(2)]
                actT = [S.sbuf(f"actT{j}", [128, BLK], BF16) for j in range(8)]
                g32 = [S.sbuf(f"g32{i}", [128, BLK], F32) for i in range(3)]
                sg = [S.sbuf(f"sg{i}", [128, BLK], F32) for i in range(3)]
                l32 = [S.sbuf(f"l32{i}", [128, BLK], F32) for i in range(3)]
                gs = [S.sbuf(f"gs{i}", [128, BLK], F32) for i in range(3)]
                yts = [S.sbuf(f"yts{i}", [128, D], F32) for i in range(2)]
                w1tab = I["exp_w1"].rearrange("l e r n -> (l e r) n")
                w2tab = I["exp_w2"].rearrange("l e r n -> (l e r) n")
                b1tab = I["exp_b1T"].rearrange("l r c -> (l r) c")

                def weight_jobs(b):
                    buf = b % 2
                    jobs = [lambda: S.idma(b1s[buf][:, :], b1tab, b1s[buf], IDXB[:, b:b + 1], False, None, reads=[IDXB], writes=[b1s[buf]])]
                    for k in range(8):
                        jobs.append(lambda k=k: S.idma(w1b[buf][:, k, :], w1tab, w1b[buf], IDX[:, b * 8 + k:b * 8 + k + 1], False, None,
                                                       reads=[IDX], writes=[w1b[buf]]))
                    for k in range(8):
                        jobs.append(lambda k=k: S.idma(w2b[buf][:, k, :], w2tab, w2b[buf], IDX[:, b * 8 + k:b * 8 + k + 1], False, None,
                                                       reads=[IDX], writes=[w2b[buf]]))
                    return jobs

                def load_rows(bb):
                    hT_ = hTs[bb % 2]
                    for c in range(4):
                        xt = xts[c]
                        S.dma(S.sp, xt[:], self.XS[bb * BLK + c * 128:bb * BLK + (c + 1) * 128, :], xt, reads=[self.XSb[bb], self.XSall], writes=[xt])
                        pt = self.pb[c % 2]

                        def trx(e, xt=xt, pt=pt):
                            for cc in range(8):
                                ins = e.transpose(pt[:, cc * 128:(cc + 1) * 128], xt[:, cc * 128:(cc + 1) * 128], self.idb[:])
                            return ins
                        S.op(S.pe, trx, reads=[xt, self.idb], writes=[pt])
                        S.op(S.act, lambda e, pt=pt, hT_=hT_, c=c: e.copy(hT_[:, :, c * 128:(c + 1) * 128], pt[:].rearrange("p (c t) -> p c t", c=8)),
                             reads=[pt], writes=[hT_])
                pending = weight_jobs(0)
                for jb in pending:
                    jb()
                pending = []
                na = 0
                for b in range(nblk):
                    buf = b % 2
                    hT = hTs[b % 2]
                    if b + 1 < nblk:
                        pending = weight_jobs(b + 1)
                    if b == 0:
                        load_rows(0)
                    for j in range(8):
                        a = na % 2
                        na += 1
                        pgl, pll = self.ps[2 * a], self.ps[2 * a + 1]

                        def mm1(e, j=j, pgl=pgl, pll=pll, hT=hT, buf=buf):
                            for k in range(8):
                                e.matmul(pgl[:], w1b[buf][:, k, j * 128:(j + 1) * 128], hT[:, k, :], start=(k == 0), stop=(k == 7))
                            for k in range(8):
                                ins = e.matmul(pll[:], w1b[buf][:, k, D + j * 128:D + (j + 1) * 128], hT[:, k, :], start=(k == 0), stop=(k == 7))
                            return ins
                        S.op(S.pe, mm1, reads=[w1b[buf], hT], writes=[pgl, pll])
                        a3 = (na - 1) % 3
                        g_, s_, l_, gs_ = g32[a3], sg[a3], l32[a3], gs[a3]
                        S.op(S.dve, lambda e, g_=g_, pgl=pgl, j=j, buf=buf: e.tensor_scalar(g_[:], pgl[:], b1s[buf][:, j:j + 1], 7.0, ALU.add, ALU.min),
                             reads=[pgl, b1s[buf]], writes=[g_])
                        S.op(S.act, lambda e, g_=g_, s_=s_: e.activation(s_[:], g_[:], AF.Sigmoid, scale=1.702), reads=[g_], writes=[s_])
                        S.op(S.dve, lambda e, l_=l_, pll=pll, j=j, buf=buf: e.tensor_scalar(l_[:], pll[:], b1s[buf][:, 8 + j:9 + j], 7.0, ALU.add, ALU.min),
                             reads=[pll, b1s[buf]], writes=[l_])
                        S.op(S.dve, lambda e, l_=l_: e.tensor_scalar(l_[:], l_[:], -7.0, 1.0, ALU.max, ALU.add), reads=[l_], writes=[l_])
                        S.op(S.dve, lambda e, g_=g_, s_=s_, gs_=gs_: e.tensor_tensor(gs_[:], g_[:], s_[:], ALU.mult), reads=[g_, s_], writes=[gs_])
                        S.op(S.dve, lambda e, gs_=gs_, l_=l_, j=j: e.tensor_tensor(actT[j][:], gs_[:], l_[:], ALU.mult), reads=[gs_, l_], writes=[actT[j]])
                        for _ in range(3):
                            if pending:
                                pending.pop(0)()
                    if b + 1 < nblk:
                        load_rows(b + 1)
                    n = 0
                    for c in range(4):
                        yt = yts[c % 2]
                        for hf in range(2):
                            py = self.ps[4 + n % 2]
                            n += 1

                            def mm2(e, py=py, c=c, hf=hf, buf=buf):
                                for j in range(8):
                                    ins = e.matmul(py[:], actT[j][:, c * 128:(c + 1) * 128], w2b[buf][:, j, hf * 512:(hf + 1) * 512],
                                                   start=(j == 0), stop=(j == 7))
                                return ins
                            S.op(S.pe, mm2, reads=actT + [w2b[buf]], writes=[py])
                            if hf == 0:
                                S.op(S.dve, lambda e, py=py, yt=yt: e.tensor_copy(yt[:, 0:512], py[:]), reads=[py], writes=[yt])
                            else:
                                S.op(S.act, lambda e, py=py, yt=yt: e.copy(yt[:, 512:1024], py[:]), reads=[py], writes=[yt])
                        S.dma(S.sp, self.YS[b * BLK + c * 128:b * BLK + (c + 1) * 128, :], yt[:], yt, reads=[yt], writes=[self.YSb[b]])
                    while pending:
                        pending.pop(0)()
            with S.scope():
                b2t = S.sbuf("b2t", [NE, D], F32)
                g2 = S.sbuf("g2", [128, D], F32)
                lng = S.sbuf("lng", [128, D], F32)
                lnb = S.sbuf("lnb", [128, D], F32)
                xb = [S.sbuf(f"xb{i}", [128, D], F32) for i in range(2)]
                zb = [S.sbuf(f"zb{i}", [128, D], F32) for i in range(2)]
                ob = [S.sbuf(f"ob{i}", [128, D], F32) for i in range(2)]
                yk = [[S.sbuf(f"yk{i}_{k}", [128, D], F32) for k in range(4)] for i in range(2)]
                sms = [S.sbuf(f"sml{i}", [128, 16], F32) for i in range(2)]
                gts_ = [S.sbuf(f"gtt{i}", [NE, 128], F32) for i in range(2)]
                S.dma(S.sp, b2t[:], I["exp_b2"][l], b2t, writes=[b2t])
                self.load_vec(lng, I["ln_g"][l, 1])
                self.load_vec(lnb, I["ln_b"][l, 1])
                def c_loads(tj):
                    S.dma(S.sp, xb[tj % 2][:], self.xsrc(tj).t, xb[tj % 2], reads=[self.xsrc(tj)], writes=[xb[tj % 2]])
                    for k in range(4):
                        S.idma(yk[tj % 2][k][:, :], self.YS[:, :], yk[tj % 2][k], D4[:, tj * 4 + k:tj * 4 + k + 1], False, NBLK * BLK - 1,
                               reads=self.YSb[:nblk] + [D4], writes=[yk[tj % 2][k]])
                kind = None
                for ti in range(ntile):
                    i = ti
                    k2 = 0 if i < NTL else 1
                    if k2 != kind:
                        kind = k2
                        self.load_mod(g2, l, kind, 5)
                    x, z, o, sm = xb[ti % 2], zb[ti % 2], ob[ti % 2], sms[ti % 2]
                    if ti == 0:
                        c_loads(0)
                    if ti + 1 < ntile:
                        c_loads(ti + 1)
                    pg = self.ps[3]
                    gt = gts_[ti % 2]
                    S.op(S.pe, lambda e, ti=ti, pg=pg: e.transpose(pg[0:NE, 0:128], G[:, ti, :], self.idf[:]), reads=[G, self.idf], writes=[pg])
                    S.op(S.act, lambda e, gt=gt, pg=pg: e.copy(gt[:], pg[0:NE, 0:128]), reads=[pg], writes=[gt])
                    for hf in range(2):
                        py = self.ps[hf]
                        S.op(S.pe, lambda e, py=py, gt=gt, hf=hf: e.matmul(py[:], gt[:], b2t[:, hf * 512:(hf + 1) * 512], start=True, stop=True),
                             reads=[gt, b2t], writes=[py])
                        S.op(S.act, lambda e, py=py, z=z, hf=hf: e.copy(z[:, hf * 512:(hf + 1) * 512], py[:]), reads=[py], writes=[z])
                    for k in range(4):
                        ykk = yk[ti % 2][k]
                        S.op(S.dve, lambda e, z=z, ykk=ykk, ti=ti, k=k: e.scalar_tensor_tensor(z[:], ykk[:], G4[:, ti, k:k + 1], z[:], ALU.mult, ALU.add),
                             reads=[ykk, G4, z], writes=[z])
                    S.op(S.dve, lambda e, z=z: e.tensor_tensor(z[:], z[:], g2[:], ALU.mult), reads=[z, g2], writes=[z])
                    S.op(S.dve, lambda e, z=z, x=x: e.scalar_tensor_tensor(z[:], x[:], ALPHA, z[:], ALU.mult, ALU.add), reads=[x, z], writes=[z])
                    self.layernorm(z, o, lng, lnb, sm)
                    if last and l == DEPTH - 1:
                        S.dma(S.sp, self.out[i * 128:(i + 1) * 128, :], o[:], o, reads=[o], writes=[self.OUTt[i]], is_output=True)
                    else:
                        S.dma(S.sp, self.Xt[i].t, o[:], o, reads=[o], writes=[self.Xt[i]])

    def ffn_group(self, l, last, t0, t1):
        S, I = self.S, self.I
        ng = t1 - t0
        ncols = ng * 128
        with S.scope():
            HT = S.sbuf("HT", [128, 8, ncols], BF16)
            yacc = S.sbuf("yacc", [128, ng, D], F32)
            yb = [[S.view(f"y{a}_{b}", yacc[:, a, b * 512:(b + 1) * 512]) for b in range(2)] for a in range(ng)]
            G = S.sbuf("G", [128, ng, NE], F32)
            GT = S.sbuf("GT", [NE, ncols], F32)
            with S.scope():
                s2 = S.sbuf("s2", [128, D], F32)
                h2 = S.sbuf("h2", [128, D], F32)
                rw = S.sbuf("rw", [128, 8, NE], F32)
                rb = S.sbuf("rb", [128, NE], F32)
                xb = [S.sbuf(f"xb{i}", [128, D], F32) for i in range(2)]
                h32 = [S.sbuf(f"h32{i}", [128, D], F32) for i in range(2)]
                hT32 = S.sbuf("hT32", [128, 8, 128], F32)
                sm = S.sbuf("smr", [128, 128], F32)
                S.dma(S.sp, rw[:], I["router_w"][l].rearrange("(c p) n -> p c n", p=128), rw, writes=[rw])
                self.load_vec(rb, I["router_b"][l])
                kind = None
                S.dma(S.sp, xb[0][:], self.xsrc(t0).t, xb[0], reads=[self.xsrc(t0)], writes=[xb[0]])
                for ti in range(ng):
                    i = t0 + ti
                    k2 = 0 if i < NTL else 1
                    if k2 != kind:
                        kind = k2
                        self.load_mod(s2, l, kind, 4)
                        self.load_mod(h2, l, kind, 3)
                    x = xb[ti % 2]
                    h = h32[ti % 2]
                    if ti + 1 < ng:
                        xn = xb[(ti + 1) % 2]
                        S.dma(S.sp, xn[:], self.xsrc(i + 1).t, xn, reads=[self.xsrc(i + 1)], writes=[xn])
                    S.op(S.dve, lambda e, h=h, x=x: e.tensor_tensor(h[:], x[:], s2[:], ALU.mult), reads=[x, s2], writes=[h])
                    S.op(S.dve, lambda e, h=h: e.tensor_tensor(h[:], h[:], h2[:], ALU.add), reads=[h, h2], writes=[h])
                    cols = slice(ti * 128, (ti + 1) * 128)
                    lvl = self.cfg.get('f1_level', 9)
                    if lvl <= 1:
                        continue
                    for half in range(2):
                        pp = self.ps[half]

                        def tr(e, pp=pp, half=half, h=h):
                            for c in range(4):
                                cc = half * 4 + c
                                ins = e.transpose(pp[:, c * 128:(c + 1) * 128], h[:, cc * 128:(cc + 1) * 128], self.idf[:])
                            return ins
                        S.op(S.pe, tr, reads=[h, self.idf], writes=[pp])
                        S.op(S.act, lambda e, pp=pp, half=half: e.copy(hT32[:, half * 4:(half + 1) * 4, :],
                                                                          pp[:].rearrange("p (c t) -> p c t", c=4)),
                             reads=[pp], writes=[hT32])
                        S.op(S.dve, lambda e, pp=pp, half=half, cols=cols: e.tensor_copy(
                            HT[:, half * 4:(half + 1) * 4, cols], pp[:].rearrange("p (c t) -> p c t", c=4)),
                            reads=[pp], writes=[HT])
                    if lvl <= 2:
                        continue
                    pr = self.ps[2]

                    def rmm(e):
                        for k in range(8):
                            ins = e.matmul(pr[:, 0:NE], hT32[:, k, :], rw[:, k, :], start=(k == 0), stop=(k == 7))
                        return ins
                    S.op(S.pe, rmm, reads=[hT32, rw], writes=[pr])
                    if lvl <= 3:
                        continue
                    lg, top8, nmx, ex, mk, ssum = sm[:, 0:32], sm[:, 32:40], sm[:, 40:41], sm[:, 48:80], sm[:, 80:112], sm[:, 112:113]
                    S.op(S.dve, lambda e: e.tensor_tensor(lg, pr[:, 0:NE], rb[:], ALU.add), reads=[pr, rb], writes=[sm])
                    S.op(S.dve, lambda e: e.max(top8, lg), reads=[sm], writes=[sm])
                    S.op(S.dve, lambda e: e.tensor_scalar_mul(nmx, top8[:, 0:1], -1.0), reads=[sm], writes=[sm])
                    S.op(S.dve, lambda e: e.tensor_scalar(mk, lg, top8[:, 3:4], None, ALU.is_ge), reads=[sm], writes=[sm])
                    S.op(S.act, lambda e: e.activation(ex, lg, AF.Exp, bias=nmx, scale=1.0), reads=[sm], writes=[sm])
                    S.op(S.dve, lambda e: e.tensor_tensor(ex, ex, mk, ALU.mult), reads=[sm], writes=[sm])
                    S.op(S.dve, lambda e: e.reduce_sum(ssum, ex, AX.X), reads=[sm], writes=[sm])
                    S.op(S.dve, lambda e: e.reciprocal(ssum, ssum), reads=[sm], writes=[sm])
                    S.op(S.dve, lambda e, ti=ti: e.tensor_scalar_mul(G[:, ti, :], ex, ssum), reads=[sm], writes=[G])
                    if lvl <= 4:
                        continue
                    pg = self.ps[3]
                    S.op(S.pe, lambda e, ti=ti: e.transpose(pg[0:NE, 0:128], G[:, ti, :], self.idf[:]), reads=[G, self.idf], writes=[pg])
                    S.op(S.act, lambda e, cols=cols: e.copy(GT[:, cols], pg[0:NE, 0:128]), reads=[pg], writes=[GT])
            if self.cfg.get("tap_gates") and t0 == 0:
                tg = self.tap("gates", [128, 12, NE])
                S.dma(S.sp, tg, G[:], G, reads=[G], is_output=True)
            if self.cfg.get('stop') == 'F1':
                return
            with S.scope():
                w1p = [S.sbuf(f"w1p{i}", [128, 8, 256], BF16) for i in range(4)]
                w2b = [S.sbuf(f"w2b{i}", [128, 8, D], BF16) for i in range(2)]
                b1t = S.sbuf("b1t", [128, NE, 16], F32)
                b2t = S.sbuf("b2t", [NE, D], F32)
                actT = [S.sbuf(f"actT{j}", [128, ncols], BF16) for j in range(8)]
                g32 = [S.sbuf(f"g32{i}", [128, 512], F32) for i in range(2)]
                sg = [S.sbuf(f"sg{i}", [128, 512], F32) for i in range(2)]
                l32 = [S.sbuf(f"l32{i}", [128, 512], F32) for i in range(2)]
                gs = [S.sbuf(f"gs{i}", [128, 512], F32) for i in range(2)]
                S.dma(S.sp, b1t[:], I["exp_b1L"][l], b1t, writes=[b1t])
                S.dma(S.sp, b2t[:], I["exp_b2"][l], b2t, writes=[b2t])
                n = 0
                for ti in range(ng):
                    for hf in range(2):
                        py = self.ps[4 + n % 2]
                        n += 1
                        S.op(S.pe, lambda e, py=py, ti=ti, hf=hf: e.matmul(py[:], GT[:, ti * 128:(ti + 1) * 128],
                                                                             b2t[:, hf * 512:(hf + 1) * 512], start=True, stop=True),
                             reads=[GT, b2t], writes=[py])
                        S.op(S.act, lambda e, py=py, ti=ti, hf=hf: e.copy(yb[ti][hf][:], py[:]), reads=[py], writes=[yb[ti][hf]])
                sts = []
                c = 0
                while c < ncols:
                    sts.append((c, min(512, ncols - c)))
                    c += 512
                experts = self.cfg.get("experts", range(NE))
                lw = 0 if "exp_w1" in (self.cfg.get("dev_slice") or {}) else l
                cnt = 0
                for ei, ex_ in enumerate(experts):
                    w2 = w2b[ei % 2]
                    w1v = I["exp_w1"][lw, ex_].rearrange("(c p) n -> p c n", p=128)
                    S.dma(S.pool, w2[:], I["exp_w2"][lw, ex_].rearrange("(c p) n -> p c n", p=128), w2, writes=[w2])
                    for j in range(8):
                        pc = w1p[(ei * 8 + j) % 4]
                        S.dma(S.pool, pc[:, :, 0:128], w1v[:, :, j * 128:(j + 1) * 128], pc, writes=[pc])
                        S.dma(S.pool, pc[:, :, 128:256], w1v[:, :, D + j * 128:D + (j + 1) * 128], pc, writes=[pc])
                        for (c0, n_) in sts:
                            a = cnt % 2
                            cnt += 1
                            pgl, pll = self.ps[2 * a], self.ps[2 * a + 1]
                            cs = slice(c0, c0 + n_)

                            def mm1(e, pc=pc, pgl=pgl, pll=pll, cs=cs, n_=n_):
                                for k in range(8):
                                    e.matmul(pgl[:, 0:n_], pc[:, k, 0:128], HT[:, k, cs], start=(k == 0), stop=(k == 7))
                                for k in range(8):
                                    ins = e.matmul(pll[:, 0:n_], pc[:, k, 128:256], HT[:, k, cs], start=(k == 0), stop=(k == 7))
                                return ins
                            S.op(S.pe, mm1, reads=[pc, HT], writes=[pgl, pll])
                            g_, s_, l_, gs_ = g32[a], sg[a], l32[a], gs[a]
                            S.op(S.dve, lambda e, g_=g_, pgl=pgl, n_=n_, j=j, ex_=ex_: e.tensor_scalar(
                                g_[:, 0:n_], pgl[:, 0:n_], b1t[:, ex_, j:j + 1], 7.0, ALU.add, ALU.min),
                                reads=[pgl, b1t], writes=[g_])
                            S.op(S.act, lambda e, g_=g_, s_=s_, n_=n_: e.activation(s_[:, 0:n_], g_[:, 0:n_], AF.Sigmoid, scale=1.702),
                                 reads=[g_], writes=[s_])
                            S.op(S.dve, lambda e, l_=l_, pll=pll, n_=n_, j=j, ex_=ex_: e.tensor_scalar(
                                l_[:, 0:n_], pll[:, 0:n_], b1t[:, ex_, 8 + j:9 + j], 7.0, ALU.add, ALU.min),
                                reads=[pll, b1t], writes=[l_])
                            S.op(S.pool, lambda e, l_=l_, n_=n_: e.tensor_scalar(l_[:, 0:n_], l_[:, 0:n_], -7.0, 1.0, ALU.max, ALU.add),
                                 reads=[l_], writes=[l_])
                            S.op(S.pool, lambda e, g_=g_, s_=s_, gs_=gs_, n_=n_: e.tensor_tensor(gs_[:, 0:n_], g_[:, 0:n_], s_[:, 0:n_], ALU.mult),
                                 reads=[g_, s_], writes=[gs_])
                            S.op(S.dve, lambda e, gs_=gs_, l_=l_, j=j, cs=cs, n_=n_: e.tensor_tensor(actT[j][:, cs], gs_[:, 0:n_], l_[:, 0:n_], ALU.mult),
                                 reads=[gs_, l_], writes=[actT[j]])
                    n = 0
                    for ti in range(ng):
                        for hf in range(2):
                            py = self.ps[4 + n % 2]
                            n += 1

                            def mm2(e, py=py, ti=ti, hf=hf, w2=w2):
                                for j in range(8):
                                    ins = e.matmul(py[:], actT[j][:, ti * 128:(ti + 1) * 128], w2[:, j, hf * 512:(hf + 1) * 512],
                                                   start=(j == 0), stop=(j == 7))
                                return ins
                            S.op(S.pe, mm2, reads=actT + [w2], writes=[py])
                            yy = yb[ti][hf]
                            S.op(S.dve, lambda e, py=py, ti=ti, yy=yy, ex_=ex_: e.scalar_tensor_tensor(
                                yy[:], py[:], G[:, ti, ex_:ex_ + 1], yy[:], ALU.mult, ALU.add),
                                reads=[py, G, yy], writes=[yy])
            if self.cfg.get('stop') == 'F2':
                return
            with S.scope():
                g2 = S.sbuf("g2", [128, D], F32)
                lng = S.sbuf("lng", [128, D], F32)
                lnb = S.sbuf("lnb", [128, D], F32)
                xb = [S.sbuf(f"xb{i}", [128, D], F32) for i in range(2)]
                zb = [S.sbuf(f"zb{i}", [128, D], F32) for i in range(2)]
                ob = [S.sbuf(f"ob{i}", [128, D], F32) for i in range(2)]
                sms = [S.sbuf(f"sml{i}", [128, 16], F32) for i in range(2)]
                self.load_vec(lng, I["ln_g"][l, 1])
                self.load_vec(lnb, I["ln_b"][l, 1])
                kind = None
                S.dma(S.sp, xb[0][:], self.xsrc(t0).t, xb[0], reads=[self.xsrc(t0)], writes=[xb[0]])
                for ti in range(ng):
                    i = t0 + ti
                    k2 = 0 if i < NTL else 1
                    if k2 != kind:
                        kind = k2
                        self.load_mod(g2, l, kind, 5)
                    x, z, o, sm = xb[ti % 2], zb[ti % 2], ob[ti % 2], sms[ti % 2]
                    if ti + 1 < ng:
                        xn = xb[(ti + 1) % 2]
                        S.dma(S.sp, xn[:], self.xsrc(i + 1).t, xn, reads=[self.xsrc(i + 1)], writes=[xn])
                    S.op(S.dve, lambda e, z=z, ti=ti: e.tensor_tensor(z[:], yacc[:, ti, :], g2[:], ALU.mult),
                         reads=[yb[ti][0], yb[ti][1], g2], writes=[z])
                    S.op(S.dve, lambda e, z=z, x=x: e.scalar_tensor_tensor(z[:], x[:], ALPHA, z[:], ALU.mult, ALU.add),
                         reads=[x, z], writes=[z])
                    self.layernorm(z, o, lng, lnb, sm)
                    if last and l == DEPTH - 1:
                        S.dma(S.sp, self.out[i * 128:(i + 1) * 128, :], o[:], o, reads=[o], writes=[self.OUTt[i]], is_output=True)
                    else:
                        S.dma(S.sp, self.Xt[i].t, o[:], o, reads=[o], writes=[self.Xt[i]])


    def st_range(self, lo, hi):
        return list(range(lo // 512, (hi - 1) // 512 + 1))

    def mixer_even(self, l, last):
        S, I = self.S, self.I
        j = l // 2
        QTv = self.QT.rearrange("(f p) t -> p f t", p=128)
        KTv = self.KT.rearrange("(f p) t -> p f t", p=128)
        with S.scope():
            win = S.sbuf("win", [128, 8, 2560], BF16)
            wv = I["ab_w_in"][j].rearrange("(c p) n -> p c n", p=128)
            for b5 in range(5):
                S.dma(S.pool, win[:, :, b5 * 512:(b5 + 1) * 512], wv[:, :, b5 * 512:(b5 + 1) * 512], win, writes=[win])
            s1 = S.sbuf("s1", [128, D], F32)
            h1 = S.sbuf("h1", [128, D], F32)
            xb = [S.sbuf(f"xb{i}", [128, D], F32) for i in range(2)]
            h32 = S.sbuf("h32", [128, D], F32)
            hb = [S.sbuf(f"hb{i}", [128, D], BF16) for i in range(2)]
            hTs = [S.sbuf(f"hT{i}", [128, 8, 512], BF16) for i in range(2)]
            qks = [S.sbuf(f"qk{i}", [128, 12, 512], BF16) for i in range(2)]
            uvs = [S.sbuf(f"uv{i}", [128, 1024], BF16) for i in range(2)]
            kind = None
            nx = 0
            for s_ in range(9):
                tiles = list(range(4 * s_, 4 * s_ + 4)) if s_ < 8 else [32, 33]
                N = 128 * len(tiles)
                c0 = tiles[0] * 128
                hT = hTs[s_ % 2]
                qk = qks[s_ % 2]
                k2 = 0 if s_ < 8 else 1
                if k2 != kind:
                    kind = k2
                    self.load_mod(s1, l, kind, 1)
                    self.load_mod(h1, l, kind, 0)
                for tt, i in enumerate(tiles):
                    x = xb[nx % 2]
                    hbb = hb[nx % 2]
                    nx += 1
                    S.dma(S.sp, x[:], self.xsrc(i).t, x, reads=[self.xsrc(i)], writes=[x])
                    S.op(S.dve, lambda e, x=x: e.tensor_tensor(h32[:], x[:], s1[:], ALU.mult), reads=[x, s1], writes=[h32])
                    S.op(S.dve, lambda e, hbb=hbb: e.tensor_tensor(hbb[:], h32[:], h1[:], ALU.add), reads=[h32, h1], writes=[hbb])
                    pt = self.pb[0]

                    def tr(e, hbb=hbb, pt=pt):
                        for c in range(8):
                            ins = e.transpose(pt[:, c * 128:(c + 1) * 128], hbb[:, c * 128:(c + 1) * 128], self.idb[:])
                        return ins
                    S.op(S.pe, tr, reads=[hbb, self.idb], writes=[pt])
                    S.op(S.act, lambda e, pt=pt, hT=hT, tt=tt: e.copy(hT[:, :, tt * 128:(tt + 1) * 128],
                                                                      pt[:].rearrange("p (c t) -> p c t", c=8)),
                         reads=[pt], writes=[hT])
                for tt, i in enumerate(tiles):
                    uv = uvs[i % 2]
                    pu, pv1, pv2 = self.ps[0], self.ps[1], self.ps[2]
                    tc_ = slice(tt * 128, (tt + 1) * 128)

                    def mmuv(e, tc_=tc_, hT=hT, pu=pu, pv1=pv1, pv2=pv2):
                        for k in range(8):
                            e.matmul(pu[:, 0:256], hT[:, k, tc_], win[:, k, 0:256], start=(k == 0), stop=(k == 7))
                        for k in range(8):
                            e.matmul(pv1[:], hT[:, k, tc_], win[:, k, 1792:2304], start=(k == 0), stop=(k == 7))
                        for k in range(8):
                            ins = e.matmul(pv2[:, 0:256], hT[:, k, tc_], win[:, k, 2304:2560], start=(k == 0), stop=(k == 7))
                        return ins
                    S.op(S.pe, mmuv, reads=[hT, win], writes=[pu, pv1, pv2])
                    S.op(S.dve, lambda e, uv=uv, pu=pu: e.tensor_copy(uv[:, 0:256], pu[:, 0:256]), reads=[pu], writes=[uv])
                    S.op(S.act, lambda e, uv=uv, pv1=pv1: e.copy(uv[:, 256:768], pv1[:]), reads=[pv1], writes=[uv])
                    S.op(S.dve, lambda e, uv=uv, pv2=pv2: e.tensor_copy(uv[:, 768:1024], pv2[:, 0:256]), reads=[pv2], writes=[uv])
                    S.dma(S.pool, self.U[i * 128:(i + 1) * 128, 0:256], uv[:, 0:256], uv, reads=[uv], writes=[self.Ut[i]])
                    S.dma(S.pool, self.V[i * 128:(i + 1) * 128, 0:768], uv[:, 256:1024], uv, reads=[uv], writes=[self.Vt[i]])
                for f in range(12):
                    pq = self.ps[3 + f % 2]

                    def mmq(e, f=f, pq=pq, hT=hT, N=N):
                        for k in range(8):
                            ins = e.matmul(pq[:, 0:N], win[:, k, 256 + f * 128:256 + (f + 1) * 128], hT[:, k, 0:N],
                                           start=(k == 0), stop=(k == 7))
                        return ins
                    S.op(S.pe, mmq, reads=[hT, win], writes=[pq])
                    if f < 6:
                        S.op(S.act, lambda e, f=f, pq=pq, qk=qk, N=N: e.mul(qk[:, f, 0:N], pq[:, 0:N], 0.125), reads=[pq], writes=[qk])
                    else:
                        S.op(S.dve, lambda e, f=f, pq=pq, qk=qk, N=N: e.tensor_copy(qk[:, f, 0:N], pq[:, 0:N]), reads=[pq], writes=[qk])
                S.dma(S.pool, QTv[:, :, c0:c0 + N], qk[:, 0:6, 0:N], qk, reads=[qk], writes=[self.QTs[s_]])
                S.dma(S.pool, KTv[:, :, c0:c0 + N], qk[:, 6:12, 0:N], qk, reads=[qk], writes=[self.KTs[s_]])
        with S.scope():
            bias = S.sbuf("nab", [128, 12, 832], F32)
            kTc = S.sbuf("kTc", [128, 6, 256], BF16)
            vc = S.sbuf("vc", [128, 2, 768], BF16)
            wout = S.sbuf("wout", [128, 8, D], BF16)
            Wbd = S.sbuf("Wbd", [128, 2, 256], BF16)
            pA = S.sbuf("pA", [128, 20, 128], BF16)
            psc = S.sbuf("psc", [128, 256], F32)
            g1 = S.sbuf("g1", [128, D], F32)
            lng = S.sbuf("lng", [128, D], F32)
            lnb = S.sbuf("lnb", [128, D], F32)
            kws = [S.sbuf(f"kw{i}", [128, 6, 576], BF16) for i in range(2)]
            vws = [S.sbuf(f"vw{i}", [128, 5, 768], BF16) for i in range(2)]
            qts = [S.sbuf(f"qt{i}", [128, 6, 128], BF16) for i in range(2)]
            uws = [S.sbuf(f"uw{i}", [128, 3, 256], BF16) for i in range(2)]
            xb = [S.sbuf(f"xb{i}", [128, D], F32) for i in range(2)]
            zb = [S.sbuf(f"zb{i}", [128, D], F32) for i in range(2)]
            ob = [S.sbuf(f"ob{i}", [128, D], F32) for i in range(2)]
            sms = [S.sbuf(f"sml{i}", [128, 16], F32) for i in range(2)]
            ssb = [S.sbuf(f"ssb{i}", [128, 832], F32) for i in range(3)]
            psb = [S.sbuf(f"psb{i}", [128, 832], BF16) for i in range(3)]
            pTs = [S.sbuf(f"pT{i}", [128, 7, 128], BF16) for i in range(3)]
            st4 = [S.sbuf(f"st4{i}", [128, 4], F32) for i in range(3)]
            rs12 = [S.sbuf(f"rs12{i}", [128, 24], F32) for i in range(2)]
            cats = [S.sbuf(f"cat{i}", [128, D], BF16) for i in range(2)]
            catT = S.sbuf("catT", [128, 8, 128], BF16)
            pooled = S.sbuf("pooled", [128, 256], BF16)
            pT2 = S.sbuf("pT2", [128, 2, 128], BF16)
            S.dma(S.sp, kTc[:], KTv[:, :, TL:T], kTc, reads=[self.KTs[8]], writes=[kTc])
            S.dma(S.sp, vc[:], self.V[TL:T, 0:768].rearrange("(c p) d -> p c d", p=128), vc, reads=[self.Vt[32], self.Vt[33]], writes=[vc])
            S.dma(S.pool, wout[:], I["ab_w_out"][j].rearrange("(c p) n -> p c n", p=128), wout, writes=[wout])
            S.dma(S.pool, pA[:], I["poolA"].rearrange("v s t -> s v t"), pA, writes=[pA])
            S.op(S.dve, lambda e: e.memset(Wbd[:], 0.0), writes=[Wbd])
            for g in range(4):
                S.dma(S.pool, Wbd[(g % 2) * 64:(g % 2) * 64 + 64, g // 2, g * 64:(g + 1) * 64], I["ab_pool_w"][j, g], Wbd, writes=[Wbd])
            self.load_vec(psc, I["ab_pool_scale"][j])
            self.load_vec(lng, I["ln_g"][l, 0])
            self.load_vec(lnb, I["ln_b"][l, 0])
            S.op(S.dve, lambda e: e.memset(bias[:, :, 0:256], 0.0), writes=[bias])
            ntile = NTL if last else NT
            kind = None
            cur_pat = None
            nh = 0
            for i in range(ntile):
                isctx = i >= NTL
                k2 = 1 if isctx else 0
                if k2 != kind:
                    kind = k2
                    self.load_mod(g1, l, kind, 2)
                x, z, o, sm = xb[i % 2], zb[i % 2], ob[i % 2], sms[i % 2]
                qt, kw, vw, uw, cat = qts[i % 2], kws[i % 2], vws[i % 2], uws[i % 2], cats[i % 2]
                S.dma(S.sp, x[:], self.xsrc(i).t, x, reads=[self.xsrc(i)], writes=[x])
                S.dma(S.sp, qt[:], QTv[:, :, i * 128:(i + 1) * 128], qt, reads=[self.QTs[i // 4 if not isctx else 8]], writes=[qt])
                base, nseq = (0, NTL) if not isctx else (NTL, 2)
                li = i - base
                blocks = []
                if li > 0:
                    blocks.append((0, 0))
                blocks.append((1, 3 if li == 0 else (4 if li == nseq - 1 else 1)))
                if li < nseq - 1:
                    blocks.append((2, 2))
                for (nb, var) in blocks:
                    ii = i + nb - 1
                    S.dma(S.sp, uw[:, nb, :], self.U[ii * 128:(ii + 1) * 128, 0:256], uw, reads=[self.Ut[ii]], writes=[uw])
                if not isctx:
                    pat = 0 if i == 0 else 1 if i == 1 else 3 if i == 30 else 4 if i == 31 else 2
                    if pat != cur_pat:
                        cur_pat = pat
                        S.dma(S.sp, bias[:, :, 256:832], I["nabias"][j, pat].rearrange("h q k -> q h k"), bias, writes=[bias])
                    kr0 = min(max(2 * i - 4, 0), 55)
                    k0 = kr0 * 64
                    S.dma(S.sp, kw[:], KTv[:, :, k0:k0 + 576], kw, reads=[self.KTs[a] for a in self.st_range(k0, k0 + 576)], writes=[kw])
                    vr = [self.Vt[a] for a in range(k0 // 128, (k0 + 575) // 128 + 1)]
                    S.dma(S.sp, vw[:, 0:4, :], self.V[k0:k0 + 512, 0:768].rearrange("(c p) d -> p c d", p=128), vw, reads=vr, writes=[vw])
                    S.dma(S.sp, vw[0:64, 4, :], self.V[k0 + 512:k0 + 576, 0:768], vw, reads=vr, writes=[vw])
                pp = self.ps[4]

                def mmpool(e, blocks=blocks, uw=uw, pp=pp):
                    for g in range(4):
                        for bi, (nb, var) in enumerate(blocks):
                            ins = e.matmul(pp[:, g * 64:(g + 1) * 64], pA[:, g * 5 + var, :], uw[:, nb, g * 64:(g + 1) * 64],
                                           start=(bi == 0), stop=(bi == len(blocks) - 1))
                    return ins
                S.op(S.pe, mmpool, reads=[pA, uw], writes=[pp])
                S.op(S.act, lambda e, pp=pp: e.copy(pooled[:], pp[:, 0:256]), reads=[pp], writes=[pooled])
                ptb = self.pb[1]

                def trp(e, ptb=ptb):
                    for c in range(2):
                        ins = e.transpose(ptb[:, c * 128:(c + 1) * 128], pooled[:, c * 128:(c + 1) * 128], self.idb[:])
                    return ins
                S.op(S.pe, trp, reads=[pooled, self.idb], writes=[ptb])
                S.op(S.dve, lambda e, ptb=ptb: e.tensor_copy(pT2[:], ptb[:, 0:256].rearrange("p (c t) -> p c t", c=2)), reads=[ptb], writes=[pT2])
                py = self.ps[5]

                def mmy(e, py=py):
                    for c in range(2):
                        ins = e.matmul(py[:, 0:256], pT2[:, c, :], Wbd[:, c, :], start=(c == 0), stop=(c == 1))
                    return ins
                S.op(S.pe, mmy, reads=[pT2, Wbd], writes=[py])
                S.op(S.dve, lambda e, py=py, cat=cat: e.tensor_tensor(cat[:, 0:256], py[:, 0:256], psc[:], ALU.mult), reads=[py, psc], writes=[cat])
                for h in range(12):
                    hp, hh = h // 2, h % 2
                    psl = slice(hh * 64, hh * 64 + 64)
                    ss, pb_, pT, st = ssb[nh % 3], psb[nh % 3], pTs[nh % 3], st4[nh % 3]
                    rs = rs12[i % 2]
                    rcol = (h % 2) * 6 + h // 2
                    nh += 1
                    pS1, pS2 = self.ps[2 * (h % 3)], self.ps[2 * (h % 3) + 1]
                    nk = 256 if isctx else 832
                    nblk = 2 if isctx else 7

                    def mms(e, qt=qt, kw=kw, hp=hp, psl=psl, pS1=pS1, pS2=pS2, isctx=isctx):
                        ins = e.matmul(pS1[:, 0:256], qt[psl, hp, :], kTc[psl, hp, :], start=True, stop=True)
                        if not isctx:
                            e.matmul(pS1[:, 256:512], qt[psl, hp, :], kw[psl, hp, 0:256], start=True, stop=True)
                            ins = e.matmul(pS2[:, 0:320], qt[psl, hp, :], kw[psl, hp, 256:576], start=True, stop=True)
                        return ins
                    S.op(S.pe, mms, reads=[qt, kTc] + ([] if isctx else [kw]), writes=[pS1] + ([] if isctx else [pS2]))
                    if isctx:
                        S.op(S.act, lambda e, ss=ss, pS1=pS1: e.copy(ss[:, 0:256], pS1[:, 0:256]), reads=[pS1], writes=[ss])
                    else:
                        S.op(S.dve, lambda e, ss=ss, pS1=pS1, h=h: e.tensor_tensor(ss[:, 0:512], pS1[:], bias[:, h, 0:512], ALU.add),
                             reads=[pS1, bias], writes=[ss])
                        S.op(S.dve, lambda e, ss=ss, pS2=pS2, h=h: e.tensor_tensor(ss[:, 512:832], pS2[:, 0:320], bias[:, h, 512:832], ALU.add),
                             reads=[pS2, bias], writes=[ss])
                    S.op(S.dve, lambda e, ss=ss, st=st, nk=nk: e.reduce_max(st[:, 0:1], ss[:, 0:nk], AX.X), reads=[ss], writes=[st])
                    S.op(S.dve, lambda e, st=st: e.tensor_scalar_mul(st[:, 1:2], st[:, 0:1], -1.0), reads=[st], writes=[st])
                    S.op(S.act, lambda e, ss=ss, pb_=pb_, st=st, nk=nk: e.activation(pb_[:, 0:nk], ss[:, 0:nk], AF.Exp, bias=st[:, 1:2], scale=1.0,
                                                                               accum_out=rs[:, rcol:rcol + 1]),
                         reads=[ss, st], writes=[pb_, rs])
                    ptb = pS1
                    ptv = pS1.t[:].bitcast(BF16)

                    def trs(e, pb_=pb_, ptb=ptv, nblk=nblk):
                        for c in range(nblk):
                            if c < 6:
                                ins = e.transpose(ptb[:, c * 128:(c + 1) * 128], pb_[:, c * 128:(c + 1) * 128], self.idb[:])
                            else:
                                ins = e.transpose(ptb[0:64, 768:896], pb_[:, 768:832], self.idb[:])
                        return ins
                    S.op(S.pe, trs, reads=[pb_, self.idb], writes=[ptb])
                    cp_eng = S.act if h % 2 == 0 else S.dve
                    if cp_eng is S.act:
                        S.op(S.act, lambda e, pT=pT, ptv=ptv, nblk=nblk: e.copy(pT[:, 0:nblk, :], ptv[:, 0:nblk * 128].rearrange("p (c t) -> p c t", c=nblk)),
                             reads=[ptb], writes=[pT])
                    else:
                        S.op(S.dve, lambda e, pT=pT, ptv=ptv, nblk=nblk: e.tensor_copy(pT[:, 0:nblk, :], ptv[:, 0:nblk * 128].rearrange("p (c t) -> p c t", c=nblk)),
                             reads=[ptb], writes=[pT])
                    po = self.pb[h % 2]
                    pov = po.t[:].bitcast(F32)
                    oc = slice((h // 2) * 64, (h // 2) * 64 + 64)

                    def mmpv(e, pT=pT, vw=vw, po=pov, oc=oc, h=h, nblk=nblk):
                        for c in range(nblk):
                            if c < 2:
                                ins = e.matmul(po[:, oc], pT[:, c, :], vc[:, c, h * 64:(h + 1) * 64], start=(c == 0), stop=(c == nblk - 1))
                            elif c < 6:
                                ins = e.matmul(po[:, oc], pT[:, c, :], vw[:, c - 2, h * 64:(h + 1) * 64], start=False, stop=False)
                            else:
                                ins = e.matmul(po[:, oc], pT[0:64, 6, :], vw[0:64, 4, h * 64:(h + 1) * 64], start=False, stop=True)
                        return ins
                    S.op(S.pe, mmpv, reads=[pT, vc] + ([] if isctx else [vw]), writes=[po])
                rs = rs12[i % 2]
                S.op(S.dve, lambda e, rs=rs: e.reciprocal(rs[:, 12:24], rs[:, 0:12]), reads=[rs], writes=[rs])
                cat4 = cat[:, 256:1024].rearrange("p (a two d) -> p a two d", two=2, d=64)
                for par in range(2):
                    po = self.pb[par]
                    pov2 = po.t[:].bitcast(F32)
                    S.op(S.dve, lambda e, po=pov2, par=par, rs=rs, cat4=cat4: e.tensor_tensor(
                        cat4[:, :, par, :], po[:, 0:384].rearrange("p (a d) -> p a d", d=64),
                        rs[:, 12 + par * 6:18 + par * 6].rearrange("p (a o) -> p a o", o=1).to_broadcast([128, 6, 64]), ALU.mult),
                        reads=[po, rs], writes=[cat])
                self.out_proj_ln(l, i, cat, catT, wout, g1, x, z, o, sm, lng, lnb)
        self.src_is_input = False

    def out_proj_ln(self, l, i, cat, catT, wout, g1, x, z, o, sm, lng, lnb):
        S = self.S
        ptb = self.pb[1]

        def trc(e):
            for c in range(8):
                ins = e.transpose(ptb[:, c * 128:(c + 1) * 128], cat[:, c * 128:(c + 1) * 128], self.idb[:])
            return ins
        S.op(S.pe, trc, reads=[cat, self.idb], writes=[ptb])
        S.op(S.act, lambda e: e.copy(catT[:], ptb[:].rearrange("p (c t) -> p c t", c=8)), reads=[ptb], writes=[catT])
        for hf in range(2):
            po2 = self.ps[hf]

            def mmo(e, po2=po2, hf=hf):
                for k in range(8):
                    ins = e.matmul(po2[:], catT[:, k, :], wout[:, k, hf * 512:(hf + 1) * 512], start=(k == 0), stop=(k == 7))
                return ins
            S.op(S.pe, mmo, reads=[catT, wout], writes=[po2])
            S.op(S.dve, lambda e, po2=po2, hf=hf: e.tensor_tensor(z[:, hf * 512:(hf + 1) * 512], po2[:], g1[:, hf * 512:(hf + 1) * 512], ALU.mult),
                 reads=[po2, g1], writes=[z])
        if self.cfg.get("tap_ox"):
            if not hasattr(self, "_tox"):
                self._tox = self.tap("oxg", [T, D])
            S.dma(S.sp, self._tox[i * 128:(i + 1) * 128, :], z[:], z, reads=[z], is_output=True)
        S.op(S.dve, lambda e: e.scalar_tensor_tensor(z[:], x[:], ALPHA, z[:], ALU.mult, ALU.add), reads=[x, z], writes=[z])
        self.layernorm(z, o, lng, lnb, sm)
        S.dma(S.pool, self.Xt[i].t, o[:], o, reads=[o], writes=[self.Xt[i]])


    def mixer_odd(self, l, last):
        S, I = self.S, self.I
        j = l // 2
        QTv = self.QT[0:512, :].rearrange("(f p) t -> p f t", p=128)
        KTv = self.KT[0:512, :].rearrange("(f p) t -> p f t", p=128)
        QSCALE = 128.0 ** -0.5
        with S.scope():
            win = S.sbuf("gwin", [128, 8, 3104], BF16)
            wv = I["gla_w_in"][j].rearrange("(c p) n -> p c n", p=128)
            for b5 in range(6):
                S.dma(S.pool, win[:, :, b5 * 512:(b5 + 1) * 512], wv[:, :, b5 * 512:(b5 + 1) * 512], win, writes=[win])
            S.dma(S.pool, win[:, :, 3072:3104], wv[:, :, 3072:3104], win, writes=[win])
            s1 = S.sbuf("s1", [128, D], F32)
            h1 = S.sbuf("h1", [128, D], F32)
            xb = [S.sbuf(f"xb{i}", [128, D], F32) for i in range(2)]
            h32 = S.sbuf("h32", [128, D], F32)
            hb = [S.sbuf(f"hb{i}", [128, D], BF16) for i in range(2)]
            hTs = [S.sbuf(f"hT{i}", [128, 8, 512], BF16) for i in range(2)]
            qk32 = S.sbuf("qk32", [128, D], F32)
            tA = S.sbuf("tA", [128, 512], F32)
            tB = S.sbuf("tB", [128, 512], F32)
            qkb = [S.sbuf(f"qkb{i}", [128, D], BF16) for i in range(2)]
            qkT = [S.sbuf(f"qkT{i}", [128, 8, 128], BF16) for i in range(2)]
            vrs = [S.sbuf(f"vr{i}", [128, 2 * D], BF16) for i in range(2)]
            css = [S.sbuf(f"cs{i}", [128, 2, 64], F32) for i in range(2)]
            gsb = [S.sbuf(f"gsb{i}", [16, 2, 512], F32) for i in range(2)]
            kind = None
            nx = 0
            for s_ in range(9):
                tiles = list(range(4 * s_, 4 * s_ + 4)) if s_ < 8 else [32, 33]
                N = 128 * len(tiles)
                c0 = tiles[0] * 128
                hT = hTs[s_ % 2]
                k2 = 0 if s_ < 8 else 1
                if k2 != kind:
                    kind = k2
                    self.load_mod(s1, l, kind, 1)
                    self.load_mod(h1, l, kind, 0)
                for tt, i in enumerate(tiles):
                    x = xb[nx % 2]
                    hbb = hb[nx % 2]
                    nx += 1
                    S.dma(S.sp, x[:], self.xsrc(i).t, x, reads=[self.xsrc(i)], writes=[x])
                    S.op(S.dve, lambda e, x=x: e.tensor_tensor(h32[:], x[:], s1[:], ALU.mult), reads=[x, s1], writes=[h32])
                    S.op(S.dve, lambda e, hbb=hbb: e.tensor_tensor(hbb[:], h32[:], h1[:], ALU.add), reads=[h32, h1], writes=[hbb])
                    pt = self.pb[0]

                    def tr(e, hbb=hbb, pt=pt):
                        for c in range(8):
                            ins = e.transpose(pt[:, c * 128:(c + 1) * 128], hbb[:, c * 128:(c + 1) * 128], self.idb[:])
                        return ins
                    S.op(S.pe, tr, reads=[hbb, self.idb], writes=[pt])
                    S.op(S.act, lambda e, pt=pt, hT=hT, tt=tt: e.copy(hT[:, :, tt * 128:(tt + 1) * 128],
                                                                      pt[:].rearrange("p (c t) -> p c t", c=8)),
                         reads=[pt], writes=[hT])
                gs_ = gsb[s_ % 2]
                for z in range(2):
                    pgz = self.ps[4 + z]

                    def mmg(e, z=z, pgz=pgz, hT=hT, N=N):
                        for k in range(8):
                            ins = e.matmul(pgz[0:16, 0:N], win[:, k, 3072 + 16 * z:3088 + 16 * z], hT[:, k, 0:N], start=(k == 0), stop=(k == 7))
                        return ins
                    S.op(S.pe, mmg, reads=[hT, win], writes=[pgz])
                    S.op(S.act, lambda e, z=z, pgz=pgz, gs_=gs_, N=N: e.copy(gs_[:, z, 0:N], pgz[0:16, 0:N]), reads=[pgz], writes=[gs_])
                S.dma(S.sp, self.GS[:, :, c0:c0 + N].rearrange("z r t -> r z t"), gs_[:, :, 0:N], gs_, reads=[gs_], writes=[self.GSs[s_]])
                for tt, i in enumerate(tiles):
                    isctx = i >= NTL
                    tc_ = slice(tt * 128, (tt + 1) * 128)
                    vr = vrs[i % 2]
                    qb = qkb[i % 2]
                    qT_ = qkT[i % 2]
                    for blk in range(6):
                        pz = self.ps[blk % 4]

                        def mmt(e, blk=blk, pz=pz, hT=hT, tc_=tc_):
                            for k in range(8):
                                ins = e.matmul(pz[:], hT[:, k, tc_], win[:, k, blk * 512:(blk + 1) * 512], start=(k == 0), stop=(k == 7))
                            return ins
                        S.op(S.pe, mmt, reads=[hT, win], writes=[pz])
                        if blk == 0:
                            S.op(S.act, lambda e, pz=pz: e.mul(qk32[:, 0:512], pz[:], QSCALE), reads=[pz], writes=[qk32])
                        elif blk == 1:
                            S.op(S.act, lambda e, pz=pz: e.copy(qk32[:, 512:1024], pz[:]), reads=[pz], writes=[qk32])
                        elif blk % 2 == 0:
                            S.op(S.dve, lambda e, pz=pz, blk=blk, vr=vr: e.tensor_copy(vr[:, (blk - 2) * 512:(blk - 1) * 512], pz[:]), reads=[pz], writes=[vr])
                        else:
                            S.op(S.act, lambda e, pz=pz, blk=blk, vr=vr: e.copy(vr[:, (blk - 2) * 512:(blk - 1) * 512], pz[:]), reads=[pz], writes=[vr])
                    S.dma(S.sp, self.V[i * 128:(i + 1) * 128, :], vr[:, 0:D], vr, reads=[vr], writes=[self.Vt[i]])
                    S.dma(S.sp, self.U[i * 128:(i + 1) * 128, :], vr[:, D:2 * D], vr, reads=[vr], writes=[self.Ut[i]])
                    if isctx:
                        S.op(S.dve, lambda e, qb=qb: e.tensor_copy(qb[:], qk32[:]), reads=[qk32], writes=[qb])
                    else:
                        cs = css[i % 2]
                        S.dma(S.sp, cs[:], I["ropecs"][i * 128:(i + 1) * 128], cs, writes=[cs])
                        q4 = qk32[:].rearrange("p (h two d) -> p h two d", two=2, d=64)
                        o4 = qb[:].rearrange("p (h two d) -> p h two d", two=2, d=64)
                        x1, x2 = q4[:, :, 0, :], q4[:, :, 1, :]
                        cosb = cs[:, 0:1, :].to_broadcast([128, 8, 64])
                        sinb = cs[:, 1:2, :].to_broadcast([128, 8, 64])
                        a3 = tA[:].rearrange("p (h d) -> p h d", d=64)
                        b3 = tB[:].rearrange("p (h d) -> p h d", d=64)
                        S.op(S.dve, lambda e: e.tensor_tensor(a3, x1, cosb, ALU.mult), reads=[qk32, cs], writes=[tA])
                        S.op(S.pool, lambda e: e.tensor_tensor(b3, x2, sinb, ALU.mult), reads=[qk32, cs], writes=[tB])
                        S.op(S.dve, lambda e, o4=o4: e.tensor_tensor(o4[:, :, 0, :], a3, b3, ALU.subtract), reads=[tA, tB], writes=[qb])
                        S.op(S.dve, lambda e: e.tensor_tensor(a3, x1, sinb, ALU.mult), reads=[qk32, cs], writes=[tA])
                        S.op(S.pool, lambda e: e.tensor_tensor(b3, x2, cosb, ALU.mult), reads=[qk32, cs], writes=[tB])
                        S.op(S.dve, lambda e, o4=o4: e.tensor_tensor(o4[:, :, 1, :], a3, b3, ALU.add), reads=[tA, tB], writes=[qb])
                    pt = self.pb[1]

                    def trq(e, qb=qb, pt=pt):
                        for c in range(8):
                            ins = e.transpose(pt[:, c * 128:(c + 1) * 128], qb[:, c * 128:(c + 1) * 128], self.idb[:])
                        return ins
                    S.op(S.pe, trq, reads=[qb, self.idb], writes=[pt])
                    S.op(S.dve, lambda e, pt=pt, qT_=qT_: e.tensor_copy(qT_[:], pt[:].rearrange("p (c t) -> p c t", c=8)), reads=[pt], writes=[qT_])
                    S.dma(S.sp, QTv[:, :, i * 128:(i + 1) * 128], qT_[:, 0:4, :], qT_, reads=[qT_], writes=[self.QTs[s_]])
                    S.dma(S.sp, KTv[:, :, i * 128:(i + 1) * 128], qT_[:, 4:8, :], qT_, reads=[qT_], writes=[self.KTs[s_]])
        with S.scope():
            wg = S.sbuf("wg", [16, 2, 512], F32)
            negb = S.sbuf("negb", [128, 2, 4], F32)
            msk = S.sbuf("msk", [128, 2, 128], F32)
            ones = S.sbuf("ones", [128, 128], F32)
            S.dma(S.sp, wg[:], I["gla_w_gate"][j].rearrange("z r e -> r z e"), wg, writes=[wg])
            S.dma(S.sp, negb[:], I["gla_bgL"][j], negb, writes=[negb])
            S.op(S.dve, lambda e: e.tensor_scalar_mul(negb[:], negb[:], -1.0), reads=[negb], writes=[negb])
            S.dma(S.sp, msk[:], I["trimask"][0:2].rearrange("z s t -> s z t"), msk, writes=[msk])
            S.op(S.dve, lambda e: e.memset(ones[:], 1.0), writes=[ones])
            S32 = [[S.sbuf(f"S32_{z}{h}", [128, 256], F32) for h in range(4)] for z in range(2)]
            Sb = [[S.sbuf(f"Sb_{z}{h}", [128, 256], BF16) for h in range(4)] for z in range(2)]
            for z in range(2):
                for h in range(4):
                    S.op(S.dve, lambda e, z=z, h=h: e.memset(S32[z][h][:], 0.0), writes=[S32[z][h]])
                    S.op(S.pool, lambda e, z=z, h=h: e.memset(Sb[z][h][:], 0.0), writes=[Sb[z][h]])
            qts = [[S.sbuf(f"gq{z}{i}", [128, 4, 128], BF16) for i in range(2)] for z in range(2)]
            kts = [[S.sbuf(f"gk{z}{i}", [128, 4, 128], BF16) for i in range(2)] for z in range(2)]
            vts = [[S.sbuf(f"gv{z}{i}", [128, D], BF16) for i in range(2)] for z in range(2)]
            gts = [[S.sbuf(f"gg{z}{i}", [16, 128], F32) for i in range(2)] for z in range(2)]
            osb = [[S.sbuf(f"go{z}{i}", [128, D], F32) for i in range(2)] for z in range(2)]
            W = {}
            for a in range(4):
                for nm, sh, dt in (("e1", [128, 128], F32), ("sp", [128, 128], F32), ("cum", [128, 128], F32),
                                   ("Eq", [128, 128], F32), ("Ek", [128, 128], F32), ("El", [128, 128], F32),
                                   ("sm", [128, 4], F32), ("qe", [128, 128], BF16), ("ke", [128, 128], BF16),
                                   ("kl", [128, 128], BF16), ("klT", [128, 128], BF16), ("attm", [128, 128], BF16)):
                    W[(nm, a)] = S.sbuf(f"g{nm}{a}", sh, dt)
            order = [[32, 33] + list(range(32)), [33, 32] + list(range(31, -1, -1))]
            na = 0
            def g2_loads(step):
                for z in range(2):
                    i = order[z][step]
                    sl = step % 2
                    st_i = i // 4 if i < NTL else 8
                    cs_ = slice(i * 128, (i + 1) * 128)
                    S.dma(S.sp, qts[z][sl][:], QTv[:, :, cs_], qts[z][sl], reads=[self.QTs[st_i]], writes=[qts[z][sl]])
                    S.dma(S.sp, kts[z][sl][:], KTv[:, :, cs_], kts[z][sl], reads=[self.KTs[st_i]], writes=[kts[z][sl]])
                    S.dma(S.sp, vts[z][sl][:], self.V[cs_, :], vts[z][sl], reads=[self.Vt[i]], writes=[vts[z][sl]])
                    S.dma(S.sp, gts[z][sl][:], self.GS[z, :, cs_], gts[z][sl], reads=[self.GSs[st_i]], writes=[gts[z][sl]])
            g2_loads(0)
            for step in range(NT):
                if step + 1 < NT:
                    g2_loads(step + 1)
                for h in range(4):
                    for z in range(2):
                        i = order[z][step]
                        sl = step % 2
                        a = na % 4
                        na += 1
                        qt, kt, vt, gt, ot = qts[z][sl], kts[z][sl], vts[z][sl], gts[z][sl], osb[z][sl]
                        e1, sp, cum, Eq, Ek, El, sm = (W[(n_, a)] for n_ in ("e1", "sp", "cum", "Eq", "Ek", "El", "sm"))
                        qe, ke, kl, klT, attm = (W[(n_, a)] for n_ in ("qe", "ke", "kl", "klT", "attm"))
                        if a < 3:
                            bA, bB = self.ps[a * 2], self.ps[a * 2 + 1]
                            vA, vB, vT = bA.t[:], bB.t[:], bA.t[:].bitcast(BF16)
                        else:
                            bA, bB = self.pb[0], self.pb[1]
                            vA, vB, vT = bA.t[:].bitcast(F32), bB.t[:].bitcast(F32), bA.t[:]
                        pla = patt = ptb = bA
                        pos = bB
                        s32, sb_ = S32[z][h], Sb[z][h]
                        S.op(S.pe, lambda e, z=z, h=h, gt=gt, vA=vA: e.matmul(vA[:, 0:128], wg[:, z, h * 128:(h + 1) * 128], gt[:], start=True, stop=True),
                             reads=[wg, gt], writes=[pla])
                        S.op(S.act, lambda e, e1=e1, vA=vA, z=z, h=h: e.activation(e1[:], vA[:, 0:128], AF.Exp, bias=negb[:, z, h:h + 1], scale=-1.0),
                             reads=[pla, negb], writes=[e1])
                        S.op(S.act, lambda e, e1=e1, sp=sp: e.activation(sp[:], e1[:], AF.Ln, bias=1.0, scale=1.0), reads=[e1], writes=[sp])
                        S.op(S.dve, lambda e, cum=cum, sp=sp: e.tensor_tensor_scan(cum[:], ones[:], sp[:], 0.0, ALU.mult, ALU.add), reads=[ones, sp], writes=[cum])
                        S.op(S.dve, lambda e, sm=sm, cum=cum: e.tensor_copy(sm[:, 0:1], cum[:, 127:128]), reads=[cum], writes=[sm])
                        S.op(S.dve, lambda e, sm=sm: e.tensor_scalar_mul(sm[:, 1:2], sm[:, 0:1], -1.0 / 16.0), reads=[sm], writes=[sm])
                        if z == 1:
                            S.op(S.dve, lambda e, cum=cum, sm=sm: e.tensor_scalar(cum[:], cum[:], -1.0, sm[:, 0:1], ALU.mult, ALU.add), reads=[cum, sm], writes=[cum])
                            S.op(S.dve, lambda e, cum=cum, sp=sp: e.tensor_tensor(cum[:], cum[:], sp[:], ALU.add), reads=[cum, sp], writes=[cum])
                        S.op(S.act, lambda e, Eq=Eq, cum=cum: e.activation(Eq[:], cum[:], AF.Exp, scale=-1.0 / 16.0), reads=[cum], writes=[Eq])
                        S.op(S.act, lambda e, Ek=Ek, cum=cum: e.activation(Ek[:], cum[:], AF.Exp, scale=1.0 / 16.0), reads=[cum], writes=[Ek])
                        S.op(S.act, lambda e, El=El, cum=cum, sm=sm: e.activation(El[:], cum[:], AF.Exp, bias=sm[:, 1:2], scale=1.0 / 16.0), reads=[cum, sm], writes=[El])
                        S.op(S.act, lambda e, sm=sm: e.activation(sm[:, 2:3], sm[:, 0:1], AF.Exp, scale=-1.0 / 16.0), reads=[sm], writes=[sm])
                        S.op(S.dve, lambda e, qe=qe, qt=qt, Eq=Eq, h=h: e.tensor_tensor(qe[:], qt[:, h, :], Eq[:], ALU.mult), reads=[qt, Eq], writes=[qe])
                        S.op(S.pool, lambda e, ke=ke, kt=kt, Ek=Ek, h=h: e.tensor_tensor(ke[:], kt[:, h, :], Ek[:], ALU.mult), reads=[kt, Ek], writes=[ke])
                        S.op(S.pool, lambda e, kl=kl, kt=kt, El=El, h=h: e.tensor_tensor(kl[:], kt[:, h, :], El[:], ALU.mult), reads=[kt, El], writes=[kl])
                        S.op(S.pe, lambda e, kl=kl, vT=vT: e.transpose(vT[:, 512:640], kl[:], self.idb[:]), reads=[kl, self.idb], writes=[ptb])
                        S.op(S.act, lambda e, klT=klT, vT=vT: e.copy(klT[:], vT[:, 512:640]), reads=[ptb], writes=[klT])
                        S.op(S.pe, lambda e, ke=ke, qe=qe, vA=vA: e.matmul(vA[:, 128:256], ke[:], qe[:], start=True, stop=True), reads=[ke, qe], writes=[patt])
                        S.op(S.dve, lambda e, attm=attm, vA=vA, z=z: e.tensor_tensor(attm[:], vA[:, 128:256], msk[:, z, :], ALU.mult), reads=[patt, msk], writes=[attm])
                        vh = slice(h * 256, (h + 1) * 256)

                        def mmo(e, attm=attm, vt=vt, qe=qe, sb_=sb_, vB=vB, vh=vh):
                            e.matmul(vB[:, 0:256], attm[:], vt[:, vh], start=True, stop=False)
                            return e.matmul(vB[:, 0:256], qe[:], sb_[:], start=False, stop=True)
                        S.op(S.pe, mmo, reads=[attm, vt, qe, sb_], writes=[pos])
                        S.op(S.act, lambda e, ot=ot, vB=vB, vh=vh: e.copy(ot[:, vh], vB[:, 0:256]), reads=[pos], writes=[ot])
                        S.op(S.pe, lambda e, klT=klT, vt=vt, vB=vB, vh=vh: e.matmul(vB[:, 256:512], klT[:], vt[:, vh], start=True, stop=True),
                             reads=[klT, vt], writes=[pos])
                        S.op(S.dve, lambda e, s32=s32, sm=sm, vB=vB: e.scalar_tensor_tensor(s32[:], s32[:], sm[:, 2:3], vB[:, 256:512], ALU.mult, ALU.add),
                             reads=[s32, sm, pos], writes=[s32])
                        S.op(S.act, lambda e, sb_=sb_, s32=s32: e.copy(sb_[:], s32[:]), reads=[s32], writes=[sb_])
                for z in range(2):
                    i = order[z][step]
                    ot = osb[z][step % 2]
                    S.dma(S.sp, self.OF[z, i * 128:(i + 1) * 128, :], ot[:], ot, reads=[ot], writes=[self.OFt[z][i]])
        with S.scope():
            wout = S.sbuf("gwout", [128, 8, D], BF16)
            S.dma(S.pool, wout[:], I["gla_w_out"][j].rearrange("(c p) n -> p c n", p=128), wout, writes=[wout])
            ngb = S.sbuf("ngb", [128, 256], F32)
            g1 = S.sbuf("g1", [128, D], F32)
            lng = S.sbuf("lng", [128, D], F32)
            lnb = S.sbuf("lnb", [128, D], F32)
            self.load_vec(ngb, I["gla_norm_g"][j])
            self.load_vec(lng, I["ln_g"][l, 0])
            self.load_vec(lnb, I["ln_b"][l, 0])
            xb = [S.sbuf(f"xb{i}", [128, D], F32) for i in range(2)]
            zb = [S.sbuf(f"zb{i}", [128, D], F32) for i in range(2)]
            ob = [S.sbuf(f"ob{i}", [128, D], F32) for i in range(2)]
            ofb = [S.sbuf(f"ofb{i}", [128, D], F32) for i in range(2)]
            obb = [S.sbuf(f"obb{i}", [128, D], F32) for i in range(2)]
            rb = [S.sbuf(f"rb{i}", [128, D], BF16) for i in range(2)]
            sr = S.sbuf("sr", [128, D], F32)
            junk = S.sbuf("junk", [128, 256], F32)
            sms = [S.sbuf(f"sml{i}", [128, 16], F32) for i in range(2)]
            st8 = [S.sbuf(f"st8{i}", [128, 8], F32) for i in range(2)]
            cats = [S.sbuf(f"cat{i}", [128, D], BF16) for i in range(2)]
            catT = S.sbuf("catT", [128, 8, 128], BF16)
            ntile = NTL if last else NT
            kind = None
            for i in range(ntile):
                k2 = 1 if i >= NTL else 0
                if k2 != kind:
                    kind = k2
                    self.load_mod(g1, l, kind, 2)
                x, z_, o, sm = xb[i % 2], zb[i % 2], ob[i % 2], sms[i % 2]
                of_, ob_, r_, st, cat = ofb[i % 2], obb[i % 2], rb[i % 2], st8[i % 2], cats[i % 2]
                S.dma(S.sp, x[:], self.xsrc(i).t, x, reads=[self.xsrc(i)], writes=[x])
                S.dma(S.sp, of_[:], self.OF[0, i * 128:(i + 1) * 128, :], of_, reads=[self.OFt[0][i]], writes=[of_])
                S.dma(S.sp, ob_[:], self.OF[1, i * 128:(i + 1) * 128, :], ob_, reads=[self.OFt[1][i]], writes=[ob_])
                S.dma(S.sp, r_[:], self.U[i * 128:(i + 1) * 128, :], r_, reads=[self.Ut[i]], writes=[r_])
                S.op(S.dve, lambda e, of_=of_, ob_=ob_: e.tensor_tensor(of_[:], of_[:], ob_[:], ALU.add), reads=[of_, ob_], writes=[of_])
                for h in range(4):
                    S.op(S.act, lambda e, of_=of_, st=st, h=h: e.activation(junk[:], of_[:, h * 256:(h + 1) * 256], AF.Square, accum_out=st[:, h:h + 1]),
                         reads=[of_], writes=[junk, st])
                S.op(S.dve, lambda e, st=st: e.tensor_scalar(st[:, 4:8], st[:, 0:4], 1.0 / 256.0, 1e-6, ALU.mult, ALU.add), reads=[st], writes=[st])
                S.op(S.act, lambda e, st=st: e.sqrt(st[:, 4:8], st[:, 4:8]), reads=[st], writes=[st])
                S.op(S.dve, lambda e, st=st: e.reciprocal(st[:, 4:8], st[:, 4:8]), reads=[st], writes=[st])
                S.op(S.act, lambda e, r_=r_: e.activation(sr[:], r_[:], AF.Silu), reads=[r_], writes=[sr])
                for h in range(4):
                    hs = slice(h * 256, (h + 1) * 256)
                    S.op(S.dve, lambda e, of_=of_, st=st, h=h, hs=hs: e.scalar_tensor_tensor(of_[:, hs], of_[:, hs], st[:, 4 + h:5 + h], ngb[:], ALU.mult, ALU.mult),
                         reads=[of_, st, ngb], writes=[of_])
                S.op(S.dve, lambda e, of_=of_, cat=cat: e.tensor_tensor(cat[:], of_[:], sr[:], ALU.mult), reads=[of_, sr], writes=[cat])
                self.out_proj_ln(l, i, cat, catT, wout, g1, x, z_, o, sm, lng, lnb)
        self.src_is_input = False

    def build(self):
        cfg = self.cfg
        layers = cfg.get("layers", list(range(DEPTH)))
        self.adaln(layers)
        for l in layers:
            last = l == DEPTH - 1
            if cfg.get("mixer", True):
                if l % 2 == 0:
                    self.mixer_even(l, last)
                else:
                    self.mixer_odd(l, last)
            if cfg.get("ffn", True):
                self.ffn(l, last)
        if cfg.get("dump_mod"):
            S = self.S
            tm = self.tap("moddump", [DEPTH, 2, 6 * D])
            with S.scope():
                mb = S.sbuf("mdump", [2, 6 * D], F32)
                for l in layers:
                    S.dma(S.sp, mb[:], self.MOD[l], mb, reads=[self.MODb[l]], writes=[mb])
                    S.dma(S.sp, tm[l], mb[:], mb, reads=[mb], is_output=True)
        if cfg.get("dump_x"):
            S = self.S
            tx = self.tap("xdump", [T, D])
            with S.scope():
                xb = [S.sbuf(f"dx{i}", [128, D], F32) for i in range(2)]
                for i in range(NT):
                    b = xb[i % 2]
                    S.dma(S.sp, b[:], self.Xt[i].t, b, reads=[self.Xt[i]], writes=[b])
                    S.dma(S.sp, tx[i * 128:(i + 1) * 128, :], b[:], b, reads=[b], is_output=True)
        self.S.finish()
        return self.nc


def na_bias_tables(rpb):
    out = np.full((rpb.shape[0], 5, 12, 128, 576), -30000.0, np.float32)
    qi = np.arange(128)
    ki = np.arange(576)
    for p, i in enumerate((0, 1, 2, 30, 31)):
        r0 = 2 * i
        kr0 = min(max(2 * i - 4, 0), 55)
        r = r0 + qi // 64
        col = qi % 64
        krow = kr0 + ki // 64
        kcol = ki % 64
        rs = np.clip(r - 4, 0, 56)
        ws = np.clip(col - 8, 0, 48)
        ok = ((krow[None, :] >= rs[:, None]) & (krow[None, :] < rs[:, None] + 8) &
              (kcol[None, :] >= ws[:, None]) & (kcol[None, :] < ws[:, None] + 16))
        drow = np.clip(krow[None, :] - r[:, None] + 7, 0, 14)
        dcol = np.clip(kcol[None, :] - col[:, None] + 15, 0, 30)
        g = rpb[:, :, drow, dcol]
        out[:, p] = np.where(ok[None, None], g, np.float32(-30000.0))
    return out


def pool_matrices():
    A = np.zeros((4, 5, 128, 128), np.float64)
    Tseq = 128 * 4
    for g, w in enumerate((2, 4, 8, 16)):
        for var, (ti, nb) in enumerate(((1, -1), (1, 0), (1, 1), (0, 0), (3, 0))):
            for tl in range(128):
                t = ti * 128 + tl
                lo = min(max(t - w // 2, 0), Tseq)
                hi = min(max(t - w // 2 + w, 0), Tseq)
                for sg in range(lo, hi):
                    sl = sg - (ti + nb) * 128
                    if 0 <= sl < 128:
                        A[g, var, sl, tl] += 1.0 / (hi - lo)
                if nb == 0:
                    A[g, var, tl, tl] -= 1.0
    return np.ascontiguousarray(A.reshape(20, 128, 128).astype(np.float32))


def rope_tables():
    t = np.arange(TL)
    row = (t // GW).astype(np.float32)
    col = (t % GW).astype(np.float32)
    nf = 32
    freqs = (np.float32(10000.0) ** (-np.arange(nf, dtype=np.float32) / nf)).astype(np.float32)
    ang = np.concatenate([row[:, None] * freqs, col[:, None] * freqs], axis=-1).astype(np.float32)
    return np.ascontiguousarray(np.stack([np.cos(ang), np.sin(ang)], axis=1).astype(np.float32))


def tri_masks():
    s_ = np.arange(128)[:, None]
    t_ = np.arange(128)[None, :]
    return np.ascontiguousarray(np.stack([(s_ <= t_), (s_ >= t_), (s_ < t_)], 0).astype(np.float32))


def host_inputs(inputs, cores):
    f = lambda a: np.ascontiguousarray(np.asarray(a, dtype=np.float32))
    shared = {
        "ada_w": f(inputs["ada_w"]), "ada_b": f(inputs["ada_b"]),
        "ln_g": f(inputs["ln_g"]), "ln_b": f(inputs["ln_b"]),
        "router_w": f(inputs["router_w"]), "router_b": f(inputs["router_b"]),
        "exp_w1": f(inputs["exp_w1"]), "exp_w2": f(inputs["exp_w2"]), "exp_b2": f(inputs["exp_b2"]),
        "exp_b1L": f(np.asarray(inputs["exp_b1"]).reshape(DEPTH, NE, 16, 128).transpose(0, 3, 1, 2)),
        "ident": np.eye(128, dtype=np.float32),
        "ab_w_in": f(inputs["ab_w_in"]), "ab_pool_w": f(inputs["ab_pool_w"]), "ab_pool_scale": f(inputs["ab_pool_scale"]),
        "ab_w_out": f(inputs["ab_w_out"]), "nabias": na_bias_tables(np.asarray(inputs["ab_rpb"], dtype=np.float32)),
        "poolA": pool_matrices(),
        "gla_w_in": f(inputs["gla_w_in"]), "gla_w_gate": f(inputs["gla_w_gate"]),
        "gla_bgL": f(np.asarray(inputs["gla_b_gate"]).reshape(2, 2, 4, 128).transpose(0, 3, 1, 2)),
        "gla_norm_g": f(inputs["gla_norm_g"]), "gla_w_out": f(inputs["gla_w_out"]),
        "ropecs": rope_tables(), "trimask": tri_masks(),
        "blkstart": (np.arange(128, dtype=np.float32) * BLK).reshape(128, 1),
        "rowbase": np.ascontiguousarray(np.concatenate([np.arange(8)[None, :] * 128 + np.arange(128)[:, None],
                                                        np.arange(128)[:, None]], axis=1).astype(np.float32)),
        "exp_b1T": f(np.asarray(inputs["exp_b1"]).reshape(DEPTH, NE, 16, 128).transpose(0, 1, 3, 2).reshape(DEPTH, NE * 128, 16)),
    }
    maps = []
    cc = np.asarray(inputs["c_ctx"], dtype=np.float32).reshape(8, 128).T
    for b in cores:
        m = dict(shared)
        m["xc"] = f(np.concatenate([inputs["x"][b], inputs["ctx"][b]], axis=0))
        cb = np.asarray(inputs["c"][b], dtype=np.float32).reshape(8, 128).T
        m["cvec"] = f(np.stack([cb, cc], axis=-1))
        maps.append(m)
    return maps


def kernel(**inputs):
    prog = Prog({})
    nc = prog.build()
    maps = host_inputs(inputs, list(range(8)))
    res = run_bass_kernel_spmd(nc, maps, core_ids=list(range(8)))
    out = np.stack([np.asarray(r["out"]) for r in res.results], axis=0)
    return out.astype(np.float32)
```

```python
import contextlib
import numpy as np
import concourse.bass as bass
import concourse.mybir as mybir
from concourse.bass_utils import run_bass_kernel_spmd

F32 = mybir.dt.float32
BF16 = mybir.dt.bfloat16
AF = mybir.ActivationFunctionType
ALU = mybir.AluOpType
AX = mybir.AxisListType

D = 1024
TL = 4096
NCTX = 256
T = TL + NCTX
NT = T // 128
NTL = TL // 128
DEPTH = 4
ALPHA = (2.0 * DEPTH) ** 0.25
NE = 32
GW = 64
BLK = 512
NBLK = (T * 4 + NE * (BLK - 1)) // BLK + 1
I32 = mybir.dt.int32

SAME_ENGINE_SYNC = True


class Buf:
    __slots__ = ("name", "t", "w", "r", "dsem", "dcnt", "excl")

    def __init__(self, name, t):
        self.excl = False
        self.name = name
        self.t = t
        self.w = None
        self.r = {}
        self.dsem = None
        self.dcnt = 0

    def __getitem__(self, k):
        return self.t[k]


class Eng:
    def __init__(self, name, h, sem):
        self.name = name
        self.h = h
        self.sem = sem
        self.cnt = 0
        self.seen = {}


class Sched:
    def __init__(self, nc):
        self.nc = nc
        self.es = contextlib.ExitStack()
        self.nsem = 0
        self.dma_latest = {}
        self.free_dsems = []
        self.free_dsems_sw = []
        self.sw_sems = set()
        self.scope_bufs = [[]]
        self.pe = self._eng("pe", nc.tensor)
        self.act = self._eng("act", nc.scalar)
        self.dve = self._eng("dve", nc.vector)
        self.pool = self._eng("pool", nc.gpsimd)
        self.sp = self._eng("sp", nc.sync)
        self.engs = [self.pe, self.act, self.dve, self.pool, self.sp]
        self.out_toks = []
        self.n_ins = 0

    def new_sem(self, name):
        self.nsem += 1
        return self.es.enter_context(self.nc.semaphore(f"{name}_{self.nsem}"))

    def _eng(self, name, h):
        return Eng(name, h, self.new_sem("e" + name))

    def sbuf(self, name, shape, dtype):
        st = self.scope_stacks[-1] if getattr(self, "scope_stacks", None) else self.es
        self.nbuf = getattr(self, "nbuf", 0) + 1
        t = st.enter_context(self.nc.sbuf_tensor(f"{name}_{self.nbuf}", list(shape), dtype))
        b = Buf(name, t)
        self.scope_bufs[-1].append(b)
        return b

    def psum(self, name, shape, dtype):
        t = self.es.enter_context(self.nc.psum_tensor(name, list(shape), dtype))
        b = Buf(name, t)
        b.excl = True
        return b

    def dram(self, name, shape, dtype):
        t = self.nc.dram_tensor(name, list(shape), dtype, kind="Internal")
        return t.ap()

    def view(self, name, ap):
        return Buf(name, ap)

    @contextlib.contextmanager
    def scope(self):
        if not getattr(self, "scope_stacks", None):
            self.scope_stacks = []
        st = contextlib.ExitStack()
        self.scope_stacks.append(st)
        self.scope_bufs.append([])
        try:
            yield
        finally:
            self.barrier()
            for b in self.scope_bufs.pop():
                if b.dsem is not None:
                    for sw, ent in b.dsem.items():
                        (self.free_dsems_sw if sw else self.free_dsems).append((ent[0], ent[1]))
                    b.dsem = None
            self.scope_stacks.pop()
            st.close()

    def _wait(self, eng, sem, val):
        if sem in self.dma_latest:
            val = max(val, self.dma_latest[sem])
        if sem is eng.sem and not SAME_ENGINE_SYNC:
            return
        if eng.seen.get(sem, 0) >= val:
            return
        eng.h.wait_ge(sem, val)
        eng.seen[sem] = val
        self.n_ins += 1

    def _deps(self, eng, reads, writes, skip_sem=None):
        for b in reads:
            if b.w is not None:
                self._wait(eng, *b.w)
        for b in writes:
            if b.w is not None and b.w[0] is not skip_sem:
                self._wait(eng, *b.w)
            for s, v in b.r.items():
                self._wait(eng, s, v)

    def _commit(self, tok, reads, writes):
        for b in writes:
            b.w = tok
            b.r = {}
        s, v = tok
        for b in reads:
            if b in writes:
                continue
            if b.r.get(s, 0) < v:
                b.r[s] = v

    def op(self, eng, fn, reads=(), writes=()):
        ex = [b for b in reads if b.excl and b not in writes]
        if ex:
            writes = list(writes) + ex
            reads = [b for b in reads if not b.excl]
        self._deps(eng, reads, writes)
        ins = fn(eng.h)
        eng.cnt += 1
        ins.then_inc(eng.sem, 1)
        self.n_ins += 1
        self._commit((eng.sem, eng.cnt), reads, writes)

    def dma(self, q, out_ap, in_ap, sb, reads=(), writes=(), is_output=False, **kw):
        sw = q is self.pool
        cur = sb.dsem[sw][0] if (sb.dsem and sw in sb.dsem) else None
        self._deps(q, reads, writes, skip_sem=cur if sb in writes else None)
        sem, cnt = self.dma_sem(sb, sw)
        q.h.dma_start(out=out_ap, in_=in_ap, **kw).then_inc(sem, 16)
        self.n_ins += 1
        tok = (sem, cnt)
        self._commit(tok, reads, writes)
        if is_output:
            self.out_toks.append(tok)

    def dma_sem(self, sb, sw):
        if sb.dsem is None:
            sb.dsem = {}
        if sw not in sb.dsem:
            fl = self.free_dsems_sw if sw else self.free_dsems
            if fl:
                sb.dsem[sw] = list(fl.pop())
            else:
                sb.dsem[sw] = [self.new_sem("dsw" if sw else "d"), 0]
                if sw:
                    self.sw_sems.add(sb.dsem[sw][0])
        ent = sb.dsem[sw]
        ent[1] += 16
        self.dma_latest[ent[0]] = ent[1]
        return ent[0], ent[1]

    def idma(self, out_ap, in_ap, sb, idx_ap, scatter, bound, reads=(), writes=()):
        q = self.pool
        cur = sb.dsem[True][0] if (sb.dsem and True in sb.dsem) else None
        self._deps(q, reads, writes, skip_sem=cur if sb in writes else None)
        sem, cnt = self.dma_sem(sb, True)
        off = bass.IndirectOffsetOnAxis(ap=idx_ap, axis=0)
        q.h.indirect_dma_start(out=out_ap, out_offset=off if scatter else None, in_=in_ap,
                               in_offset=None if scatter else off).then_inc(sem, 16)
        self.n_ins += 1
        self._commit((sem, cnt), reads, writes)

    def barrier(self):
        for e in self.engs:
            for f in self.engs:
                if f is not e and f.cnt > 0:
                    self._wait(e, f.sem, f.cnt)
            for s, v in self.dma_latest.items():
                self._wait(e, s, v)

    def finish(self):
        self.barrier()

    def close(self):
        self.es.close()


class LazyInputs(dict):
    def __init__(self, prog):
        super().__init__()
        self.prog = prog

    def __missing__(self, name):
        shape = list(self.prog.shapes[name])
        dev = self.prog.cfg.get("dev_slice")
        if dev and name in dev:
            shape = list(dev[name])
        ap = self.prog.nc.dram_tensor(name, shape, F32, kind="ExternalInput").ap()
        self[name] = ap
        return ap


class Prog:
    def __init__(self, cfg):
        self.cfg = cfg
        nc = self.nc = bass.Bass("TRN2", target_bir_lowering=False)
        S = self.S = Sched(nc)
        self.taps = {}

        self.shapes = {
            "xc": [T, D], "cvec": [128, 8, 2], "ada_w": [DEPTH, D, 6 * D], "ada_b": [DEPTH, 6 * D],
            "ln_g": [DEPTH, 2, D], "ln_b": [DEPTH, 2, D], "router_w": [DEPTH, D, NE], "router_b": [DEPTH, NE],
            "exp_w1": [DEPTH, NE, D, 2 * D], "exp_b1L": [DEPTH, 128, NE, 16], "exp_w2": [DEPTH, NE, D, D],
            "exp_b2": [DEPTH, NE, D], "ident": [128, 128],
            "ab_w_in": [2, D, 2560], "ab_pool_w": [2, 4, 64, 64], "ab_pool_scale": [2, 256],
            "ab_w_out": [2, D, D], "nabias": [2, 5, 12, 128, 576], "poolA": [20, 128, 128],
            "gla_w_in": [2, D, 3104], "gla_w_gate": [2, 2, 16, 512], "gla_bgL": [2, 128, 2, 4],
            "gla_norm_g": [2, 256], "gla_w_out": [2, D, D], "ropecs": [TL, 2, 64], "trimask": [3, 128, 128], "blkstart": [128, 1], "rowbase": [128, 9], "exp_b1T": [DEPTH, NE * 128, 16],
        }
        self.I = LazyInputs(self)
        self.out = nc.dram_tensor("out", [TL, D], F32, kind="ExternalOutput").ap()

        self.X = S.dram("X", [T, D], F32)
        self.Xt = [S.view(f"X{i}", self.X[i * 128:(i + 1) * 128, :]) for i in range(NT)]
        self.XCt = [S.view(f"XC{i}", self.I["xc"][i * 128:(i + 1) * 128, :]) for i in range(NT)]
        self.OUTt = [S.view(f"O{i}", self.out[i * 128:(i + 1) * 128, :]) for i in range(NTL)]
        self.MOD = S.dram("MOD", [DEPTH, 2, 6 * D], F32)
        self.MODb = [S.view(f"MOD{l}", self.MOD[l]) for l in range(DEPTH)]
        self.src_is_input = True
        self.QT = S.dram("QT", [768, T], BF16)
        self.KT = S.dram("KT", [768, T], BF16)
        self.V = S.dram("V", [T, 1024], BF16)
        self.U = S.dram("U", [T, 1024], BF16)
        self.XS = S.dram("XS", [NBLK * BLK, D], BF16)
        self.XSb = [S.view(f"XS{b}", self.XS) for b in range(NBLK)]
        self.XSall = S.view("XSall", self.XS)
        self.YS = S.dram("YS", [NBLK * BLK, D], F32)
        self.YSb = [S.view(f"YS{b}", self.YS) for b in range(NBLK)]
        self.HB = S.dram("HB", [T, D], BF16)
        self.HBt = [S.view(f"HB{i}", self.HB) for i in range(NT)]
        self.EB = S.dram("EB", [128, 1], I32)
        self.EBb = S.view("EBb", self.EB)
        self.xs_zeroed = False
        self.OF = S.dram("OF", [2, T, D], F32)
        self.OFt = [[S.view(f"OF{z}_{i}", self.OF) for i in range(NT)] for z in range(2)]
        self.GS = S.dram("GS", [2, 16, T], F32)
        self.GSs = [S.view(f"GS{i}", self.GS) for i in range(9)]
        self.QTs = [S.view(f"QT{i}", self.QT) for i in range(9)]
        self.KTs = [S.view(f"KT{i}", self.KT) for i in range(9)]
        self.Vt = [S.view(f"V{i}", self.V) for i in range(NT)]
        self.Ut = [S.view(f"U{i}", self.U) for i in range(NT)]

        self.idf = S.sbuf("idf", [128, 128], F32)
        self.idb = S.sbuf("idb", [128, 128], BF16)
        S.dma(S.sp, self.idf[:], self.I["ident"], self.idf, writes=[self.idf])
        S.op(S.dve, lambda e: e.tensor_copy(self.idb[:], self.idf[:]), reads=[self.idf], writes=[self.idb])
        self.ps = [S.psum(f"ps{i}", [128, 512], F32) for i in range(6)]
        self.pb = [S.psum(f"pb{i}", [128, 1024], BF16) for i in range(2)]

    def tap(self, name, shape, dt=F32):
        ap = self.nc.dram_tensor(name, list(shape), dt, kind="ExternalOutput").ap()
        self.taps[name] = ap
        return ap

    def xsrc(self, i):
        return self.XCt[i] if self.src_is_input else self.Xt[i]

    def adaln(self, layers):
        S, I = self.S, self.I
        with S.scope():
            cv = S.sbuf("cv", [128, 8, 2], F32)
            sc = S.sbuf("sc", [128, 8, 2], F32)
            S.dma(S.sp, cv[:], I["cvec"], cv, writes=[cv])
            S.op(S.act, lambda e: e.activation(sc[:], cv[:], AF.Silu), reads=[cv], writes=[sc])
            wts = [S.sbuf(f"adw{i}", [128, 8, 512], F32) for i in range(2)]
            bts = [S.sbuf(f"adb{i}", [2, 512], F32) for i in range(2)]
            msb = S.sbuf("msb", [2, 6 * D], F32)
            n = 0
            for l in layers:
                wv = I["ada_w"][l].rearrange("(c p) n -> p c n", p=128)
                for blk in range(12):
                    wt, bt = wts[n % 2], bts[n % 2]
                    ps = self.ps[n % 2]
                    n += 1
                    cs = slice(blk * 512, (blk + 1) * 512)
                    S.dma(S.sp, wt[:], wv[:, :, cs], wt, writes=[wt])
                    S.dma(S.sp, bt[:], I["ada_b"][l, cs].partition_broadcast(2), bt, writes=[bt])

                    def mm(e, wt=wt, ps=ps):
                        for k in range(8):
                            ins = e.matmul(ps[0:2, :], sc[:, k, :], wt[:, k, :], start=(k == 0), stop=(k == 7))
                        return ins
                    S.op(S.pe, mm, reads=[sc, wt], writes=[ps])
                    S.op(S.dve, lambda e, ps=ps, bt=bt, cs=cs: e.tensor_tensor(msb[:, cs], ps[0:2, :], bt[:], ALU.add),
                         reads=[ps, bt], writes=[msb])
                for j in (1, 4):
                    S.op(S.dve, lambda e, j=j: e.tensor_scalar_add(msb[:, j * D:(j + 1) * D], msb[:, j * D:(j + 1) * D], 1.0),
                         reads=[msb], writes=[msb])
                S.dma(S.sp, self.MOD[l], msb[:], msb, reads=[msb], writes=[self.MODb[l]])

    def load_mod(self, dst, l, which, j):
        S = self.S
        S.dma(S.sp, dst[:], self.MOD[l, which, j * D:(j + 1) * D].partition_broadcast(128), dst,
              reads=[self.MODb[l]], writes=[dst])

    def load_vec(self, dst, ap1d):
        S = self.S
        S.dma(S.sp, dst[:], ap1d.partition_broadcast(128), dst, writes=[dst])

    def layernorm(self, z, o, lng, lnb, sm):
        S = self.S

        S.op(S.dve, lambda e: e.bn_stats(sm[:, 0:6], z[:, 0:512]), reads=[z], writes=[sm])
        S.op(S.dve, lambda e: e.bn_stats(sm[:, 6:12], z[:, 512:1024]), reads=[z], writes=[sm])
        S.op(S.dve, lambda e: e.bn_aggr(sm[:, 12:14], sm[:, 0:12]), reads=[sm], writes=[sm])
        S.op(S.dve, lambda e: e.tensor_scalar_add(sm[:, 14:15], sm[:, 13:14], 1e-5), reads=[sm], writes=[sm])
        S.op(S.act, lambda e: e.sqrt(sm[:, 14:15], sm[:, 14:15]), reads=[sm], writes=[sm])
        S.op(S.dve, lambda e: e.reciprocal(sm[:, 14:15], sm[:, 14:15]), reads=[sm], writes=[sm])
        S.op(S.dve, lambda e: e.tensor_scalar(sm[:, 15:16], sm[:, 12:13], sm[:, 14:15], -1.0, ALU.mult, ALU.mult),
             reads=[sm], writes=[sm])
        S.op(S.act, lambda e: e.activation(o[:], z[:], AF.Identity, bias=sm[:, 15:16], scale=sm[:, 14:15]),
             reads=[z, sm], writes=[o])
        S.op(S.dve, lambda e: e.tensor_tensor(o[:], o[:], lng[:], ALU.mult), reads=[o, lng], writes=[o])
        S.op(S.dve, lambda e: e.tensor_tensor(o[:], o[:], lnb[:], ALU.add), reads=[o, lnb], writes=[o])

    def ffn(self, l, last):
        S, I = self.S, self.I
        if self.cfg.get("moe", "sparse") == "sparse":
            self.ffn_sparse(l, last)
            self.src_is_input = False
            return
        ntiles = NTL if last else NT
        groups = []
        t = 0
        while t < ntiles:
            groups.append((t, min(t + 12, ntiles)))
            t += 12
        for (t0, t1) in groups:
            self.ffn_group(l, last, t0, t1)
        self.src_is_input = False

    def ffn_sparse(self, l, last):
        S, I = self.S, self.I
        ntile = NTL if last else NT
        nblk = (ntile * 128 * 4 + NE * (BLK - 1)) // BLK + 1
        lw = 0 if "exp_w1" in (self.cfg.get("dev_slice") or {}) else l
        with S.scope():
            G = S.sbuf("G", [128, ntile, NE], F32)
            G4 = S.sbuf("G4", [128, ntile, 4], F32)
            D4 = S.sbuf("D4", [128, ntile * 4], I32)
            IDX = S.sbuf("IDX", [128, 128 * 8], I32)
            IDXB = S.sbuf("IDXB", [128, 128], I32)
            if not self.xs_zeroed:
                self.xs_zeroed = True
                with S.scope():
                    zt = S.sbuf("zt", [128, 4, D], BF16)
                    S.op(S.dve, lambda e: e.memset(zt[:], 0.0), writes=[zt])
                    for b in range(NBLK):
                        S.dma(S.sp, self.XS[b * BLK:(b + 1) * BLK, :].rearrange("(c p) d -> p c d", p=128), zt[:], zt, reads=[zt], writes=[self.XSb[b]])
            with S.scope():
                MK = S.sbuf("MK", [128, ntile, NE], F32)
                RK = S.sbuf("RK", [128, ntile, NE], F32)
                s2 = S.sbuf("s2", [128, D], F32)
                h2 = S.sbuf("h2", [128, D], F32)
                rw = S.sbuf("rw", [128, 8, NE], F32)
                rb = S.sbuf("rb", [128, NE], F32)
                tri = S.sbuf("tri", [128, 128], F32)
                ustr = S.sbuf("ustr", [128, 128], BF16)
                onesb = S.sbuf("onesb", [128, 128], BF16)
                onesf = S.sbuf("onesf", [128, NE], F32)
                blk = S.sbuf("blk", [128, 1], F32)
                cnt = S.sbuf("cnt", [128, NE], F32)
                xb = [S.sbuf(f"xb{i}", [128, D], F32) for i in range(2)]
                h32 = [S.sbuf(f"h32{i}", [128, D], F32) for i in range(2)]
                hbs = [S.sbuf(f"hbs{i}", [128, D], BF16) for i in range(2)]
                hT32s = [S.sbuf(f"hT32{i}", [128, 8, 128], F32) for i in range(2)]
                sms_ = [S.sbuf(f"smr{i}", [128, 160], F32) for i in range(2)]
                mkbs = [S.sbuf(f"mkb{i}", [128, NE], BF16) for i in range(2)]
                sm = sms_[0]
                S.dma(S.sp, rw[:], I["router_w"][l].rearrange("(c p) n -> p c n", p=128), rw, writes=[rw])
                self.load_vec(rb, I["router_b"][l])
                S.dma(S.sp, tri[:], I["trimask"][2], tri, writes=[tri])
                S.dma(S.sp, blk[:], I["blkstart"], blk, writes=[blk])
                S.op(S.dve, lambda e: e.tensor_copy(ustr[:], tri[:]), reads=[tri], writes=[ustr])
                S.op(S.dve, lambda e: e.memset(onesb[:], 1.0), writes=[onesb])
                S.op(S.dve, lambda e: e.memset(onesf[:], 1.0), writes=[onesf])
                S.op(S.dve, lambda e: e.memset(cnt[:], 0.0), writes=[cnt])
                kind = None
                for ti in range(ntile):
                    i = ti
                    k2 = 0 if i < NTL else 1
                    if k2 != kind:
                        kind = k2
                        self.load_mod(s2, l, kind, 4)
                        self.load_mod(h2, l, kind, 3)
                    x, h, hb_ = xb[ti % 2], h32[ti % 2], hbs[ti % 2]
                    hT32, sm, mkb = hT32s[ti % 2], sms_[ti % 2], mkbs[ti % 2]
                    S.dma(S.sp, x[:], self.xsrc(i).t, x, reads=[self.xsrc(i)], writes=[x])
                    S.op(S.dve, lambda e, h=h, x=x: e.tensor_tensor(h[:], x[:], s2[:], ALU.mult), reads=[x, s2], writes=[h])
                    S.op(S.dve, lambda e, h=h: e.tensor_tensor(h[:], h[:], h2[:], ALU.add), reads=[h, h2], writes=[h])
                    S.op(S.act, lambda e, h=h, hb_=hb_: e.copy(hb_[:], h[:]), reads=[h], writes=[hb_])
                    S.dma(S.pool, self.HB[i * 128:(i + 1) * 128, :], hb_[:], hb_, reads=[hb_], writes=[self.HBt[i]])
                    cols = slice(ti * 128, (ti + 1) * 128)
                    for half in range(2):
                        pp = self.ps[half + 2 * (ti % 2)]

                        def tr(e, pp=pp, half=half, h=h):
                            for c in range(4):
                                cc = half * 4 + c
                                ins = e.transpose(pp[:, c * 128:(c + 1) * 128], h[:, cc * 128:(cc + 1) * 128], self.idf[:])
                            return ins
                        S.op(S.pe, tr, reads=[h, self.idf], writes=[pp])
                        S.op(S.act, lambda e, pp=pp, half=half, hT32=hT32: e.copy(hT32[:, half * 4:(half + 1) * 4, :],
                                                                          pp[:].rearrange("p (c t) -> p c t", c=4)),
                             reads=[pp], writes=[hT32])
                    pr = self.ps[4 + ti % 2]

                    def rmm(e, pr=pr, hT32=hT32):
                        for k in range(8):
                            ins = e.matmul(pr[:, 0:NE], hT32[:, k, :], rw[:, k, :], start=(k == 0), stop=(k == 7))
                        return ins
                    S.op(S.pe, rmm, reads=[hT32, rw], writes=[pr])
                    lg, top8, nmx, ex, ssum = sm[:, 0:32], sm[:, 32:40], sm[:, 40:41], sm[:, 48:80], sm[:, 112:113]
                    smB = sm
                    mk = MK[:, ti, :]
                    S.op(S.dve, lambda e: e.tensor_tensor(lg, pr[:, 0:NE], rb[:], ALU.add), reads=[pr, rb], writes=[sm])
                    S.op(S.dve, lambda e: e.max(top8, lg), reads=[sm], writes=[sm])
                    S.op(S.dve, lambda e: e.tensor_scalar_mul(nmx, top8[:, 0:1], -1.0), reads=[sm], writes=[sm])
                    S.op(S.dve, lambda e, mk=mk: e.tensor_scalar(mk, lg, top8[:, 3:4], None, ALU.is_ge), reads=[sm], writes=[MK])
                    S.op(S.act, lambda e: e.activation(ex, lg, AF.Exp, bias=nmx, scale=1.0), reads=[sm], writes=[sm])
                    S.op(S.dve, lambda e, mk=mk: e.tensor_tensor(ex, ex, mk, ALU.mult), reads=[sm, MK], writes=[sm])
                    S.op(S.dve, lambda e: e.reduce_sum(ssum, ex, AX.X), reads=[sm], writes=[sm])
                    S.op(S.dve, lambda e: e.reciprocal(ssum, ssum), reads=[sm], writes=[sm])
                    S.op(S.dve, lambda e, ti=ti: e.tensor_scalar_mul(G[:, ti, :], ex, ssum), reads=[sm], writes=[G])
                    S.op(S.dve, lambda e, mk=mk: e.tensor_copy(mkb[:], mk), reads=[MK], writes=[mkb])
                    pk = self.ps[4]

                    def rkmm(e):
                        e.matmul(pk[:, 0:NE], ustr[:], mkb[:], start=True, stop=True)
                        return e.matmul(pk[:, NE:2 * NE], onesb[:], mkb[:], start=True, stop=True)
                    S.op(S.pe, rkmm, reads=[ustr, onesb, mkb], writes=[pk])
                    S.op(S.dve, lambda e, ti=ti: e.tensor_tensor(RK[:, ti, :], pk[:, 0:NE], cnt[:], ALU.add), reads=[pk, cnt], writes=[RK])
                    S.op(S.dve, lambda e: e.tensor_tensor(cnt[:], cnt[:], pk[:, NE:2 * NE], ALU.add), reads=[pk, cnt], writes=[cnt])
                pad, pend, ps1, tmp = sm[:, 0:32], sm[:, 32:64], sm[:, 64:96], sm[:, 96:128]
                S.op(S.dve, lambda e: e.tensor_single_scalar(tmp, cnt[:], 0.0, ALU.is_gt), reads=[cnt], writes=[sm])
                for m in range(1, (T + BLK - 1) // BLK):
                    S.op(S.dve, lambda e, m=m: e.scalar_tensor_tensor(tmp, cnt[:], float(m * BLK), tmp, ALU.is_gt, ALU.add), reads=[cnt, sm], writes=[sm])
                S.op(S.dve, lambda e: e.tensor_scalar_mul(pad, tmp, float(BLK)), reads=[sm], writes=[sm])
                S.op(S.dve, lambda e: e.tensor_tensor_scan(pend, onesf[:], pad, 0.0, ALU.mult, ALU.add), reads=[sm, onesf], writes=[sm])
                S.op(S.dve, lambda e: e.tensor_tensor(ps1, pend, pad, ALU.subtract), reads=[sm], writes=[sm])
                S.op(S.dve, lambda e: e.tensor_scalar_add(ps1, ps1, 1.0), reads=[sm], writes=[sm])
                S.op(S.dve, lambda e: e.tensor_scalar(tmp, pend, blk[:, 0:1], None, ALU.is_le), reads=[sm, blk], writes=[sm])
                S.op(S.dve, lambda e: e.reduce_sum(sm[:, 128:129], tmp, AX.X), reads=[sm], writes=[sm])
                S.op(S.dve, lambda e: e.tensor_scalar_min(sm[:, 128:129], sm[:, 128:129], float(NE - 1)), reads=[sm], writes=[sm])
                ebi = S.sbuf("ebi", [128, 1], I32)
                S.op(S.dve, lambda e: e.tensor_copy(ebi[:], sm[:, 128:129]), reads=[sm], writes=[ebi])
                S.dma(S.sp, self.EB, ebi[:], ebi, reads=[ebi], writes=[self.EBb])
                ebB = S.sbuf("ebB", [128, 128], I32)
                ebF = S.sbuf("ebF", [128, 128], F32)
                rbase = S.sbuf("rbase", [128, 9], F32)
                idxf = S.sbuf("idxf", [128, 128 * 8], F32)
                S.dma(S.sp, ebB[:], self.EB.rearrange("p o -> (p o)").partition_broadcast(128), ebB, reads=[self.EBb], writes=[ebB])
                S.dma(S.sp, rbase[:], I["rowbase"], rbase, writes=[rbase])
                S.op(S.dve, lambda e: e.tensor_copy(ebF[:], ebB[:]), reads=[ebB], writes=[ebF])
                S.op(S.dve, lambda e: e.scalar_tensor_tensor(idxf[:].rearrange("p (b k) -> p b k", k=8),
                                                             ebF[:].rearrange("p (b o) -> p b o", o=1).to_broadcast([128, 128, 8]), 1024.0,
                                                             rbase[:, 0:8].rearrange("p (o k) -> p o k", o=1).to_broadcast([128, 128, 8]),
                                                             ALU.mult, ALU.add), reads=[ebF, rbase], writes=[idxf])
                if lw:
                    S.op(S.dve, lambda e: e.tensor_scalar_add(idxf[:], idxf[:], float(lw * NE * D)), reads=[idxf], writes=[idxf])
                S.op(S.dve, lambda e: e.tensor_copy(IDX[:], idxf[:]), reads=[idxf], writes=[IDX])
                S.op(S.dve, lambda e: e.tensor_scalar(idxf[:, 0:128], ebF[:], 128.0, rbase[:, 8:9], ALU.mult, ALU.add), reads=[ebF, rbase, idxf], writes=[idxf])
                if l:
                    S.op(S.dve, lambda e: e.tensor_scalar_add(idxf[:, 0:128], idxf[:, 0:128], float(l * NE * 128)), reads=[idxf], writes=[idxf])
                S.op(S.dve, lambda e: e.tensor_copy(IDXB[:], idxf[:, 0:128]), reads=[idxf], writes=[IDXB])
                key, t8, eq, d4f = sm[:, 0:32], sm[:, 96:104], sm[:, 104:136], sm[:, 136:140]
                for ti in range(ntile):
                    hb_ = hbs[ti % 2]
                    S.dma(S.sp, hb_[:], self.HB[ti * 128:(ti + 1) * 128, :], hb_, reads=[self.HBt[ti]], writes=[hb_])
                    S.op(S.dve, lambda e, ti=ti: e.tensor_tensor(key, RK[:, ti, :], ps1, ALU.add), reads=[RK, sm], writes=[sm])
                    S.op(S.dve, lambda e, ti=ti: e.tensor_tensor(key, key, MK[:, ti, :], ALU.mult), reads=[MK, sm], writes=[sm])
                    S.op(S.dve, lambda e: e.max(t8, key), reads=[sm], writes=[sm])
                    S.op(S.dve, lambda e: e.tensor_scalar_add(d4f, t8[:, 0:4], -1.0), reads=[sm], writes=[sm])
                    S.op(S.dve, lambda e, ti=ti: e.tensor_copy(D4[:, ti * 4:ti * 4 + 4], d4f), reads=[sm], writes=[D4])
                    for k in range(4):
                        S.op(S.dve, lambda e, k=k: e.tensor_scalar(eq, key, t8[:, k:k + 1], None, ALU.is_equal), reads=[sm], writes=[sm])
                        S.op(S.dve, lambda e, ti=ti: e.tensor_tensor(eq, eq, G[:, ti, :], ALU.mult), reads=[sm, G], writes=[sm])
                        S.op(S.dve, lambda e, ti=ti, k=k: e.reduce_sum(G4[:, ti, k:k + 1], eq, AX.X), reads=[sm], writes=[G4])
                    for k in range(4):
                        S.idma(self.XS[:, :], hb_[:, :], hb_, D4[:, ti * 4 + k:ti * 4 + k + 1], True, NBLK * BLK - 1,
                               reads=[hb_, D4], writes=[self.XSall] + self.XSb)
            if self.cfg.get("tap_route"):
                tg = self.tap("t_g4", [128, ntile, 4])
                S.dma(S.sp, tg, G4[:], G4, reads=[G4], is_output=True)
                td = self.tap("t_d4", [128, ntile * 4], I32)
                S.dma(S.sp, td, D4[:], D4, reads=[D4], is_output=True)
                te = self.tap("t_eb", [128, 128], I32)
                S.dma(S.sp, te, IDXB[:], IDXB, reads=[IDXB], is_output=True)
            with S.scope():
                w1b = [S.sbuf(f"w1b{i}", [128, 8, 2 * D], BF16) for i in range(2)]
                w2b = [S.sbuf(f"w2b{i}", [128, 8, D], BF16) for i in range(2)]
                b1s = [S.sbuf(f"b1s{i}", [128, 16], F32) for i in range(2)]
                xts = [S.sbuf(f"xts{i}", [128, D], BF16) for i in range(4)]
                hTs = [S.sbuf(f"hTb{i}", [128, 8, BLK], BF16) for i in range(2)]
                actT = [S.sbuf(f"actT{j}", [128, BLK], BF16) for j in range(8)]
                g32 = [S.sbuf(f"g32{i}", [128, BLK], F32) for i in range(3)]
                sg = [S.sbuf(f"sg{i}", [128, BLK], F32) for i in range(3)]
                l32 = [S.sbuf(f"l32{i}", [128, BLK], F32) for i in range(3)]
                gs = [S.sbuf(f"gs{i}", [128, BLK], F32) for i in range(3)]
                yts = [S.sbuf(f"yts{i}", [128, D], F32) for i in range(2)]
                w1tab = I["exp_w1"].rearrange("l e r n -> (l e r) n")
                w2tab = I["exp_w2"].rearrange("l e r n -> (l e r) n")
                b1tab = I["exp_b1T"].rearrange("l r c -> (l r) c")

                def weight_jobs(b):
                    buf = b % 2
                    jobs = [lambda: S.idma(b1s[buf][:, :], b1tab, b1s[buf], IDXB[:, b:b + 1], False, None, reads=[IDXB], writes=[b1s[buf]])]
                    for k in range(8):
                        jobs.append(lambda k=k: S.idma(w1b[buf][:, k, :], w1tab, w1b[buf], IDX[:, b * 8 + k:b * 8 + k + 1], False, None,
                                                       reads=[IDX], writes=[w1b[buf]]))
                    for k in range(8):
                        jobs.append(lambda k=k: S.idma(w2b[buf][:, k, :], w2tab, w2b[buf], IDX[:, b * 8 + k:b * 8 + k + 1], False, None,
                                                       reads=[IDX], writes=[w2b[buf]]))
                    return jobs

                def load_rows(bb):
                    hT_ = hTs[bb % 2]
                    for c in range(4):
                        xt = xts[c]
                        S.dma(S.sp, xt[:], self.XS[bb * BLK + c * 128:bb * BLK + (c + 1) * 128, :], xt, reads=[self.XSb[bb], self.XSall], writes=[xt])
                        pt = self.pb[c % 2]

                        def trx(e, xt=xt, pt=pt):
                            for cc in range(8):
                                ins = e.transpose(pt[:, cc * 128:(cc + 1) * 128], xt[:, cc * 128:(cc + 1) * 128], self.idb[:])
                            return ins
                        S.op(S.pe, trx, reads=[xt, self.idb], writes=[pt])
                        S.op(S.act, lambda e, pt=pt, hT_=hT_, c=c: e.copy(hT_[:, :, c * 128:(c + 1) * 128], pt[:].rearrange("p (c t) -> p c t", c=8)),
                             reads=[pt], writes=[hT_])
                pending = weight_jobs(0)
                for jb in pending:
                    jb()
                pending = []
                na = 0
                for b in range(nblk):
                    buf = b % 2
                    hT = hTs[b % 2]
                    if b + 1 < nblk:
                        pending = weight_jobs(b + 1)
                    if b == 0:
                        load_rows(0)
                    for j in range(8):
                        a = na % 2
                        na += 1
                        pgl, pll = self.ps[2 * a], self.ps[2 * a + 1]

                        def mm1(e, j=j, pgl=pgl, pll=pll, hT=hT, buf=buf):
                            for k in range(8):
                                e.matmul(pgl[:], w1b[buf][:, k, j * 128:(j + 1) * 128], hT[:, k, :], start=(k == 0), stop=(k == 7))
                            for k in range(8):
                                ins = e.matmul(pll[:], w1b[buf][:, k, D + j * 128:D + (j + 1) * 128], hT[:, k, :], start=(k == 0), stop=(k == 7))
                            return ins
                        S.op(S.pe, mm1, reads=[w1b[buf], hT], writes=[pgl, pll])
                        a3 = (na - 1) % 3
                        g_, s_, l_, gs_ = g32[a3], sg[a3], l32[a3], gs[a3]
                        S.op(S.dve, lambda e, g_=g_, pgl=pgl, j=j, buf=buf: e.tensor_scalar(g_[:], pgl[:], b1s[buf][:, j:j + 1], 7.0, ALU.add, ALU.min),
                             reads=[pgl, b1s[buf]], writes=[g_])
                        S.op(S.act, lambda e, g_=g_, s_=s_: e.activation(s_[:], g_[:], AF.Sigmoid, scale=1.702), reads=[g_], writes=[s_])
                        S.op(S.dve, lambda e, l_=l_, pll=pll, j=j, buf=buf: e.tensor_scalar(l_[:], pll[:], b1s[buf][:, 8 + j:9 + j], 7.0, ALU.add, ALU.min),
                             reads=[pll, b1s[buf]], writes=[l_])
                        S.op(S.dve, lambda e, l_=l_: e.tensor_scalar(l_[:], l_[:], -7.0, 1.0, ALU.max, ALU.add), reads=[l_], writes=[l_])
                        S.op(S.dve, lambda e, g_=g_, s_=s_, gs_=gs_: e.tensor_tensor(gs_[:], g_[:], s_[:], ALU.mult), reads=[g_, s_], writes=[gs_])
                        S.op(S.dve, lambda e, gs_=gs_, l_=l_, j=j: e.tensor_tensor(actT[j][:], gs_[:], l_[:], ALU.mult), reads=[gs_, l_], writes=[actT[j]])
                        for _ in range(3):
                            if pending:
                                pending.pop(0)()
                    if b + 1 < nblk:
                        load_rows(b + 1)
                    n = 0
                    for c in range(4):
                        yt = yts[c % 2]
                        for hf in range(2):
                            py = self.ps[4 + n % 2]
                            n += 1

                            def mm2(e, py=py, c=c, hf=hf, buf=buf):
                                for j in range(8):
                                    ins = e.matmul(py[:], actT[j][:, c * 128:(c + 1) * 128], w2b[buf][:, j, hf * 512:(hf + 1) * 512],
                                                   start=(j == 0), stop=(j == 7))
                                return ins
                            S.op(S.pe, mm2, reads=actT + [w2b[buf]], writes=[py])
                            if hf == 0:
                                S.op(S.dve, lambda e, py=py, yt=yt: e.tensor_copy(yt[:, 0:512], py[:]), reads=[py], writes=[yt])
                            else:
                                S.op(S.act, lambda e, py=py, yt=yt: e.copy(yt[:, 512:1024], py[:]), reads=[py], writes=[yt])
                        S.dma(S.sp, self.YS[b * BLK + c * 128:b * BLK + (c + 1) * 128, :], yt[:], yt, reads=[yt], writes=[self.YSb[b]])
                    while pending:
                        pending.pop(0)()
            with S.scope():
                b2t = S.sbuf("b2t", [NE, D], F32)
                g2 = S.sbuf("g2", [128, D], F32)
                lng = S.sbuf("lng", [128, D], F32)
                lnb = S.sbuf("lnb", [128, D], F32)
                xb = [S.sbuf(f"xb{i}", [128, D], F32) for i in range(2)]
                zb = [S.sbuf(f"zb{i}", [128, D], F32) for i in range(2)]
                ob = [S.sbuf(f"ob{i}", [128, D], F32) for i in range(2)]
                yk = [[S.sbuf(f"yk{i}_{k}", [128, D], F32) for k in range(4)] for i in range(2)]
                sms = [S.sbuf(f"sml{i}", [128, 16], F32) for i in range(2)]
                gts_ = [S.sbuf(f"gtt{i}", [NE, 128], F32) for i in range(2)]
                S.dma(S.sp, b2t[:], I["exp_b2"][l], b2t, writes=[b2t])
                self.load_vec(lng, I["ln_g"][l, 1])
                self.load_vec(lnb, I["ln_b"][l, 1])
                def c_loads(tj):
                    S.dma(S.sp, xb[tj % 2][:], self.xsrc(tj).t, xb[tj % 2], reads=[self.xsrc(tj)], writes=[xb[tj % 2]])
                    for k in range(4):
                        S.idma(yk[tj % 2][k][:, :], self.YS[:, :], yk[tj % 2][k], D4[:, tj * 4 + k:tj * 4 + k + 1], False, NBLK * BLK - 1,
                               reads=self.YSb[:nblk] + [D4], writes=[yk[tj % 2][k]])
                kind = None
                for ti in range(ntile):
                    i = ti
                    k2 = 0 if i < NTL else 1
                    if k2 != kind:
                        kind = k2
                        self.load_mod(g2, l, kind, 5)
                    x, z, o, sm = xb[ti % 2], zb[ti % 2], ob[ti % 2], sms[ti % 2]
                    if ti == 0:
                        c_loads(0)
                    if ti + 1 < ntile:
                        c_loads(ti + 1)
                    pg = self.ps[3]
                    gt = gts_[ti % 2]
                    S.op(S.pe, lambda e, ti=ti, pg=pg: e.transpose(pg[0:NE, 0:128], G[:, ti, :], self.idf[:]), reads=[G, self.idf], writes=[pg])
                    S.op(S.act, lambda e, gt=gt, pg=pg: e.copy(gt[:], pg[0:NE, 0:128]), reads=[pg], writes=[gt])
                    for hf in range(2):
                        py = self.ps[hf]
                        S.op(S.pe, lambda e, py=py, gt=gt, hf=hf: e.matmul(py[:], gt[:], b2t[:, hf * 512:(hf + 1) * 512], start=True, stop=True),
                             reads=[gt, b2t], writes=[py])
                        S.op(S.act, lambda e, py=py, z=z, hf=hf: e.copy(z[:, hf * 512:(hf + 1) * 512], py[:]), reads=[py], writes=[z])
                    for k in range(4):
                        ykk = yk[ti % 2][k]
                        S.op(S.dve, lambda e, z=z, ykk=ykk, ti=ti, k=k: e.scalar_tensor_tensor(z[:], ykk[:], G4[:, ti, k:k + 1], z[:], ALU.mult, ALU.add),
                             reads=[ykk, G4, z], writes=[z])
                    S.op(S.dve, lambda e, z=z: e.tensor_tensor(z[:], z[:], g2[:], ALU.mult), reads=[z, g2], writes=[z])
                    S.op(S.dve, lambda e, z=z, x=x: e.scalar_tensor_tensor(z[:], x[:], ALPHA, z[:], ALU.mult, ALU.add), reads=[x, z], writes=[z])
                    self.layernorm(z, o, lng, lnb, sm)
                    if last and l == DEPTH - 1:
                        S.dma(S.sp, self.out[i * 128:(i + 1) * 128, :], o[:], o, reads=[o], writes=[self.OUTt[i]], is_output=True)
                    else:
                        S.dma(S.sp, self.Xt[i].t, o[:], o, reads=[o], writes=[self.Xt[i]])

    def ffn_group(self, l, last, t0, t1):
        S, I = self.S, self.I
        ng = t1 - t0
        ncols = ng * 128
        with S.scope():
            HT = S.sbuf("HT", [128, 8, ncols], BF16)
            yacc = S.sbuf("yacc", [128, ng, D], F32)
            yb = [[S.view(f"y{a}_{b}", yacc[:, a, b * 512:(b + 1) * 512]) for b in range(2)] for a in range(ng)]
            G = S.sbuf("G", [128, ng, NE], F32)
            GT = S.sbuf("GT", [NE, ncols], F32)
            with S.scope():
                s2 = S.sbuf("s2", [128, D], F32)
                h2 = S.sbuf("h2", [128, D], F32)
                rw = S.sbuf("rw", [128, 8, NE], F32)
                rb = S.sbuf("rb", [128, NE], F32)
                xb = [S.sbuf(f"xb{i}", [128, D], F32) for i in range(2)]
                h32 = [S.sbuf(f"h32{i}", [128, D], F32) for i in range(2)]
                hT32 = S.sbuf("hT32", [128, 8, 128], F32)
                sm = S.sbuf("smr", [128, 128], F32)
                S.dma(S.sp, rw[:], I["router_w"][l].rearrange("(c p) n -> p c n", p=128), rw, writes=[rw])
                self.load_vec(rb, I["router_b"][l])
                kind = None
                S.dma(S.sp, xb[0][:], self.xsrc(t0).t, xb[0], reads=[self.xsrc(t0)], writes=[xb[0]])
                for ti in range(ng):
                    i = t0 + ti
                    k2 = 0 if i < NTL else 1
                    if k2 != kind:
                        kind = k2
                        self.load_mod(s2, l, kind, 4)
                        self.load_mod(h2, l, kind, 3)
                    x = xb[ti % 2]
                    h = h32[ti % 2]
                    if ti + 1 < ng:
                        xn = xb[(ti + 1) % 2]
                        S.dma(S.sp, xn[:], self.xsrc(i + 1).t, xn, reads=[self.xsrc(i + 1)], writes=[xn])
                    S.op(S.dve, lambda e, h=h, x=x: e.tensor_tensor(h[:], x[:], s2[:], ALU.mult), reads=[x, s2], writes=[h])
                    S.op(S.dve, lambda e, h=h: e.tensor_tensor(h[:], h[:], h2[:], ALU.add), reads=[h, h2], writes=[h])
                    cols = slice(ti * 128, (ti + 1) * 128)
                    lvl = self.cfg.get('f1_level', 9)
                    if lvl <= 1:
                        continue
                    for half in range(2):
                        pp = self.ps[half]

                        def tr(e, pp=pp, half=half, h=h):
                            for c in range(4):
                                cc = half * 4 + c
                                ins = e.transpose(pp[:, c * 128:(c + 1) * 128], h[:, cc * 128:(cc + 1) * 128], self.idf[:])
                            return ins
                        S.op(S.pe, tr, reads=[h, self.idf], writes=[pp])
                        S.op(S.act, lambda e, pp=pp, half=half: e.copy(hT32[:, half * 4:(half + 1) * 4, :],
                                                                          pp[:].rearrange("p (c t) -> p c t", c=4)),
                             reads=[pp], writes=[hT32])
                        S.op(S.dve, lambda e, pp=pp, half=half, cols=cols: e.tensor_copy(
                            HT[:, half * 4:(half + 1) * 4, cols], pp[:].rearrange("p (c t) -> p c t", c=4)),
                            reads=[pp], writes=[HT])
                    if lvl <= 2:
                        continue
                    pr = self.ps[2]

                    def rmm(e):
                        for k in range(8):
                            ins = e.matmul(pr[:, 0:NE], hT32[:, k, :], rw[:, k, :], start=(k == 0), stop=(k == 7))
                        return ins
                    S.op(S.pe, rmm, reads=[hT32, rw], writes=[pr])
                    if lvl <= 3:
                        continue
                    lg, top8, nmx, ex, mk, ssum = sm[:, 0:32], sm[:, 32:40], sm[:, 40:41], sm[:, 48:80], sm[:, 80:112], sm[:, 112:113]
                    S.op(S.dve, lambda e: e.tensor_tensor(lg, pr[:, 0:NE], rb[:], ALU.add), reads=[pr, rb], writes=[sm])
                    S.op(S.dve, lambda e: e.max(top8, lg), reads=[sm], writes=[sm])
                    S.op(S.dve, lambda e: e.tensor_scalar_mul(nmx, top8[:, 0:1], -1.0), reads=[sm], writes=[sm])
                    S.op(S.dve, lambda e: e.tensor_scalar(mk, lg, top8[:, 3:4], None, ALU.is_ge), reads=[sm], writes=[sm])
                    S.op(S.act, lambda e: e.activation(ex, lg, AF.Exp, bias=nmx, scale=1.0), reads=[sm], writes=[sm])
                    S.op(S.dve, lambda e: e.tensor_tensor(ex, ex, mk, ALU.mult), reads=[sm], writes=[sm])
                    S.op(S.dve, lambda e: e.reduce_sum(ssum, ex, AX.X), reads=[sm], writes=[sm])
                    S.op(S.dve, lambda e: e.reciprocal(ssum, ssum), reads=[sm], writes=[sm])
                    S.op(S.dve, lambda e, ti=ti: e.tensor_scalar_mul(G[:, ti, :], ex, ssum), reads=[sm], writes=[G])
                    if lvl <= 4:
                        continue
                    pg = self.ps[3]
                    S.op(S.pe, lambda e, ti=ti: e.transpose(pg[0:NE, 0:128], G[:, ti, :], self.idf[:]), reads=[G, self.idf], writes=[pg])
                    S.op(S.act, lambda e, cols=cols: e.copy(GT[:, cols], pg[0:NE, 0:128]), reads=[pg], writes=[GT])
            if self.cfg.get("tap_gates") and t0 == 0:
                tg = self.tap("gates", [128, 12, NE])
                S.dma(S.sp, tg, G[:], G, reads=[G], is_output=True)
            if self.cfg.get('stop') == 'F1':
                return
            with S.scope():
                w1p = [S.sbuf(f"w1p{i}", [128, 8, 256], BF16) for i in range(4)]
                w2b = [S.sbuf(f"w2b{i}", [128, 8, D], BF16) for i in range(2)]
                b1t = S.sbuf("b1t", [128, NE, 16], F32)
                b2t = S.sbuf("b2t", [NE, D], F32)
                actT = [S.sbuf(f"actT{j}", [128, ncols], BF16) for j in range(8)]
                g32 = [S.sbuf(f"g32{i}", [128, 512], F32) for i in range(2)]
                sg = [S.sbuf(f"sg{i}", [128, 512], F32) for i in range(2)]
                l32 = [S.sbuf(f"l32{i}", [128, 512], F32) for i in range(2)]
                gs = [S.sbuf(f"gs{i}", [128, 512], F32) for i in range(2)]
                S.dma(S.sp, b1t[:], I["exp_b1L"][l], b1t, writes=[b1t])
                S.dma(S.sp, b2t[:], I["exp_b2"][l], b2t, writes=[b2t])
                n = 0
                for ti in range(ng):
                    for hf in range(2):
                        py = self.ps[4 + n % 2]
                        n += 1
                        S.op(S.pe, lambda e, py=py, ti=ti, hf=hf: e.matmul(py[:], GT[:, ti * 128:(ti + 1) * 128],
                                                                             b2t[:, hf * 512:(hf + 1) * 512], start=True, stop=True),
                             reads=[GT, b2t], writes=[py])
                        S.op(S.act, lambda e, py=py, ti=ti, hf=hf: e.copy(yb[ti][hf][:], py[:]), reads=[py], writes=[yb[ti][hf]])
                sts = []
                c = 0
                while c < ncols:
                    sts.append((c, min(512, ncols - c)))
                    c += 512
                experts = self.cfg.get("experts", range(NE))
                lw = 0 if "exp_w1" in (self.cfg.get("dev_slice") or {}) else l
                cnt = 0
                for ei, ex_ in enumerate(experts):
                    w2 = w2b[ei % 2]
                    w1v = I["exp_w1"][lw, ex_].rearrange("(c p) n -> p c n", p=128)
                    S.dma(S.pool, w2[:], I["exp_w2"][lw, ex_].rearrange("(c p) n -> p c n", p=128), w2, writes=[w2])
                    for j in range(8):
                        pc = w1p[(ei * 8 + j) % 4]
                        S.dma(S.pool, pc[:, :, 0:128], w1v[:, :, j * 128:(j + 1) * 128], pc, writes=[pc])
                        S.dma(S.pool, pc[:, :, 128:256], w1v[:, :, D + j * 128:D + (j + 1) * 128], pc, writes=[pc])
                        for (c0, n_) in sts:
                            a = cnt % 2
                            cnt += 1
                            pgl, pll = self.ps[2 * a], self.ps[2 * a + 1]
                            cs = slice(c0, c0 + n_)

                            def mm1(e, pc=pc, pgl=pgl, pll=pll, cs=cs, n_=n_):
                                for k in range(8):
                                    e.matmul(pgl[:, 0:n_], pc[:, k, 0:128], HT[:, k, cs], start=(k == 0), stop=(k == 7))
                                for k in range(8):
                                    ins = e.matmul(pll[:, 0:n_], pc[:, k, 128:256], HT[:, k, cs], start=(k == 0), stop=(k == 7))
                                return ins
                            S.op(S.pe, mm1, reads=[pc, HT], writes=[pgl, pll])
                            g_, s_, l_, gs_ = g32[a], sg[a], l32[a], gs[a]
                            S.op(S.dve, lambda e, g_=g_, pgl=pgl, n_=n_, j=j, ex_=ex_: e.tensor_scalar(
                                g_[:, 0:n_], pgl[:, 0:n_], b1t[:, ex_, j:j + 1], 7.0, ALU.add, ALU.min),
                                reads=[pgl, b1t], writes=[g_])
                            S.op(S.act, lambda e, g_=g_, s_=s_, n_=n_: e.activation(s_[:, 0:n_], g_[:, 0:n_], AF.Sigmoid, scale=1.702),
                                 reads=[g_], writes=[s_])
                            S.op(S.dve, lambda e, l_=l_, pll=pll, n_=n_, j=j, ex_=ex_: e.tensor_scalar(
                                l_[:, 0:n_], pll[:, 0:n_], b1t[:, ex_, 8 + j:9 + j], 7.0, ALU.add, ALU.min),
                                reads=[pll, b1t], writes=[l_])
                            S.op(S.pool, lambda e, l_=l_, n_=n_: e.tensor_scalar(l_[:, 0:n_], l_[:, 0:n_], -7.0, 1.0, ALU.max, ALU.add),
                                 reads=[l_], writes=[l_])
                            S.op(S.pool, lambda e, g_=g_, s_=s_, gs_=gs_, n_=n_: e.tensor_tensor(gs_[:, 0:n_], g_[:, 0:n_], s_[:, 0:n_], ALU.mult),
                                 reads=[g_, s_], writes=[gs_])
                            S.op(S.dve, lambda e, gs_=gs_, l_=l_, j=j, cs=cs, n_=n_: e.tensor_tensor(actT[j][:, cs], gs_[:, 0:n_], l_[:, 0:n_], ALU.mult),
                                 reads=[gs_, l_], writes=[actT[j]])
                    n = 0
                    for ti in range(ng):
                        for hf in range(2):
                            py = self.ps[4 + n % 2]
                            n += 1

                            def mm2(e, py=py, ti=ti, hf=hf, w2=w2):
                                for j in range(8):
                                    ins = e.matmul(py[:], actT[j][:, ti * 128:(ti + 1) * 128], w2[:, j, hf * 512:(hf + 1) * 512],
                                                   start=(j == 0), stop=(j == 7))
                                return ins
                            S.op(S.pe, mm2, reads=actT + [w2], writes=[py])
                            yy = yb[ti][hf]
                            S.op(S.dve, lambda e, py=py, ti=ti, yy=yy, ex_=ex_: e.scalar_tensor_tensor(
                                yy[:], py[:], G[:, ti, ex_:ex_ + 1], yy[:], ALU.mult, ALU.add),
                                reads=[py, G, yy], writes=[yy])
            if self.cfg.get('stop') == 'F2':
                return
            with S.scope():
                g2 = S.sbuf("g2", [128, D], F32)
                lng = S.sbuf("lng", [128, D], F32)
                lnb = S.sbuf("lnb", [128, D], F32)
                xb = [S.sbuf(f"xb{i}", [128, D], F32) for i in range(2)]
                zb = [S.sbuf(f"zb{i}", [128, D], F32) for i in range(2)]
                ob = [S.sbuf(f"ob{i}", [128, D], F32) for i in range(2)]
                sms = [S.sbuf(f"sml{i}", [128, 16], F32) for i in range(2)]
                self.load_vec(lng, I["ln_g"][l, 1])
                self.load_vec(lnb, I["ln_b"][l, 1])
                kind = None
                S.dma(S.sp, xb[0][:], self.xsrc(t0).t, xb[0], reads=[self.xsrc(t0)], writes=[xb[0]])
                for ti in range(ng):
                    i = t0 + ti
                    k2 = 0 if i < NTL else 1
                    if k2 != kind:
                        kind = k2
                        self.load_mod(g2, l, kind, 5)
                    x, z, o, sm = xb[ti % 2], zb[ti % 2], ob[ti % 2], sms[ti % 2]
                    if ti + 1 < ng:
                        xn = xb[(ti + 1) % 2]
                        S.dma(S.sp, xn[:], self.xsrc(i + 1).t, xn, reads=[self.xsrc(i + 1)], writes=[xn])
                    S.op(S.dve, lambda e, z=z, ti=ti: e.tensor_tensor(z[:], yacc[:, ti, :], g2[:], ALU.mult),
                         reads=[yb[ti][0], yb[ti][1], g2], writes=[z])
                    S.op(S.dve, lambda e, z=z, x=x: e.scalar_tensor_tensor(z[:], x[:], ALPHA, z[:], ALU.mult, ALU.add),
                         reads=[x, z], writes=[z])
                    self.layernorm(z, o, lng, lnb, sm)
                    if last and l == DEPTH - 1:
                        S.dma(S.sp, self.out[i * 128:(i + 1) * 128, :], o[:], o, reads=[o], writes=[self.OUTt[i]], is_output=True)
                    else:
                        S.dma(S.sp, self.Xt[i].t, o[:], o, reads=[o], writes=[self.Xt[i]])


    def st_range(self, lo, hi):
        return list(range(lo // 512, (hi - 1) // 512 + 1))

    def mixer_even(self, l, last):
        S, I = self.S, self.I
        j = l // 2
        QTv = self.QT.rearrange("(f p) t -> p f t", p=128)
        KTv = self.KT.rearrange("(f p) t -> p f t", p=128)
        with S.scope():
            win = S.sbuf("win", [128, 8, 2560], BF16)
            wv = I["ab_w_in"][j].rearrange("(c p) n -> p c n", p=128)
            for b5 in range(5):
                S.dma(S.pool, win[:, :, b5 * 512:(b5 + 1) * 512], wv[:, :, b5 * 512:(b5 + 1) * 512], win, writes=[win])
            s1 = S.sbuf("s1", [128, D], F32)
            h1 = S.sbuf("h1", [128, D], F32)
            xb = [S.sbuf(f"xb{i}", [128, D], F32) for i in range(2)]
            h32 = S.sbuf("h32", [128, D], F32)
            hb = [S.sbuf(f"hb{i}", [128, D], BF16) for i in range(2)]
            hTs = [S.sbuf(f"hT{i}", [128, 8, 512], BF16) for i in range(2)]
            qks = [S.sbuf(f"qk{i}", [128, 12, 512], BF16) for i in range(2)]
            uvs = [S.sbuf(f"uv{i}", [128, 1024], BF16) for i in range(2)]
            kind = None
            nx = 0
            for s_ in range(9):
                tiles = list(range(4 * s_, 4 * s_ + 4)) if s_ < 8 else [32, 33]
                N = 128 * len(tiles)
                c0 = tiles[0] * 128
                hT = hTs[s_ % 2]
                qk = qks[s_ % 2]
                k2 = 0 if s_ < 8 else 1
                if k2 != kind:
                    kind = k2
                    self.load_mod(s1, l, kind, 1)
                    self.load_mod(h1, l, kind, 0)
                for tt, i in enumerate(tiles):
                    x = xb[nx % 2]
                    hbb = hb[nx % 2]
                    nx += 1
                    S.dma(S.sp, x[:], self.xsrc(i).t, x, reads=[self.xsrc(i)], writes=[x])
                    S.op(S.dve, lambda e, x=x: e.tensor_tensor(h32[:], x[:], s1[:], ALU.mult), reads=[x, s1], writes=[h32])
                    S.op(S.dve, lambda e, hbb=hbb: e.tensor_tensor(hbb[:], h32[:], h1[:], ALU.add), reads=[h32, h1], writes=[hbb])
                    pt = self.pb[0]

                    def tr(e, hbb=hbb, pt=pt):
                        for c in range(8):
                            ins = e.transpose(pt[:, c * 128:(c + 1) * 128], hbb[:, c * 128:(c + 1) * 128], self.idb[:])
                        return ins
                    S.op(S.pe, tr, reads=[hbb, self.idb], writes=[pt])
                    S.op(S.act, lambda e, pt=pt, hT=hT, tt=tt: e.copy(hT[:, :, tt * 128:(tt + 1) * 128],
                                                                      pt[:].rearrange("p (c t) -> p c t", c=8)),
                         reads=[pt], writes=[hT])
                for tt, i in enumerate(tiles):
                    uv = uvs[i % 2]
                    pu, pv1, pv2 = self.ps[0], self.ps[1], self.ps[2]
                    tc_ = slice(tt * 128, (tt + 1) * 128)

                    def mmuv(e, tc_=tc_, hT=hT, pu=pu, pv1=pv1, pv2=pv2):
                        for k in range(8):
                            e.matmul(pu[:, 0:256], hT[:, k, tc_], win[:, k, 0:256], start=(k == 0), stop=(k == 7))
                        for k in range(8):
                            e.matmul(pv1[:], hT[:, k, tc_], win[:, k, 1792:2304], start=(k == 0), stop=(k == 7))
                        for k in range(8):
                            ins = e.matmul(pv2[:, 0:256], hT[:, k, tc_], win[:, k, 2304:2560], start=(k == 0), stop=(k == 7))
                        return ins
                    S.op(S.pe, mmuv, reads=[hT, win], writes=[pu, pv1, pv2])
                    S.op(S.dve, lambda e, uv=uv, pu=pu: e.tensor_copy(uv[:, 0:256], pu[:, 0:256]), reads=[pu], writes=[uv])
                    S.op(S.act, lambda e, uv=uv, pv1=pv1: e.copy(uv[:, 256:768], pv1[:]), reads=[pv1], writes=[uv])
                    S.op(S.dve, lambda e, uv=uv, pv2=pv2: e.tensor_copy(uv[:, 768:1024], pv2[:, 0:256]), reads=[pv2], writes=[uv])
                    S.dma(S.pool, self.U[i * 128:(i + 1) * 128, 0:256], uv[:, 0:256], uv, reads=[uv], writes=[self.Ut[i]])
                    S.dma(S.pool, self.V[i * 128:(i + 1) * 128, 0:768], uv[:, 256:1024], uv, reads=[uv], writes=[self.Vt[i]])
                for f in range(12):
                    pq = self.ps[3 + f % 2]

                    def mmq(e, f=f, pq=pq, hT=hT, N=N):
                        for k in range(8):
                            ins = e.matmul(pq[:, 0:N], win[:, k, 256 + f * 128:256 + (f + 1) * 128], hT[:, k, 0:N],
                                           start=(k == 0), stop=(k == 7))
                        return ins
                    S.op(S.pe, mmq, reads=[hT, win], writes=[pq])
                    if f < 6:
                        S.op(S.act, lambda e, f=f, pq=pq, qk=qk, N=N: e.mul(qk[:, f, 0:N], pq[:, 0:N], 0.125), reads=[pq], writes=[qk])
                    else:
                        S.op(S.dve, lambda e, f=f, pq=pq, qk=qk, N=N: e.tensor_copy(qk[:, f, 0:N], pq[:, 0:N]), reads=[pq], writes=[qk])
                S.dma(S.pool, QTv[:, :, c0:c0 + N], qk[:, 0:6, 0:N], qk, reads=[qk], writes=[self.QTs[s_]])
                S.dma(S.pool, KTv[:, :, c0:c0 + N], qk[:, 6:12, 0:N], qk, reads=[qk], writes=[self.KTs[s_]])
        with S.scope():
            bias = S.sbuf("nab", [128, 12, 832], F32)
            kTc = S.sbuf("kTc", [128, 6, 256], BF16)
            vc = S.sbuf("vc", [128, 2, 768], BF16)
            wout = S.sbuf("wout", [128, 8, D], BF16)
            Wbd = S.sbuf("Wbd", [128, 2, 256], BF16)
            pA = S.sbuf("pA", [128, 20, 128], BF16)
            psc = S.sbuf("psc", [128, 256], F32)
            g1 = S.sbuf("g1", [128, D], F32)
            lng = S.sbuf("lng", [128, D], F32)
            lnb = S.sbuf("lnb", [128, D], F32)
            kws = [S.sbuf(f"kw{i}", [128, 6, 576], BF16) for i in range(2)]
            vws = [S.sbuf(f"vw{i}", [128, 5, 768], BF16) for i in range(2)]
            qts = [S.sbuf(f"qt{i}", [128, 6, 128], BF16) for i in range(2)]
            uws = [S.sbuf(f"uw{i}", [128, 3, 256], BF16) for i in range(2)]
            xb = [S.sbuf(f"xb{i}", [128, D], F32) for i in range(2)]
            zb = [S.sbuf(f"zb{i}", [128, D], F32) for i in range(2)]
            ob = [S.sbuf(f"ob{i}", [128, D], F32) for i in range(2)]
            sms = [S.sbuf(f"sml{i}", [128, 16], F32) for i in range(2)]
            ssb = [S.sbuf(f"ssb{i}", [128, 832], F32) for i in range(3)]
            psb = [S.sbuf(f"psb{i}", [128, 832], BF16) for i in range(3)]
            pTs = [S.sbuf(f"pT{i}", [128, 7, 128], BF16) for i in range(3)]
            st4 = [S.sbuf(f"st4{i}", [128, 4], F32) for i in range(3)]
            rs12 = [S.sbuf(f"rs12{i}", [128, 24], F32) for i in range(2)]
            cats = [S.sbuf(f"cat{i}", [128, D], BF16) for i in range(2)]
            catT = S.sbuf("catT", [128, 8, 128], BF16)
            pooled = S.sbuf("pooled", [128, 256], BF16)
            pT2 = S.sbuf("pT2", [128, 2, 128], BF16)
            S.dma(S.sp, kTc[:], KTv[:, :, TL:T], kTc, reads=[self.KTs[8]], writes=[kTc])
            S.dma(S.sp, vc[:], self.V[TL:T, 0:768].rearrange("(c p) d -> p c d", p=128), vc, reads=[self.Vt[32], self.Vt[33]], writes=[vc])
            S.dma(S.pool, wout[:], I["ab_w_out"][j].rearrange("(c p) n -> p c n", p=128), wout, writes=[wout])
            S.dma(S.pool, pA[:], I["poolA"].rearrange("v s t -> s v t"), pA, writes=[pA])
            S.op(S.dve, lambda e: e.memset(Wbd[:], 0.0), writes=[Wbd])
            for g in range(4):
                S.dma(S.pool, Wbd[(g % 2) * 64:(g % 2) * 64 + 64, g // 2, g * 64:(g + 1) * 64], I["ab_pool_w"][j, g], Wbd, writes=[Wbd])
            self.load_vec(psc, I["ab_pool_scale"][j])
            self.load_vec(lng, I["ln_g"][l, 0])
            self.load_vec(lnb, I["ln_b"][l, 0])
            S.op(S.dve, lambda e: e.memset(bias[:, :, 0:256], 0.0), writes=[bias])
            ntile = NTL if last else NT
            kind = None
            cur_pat = None
            nh = 0
            for i in range(ntile):
                isctx = i >= NTL
                k2 = 1 if isctx else 0
                if k2 != kind:
                    kind = k2
                    self.load_mod(g1, l, kind, 2)
                x, z, o, sm = xb[i % 2], zb[i % 2], ob[i % 2], sms[i % 2]
                qt, kw, vw, uw, cat = qts[i % 2], kws[i % 2], vws[i % 2], uws[i % 2], cats[i % 2]
                S.dma(S.sp, x[:], self.xsrc(i).t, x, reads=[self.xsrc(i)], writes=[x])
                S.dma(S.sp, qt[:], QTv[:, :, i * 128:(i + 1) * 128], qt, reads=[self.QTs[i // 4 if not isctx else 8]], writes=[qt])
                base, nseq = (0, NTL) if not isctx else (NTL, 2)
                li = i - base
                blocks = []
                if li > 0:
                    blocks.append((0, 0))
                blocks.append((1, 3 if li == 0 else (4 if li == nseq - 1 else 1)))
                if li < nseq - 1:
                    blocks.append((2, 2))
                for (nb, var) in blocks:
                    ii = i + nb - 1
                    S.dma(S.sp, uw[:, nb, :], self.U[ii * 128:(ii + 1) * 128, 0:256], uw, reads=[self.Ut[ii]], writes=[uw])
                if not isctx:
                    pat = 0 if i == 0 else 1 if i == 1 else 3 if i == 30 else 4 if i == 31 else 2
                    if pat != cur_pat:
                        cur_pat = pat
                        S.dma(S.sp, bias[:, :, 256:832], I["nabias"][j, pat].rearrange("h q k -> q h k"), bias, writes=[bias])
                    kr0 = min(max(2 * i - 4, 0), 55)
                    k0 = kr0 * 64
                    S.dma(S.sp, kw[:], KTv[:, :, k0:k0 + 576], kw, reads=[self.KTs[a] for a in self.st_range(k0, k0 + 576)], writes=[kw])
                    vr = [self.Vt[a] for a in range(k0 // 128, (k0 + 575) // 128 + 1)]
                    S.dma(S.sp, vw[:, 0:4, :], self.V[k0:k0 + 512, 0:768].rearrange("(c p) d -> p c d", p=128), vw, reads=vr, writes=[vw])
                    S.dma(S.sp, vw[0:64, 4, :], self.V[k0 + 512:k0 + 576, 0:768], vw, reads=vr, writes=[vw])
                pp = self.ps[4]

                def mmpool(e, blocks=blocks, uw=uw, pp=pp):
                    for g in range(4):
                        for bi, (nb, var) in enumerate(blocks):
                            ins = e.matmul(pp[:, g * 64:(g + 1) * 64], pA[:, g * 5 + var, :], uw[:, nb, g * 64:(g + 1) * 64],
                                           start=(bi == 0), stop=(bi == len(blocks) - 1))
                    return ins
                S.op(S.pe, mmpool, reads=[pA, uw], writes=[pp])
                S.op(S.act, lambda e, pp=pp: e.copy(pooled[:], pp[:, 0:256]), reads=[pp], writes=[pooled])
                ptb = self.pb[1]

                def trp(e, ptb=ptb):
                    for c in range(2):
                        ins = e.transpose(ptb[:, c * 128:(c + 1) * 128], pooled[:, c * 128:(c + 1) * 128], self.idb[:])
                    return ins
                S.op(S.pe, trp, reads=[pooled, self.idb], writes=[ptb])
                S.op(S.dve, lambda e, ptb=ptb: e.tensor_copy(pT2[:], ptb[:, 0:256].rearrange("p (c t) -> p c t", c=2)), reads=[ptb], writes=[pT2])
                py = self.ps[5]

                def mmy(e, py=py):
                    for c in range(2):
                        ins = e.matmul(py[:, 0:256], pT2[:, c, :], Wbd[:, c, :], start=(c == 0), stop=(c == 1))
                    return ins
                S.op(S.pe, mmy, reads=[pT2, Wbd], writes=[py])
                S.op(S.dve, lambda e, py=py, cat=cat: e.tensor_tensor(cat[:, 0:256], py[:, 0:256], psc[:], ALU.mult), reads=[py, psc], writes=[cat])
                hstate = {}

                def stage_a(h):
                    nonlocal nh
                    hp, hh = h // 2, h % 2
                    psl = slice(hh * 64, hh * 64 + 64)
                    ss, pb_, pT, st = ssb[nh % 3], psb[nh % 3], pTs[nh % 3], st4[nh % 3]
                    rs = rs12[i % 2]
                    rcol = (h % 2) * 6 + h // 2
                    nh += 1
                    pS1, pS2 = self.ps[2 * (h % 3)], self.ps[2 * (h % 3) + 1]
                    nk = 256 if isctx else 832
                    nblk = 2 if isctx else 7

                    def mms(e, qt=qt, kw=kw, hp=hp, psl=psl, pS1=pS1, pS2=pS2, isctx=isctx):
                        ins = e.matmul(pS1[:, 0:256], qt[psl, hp, :], kTc[psl, hp, :], start=True, stop=True)
                        if not isctx:
                            e.matmul(pS1[:, 256:512], qt[psl, hp, :], kw[psl, hp, 0:256], start=True, stop=True)
                            ins = e.matmul(pS2[:, 0:320], qt[psl, hp, :], kw[psl, hp, 256:576], start=True, stop=True)
                        return ins
                    S.op(S.pe, mms, reads=[qt, kTc] + ([] if isctx else [kw]), writes=[pS1] + ([] if isctx else [pS2]))
                    if isctx:
                        S.op(S.act, lambda e, ss=ss, pS1=pS1: e.copy(ss[:, 0:256], pS1[:, 0:256]), reads=[pS1], writes=[ss])
                    else:
                        S.op(S.dve, lambda e, ss=ss, pS1=pS1, h=h: e.tensor_tensor(ss[:, 0:512], pS1[:], bias[:, h, 0:512], ALU.add),
                             reads=[pS1, bias], writes=[ss])
                        S.op(S.dve, lambda e, ss=ss, pS2=pS2, h=h: e.tensor_tensor(ss[:, 512:832], pS2[:, 0:320], bias[:, h, 512:832], ALU.add),
                             reads=[pS2, bias], writes=[ss])
                    S.op(S.dve, lambda e, ss=ss, st=st, nk=nk: e.reduce_max(st[:, 0:1], ss[:, 0:nk], AX.X), reads=[ss], writes=[st])
                    S.op(S.dve, lambda e, st=st: e.tensor_scalar_mul(st[:, 1:2], st[:, 0:1], -1.0), reads=[st], writes=[st])
                    S.op(S.act, lambda e, ss=ss, pb_=pb_, st=st, nk=nk: e.activation(pb_[:, 0:nk], ss[:, 0:nk], AF.Exp, bias=st[:, 1:2], scale=1.0,
                                                                               accum_out=rs[:, rcol:rcol + 1]),
                         reads=[ss, st], writes=[pb_, rs])
                    hstate[h] = (pb_, pT, pS1, nblk)

                def stage_b(h):
                    pb_, pT, pS1, nblk = hstate.pop(h)
                    ptb = pS1
                    ptv = pS1.t[:].bitcast(BF16)

                    def trs(e, pb_=pb_, ptb=ptv, nblk=nblk):
                        for c in range(nblk):
                            if c < 6:
                                ins = e.transpose(ptb[:, c * 128:(c + 1) * 128], pb_[:, c * 128:(c + 1) * 128], self.idb[:])
                            else:
                                ins = e.transpose(ptb[0:64, 768:896], pb_[:, 768:832], self.idb[:])
                        return ins
                    S.op(S.pe, trs, reads=[pb_, self.idb], writes=[ptb])
                    cp_eng = S.act if h % 2 == 0 else S.dve
                    if cp_eng is S.act:
                        S.op(S.act, lambda e, pT=pT, ptv=ptv, nblk=nblk: e.copy(pT[:, 0:nblk, :], ptv[:, 0:nblk * 128].rearrange("p (c t) -> p c t", c=nblk)),
                             reads=[ptb], writes=[pT])
                    else:
                        S.op(S.dve, lambda e, pT=pT, ptv=ptv, nblk=nblk: e.tensor_copy(pT[:, 0:nblk, :], ptv[:, 0:nblk * 128].rearrange("p (c t) -> p c t", c=nblk)),
                             reads=[ptb], writes=[pT])
                    po = self.pb[h % 2]
                    pov = po.t[:].bitcast(F32)
                    oc = slice((h // 2) * 64, (h // 2) * 64 + 64)

                    def mmpv(e, pT=pT, vw=vw, po=pov, oc=oc, h=h, nblk=nblk):
                        for c in range(nblk):
                            if c < 2:
                                ins = e.matmul(po[:, oc], pT[:, c, :], vc[:, c, h * 64:(h + 1) * 64], start=(c == 0), stop=(c == nblk - 1))
                            elif c < 6:
                                ins = e.matmul(po[:, oc], pT[:, c, :], vw[:, c - 2, h * 64:(h + 1) * 64], start=False, stop=False)
                            else:
                                ins = e.matmul(po[:, oc], pT[0:64, 6, :], vw[0:64, 4, h * 64:(h + 1) * 64], start=False, stop=True)
                        return ins
                    S.op(S.pe, mmpv, reads=[pT, vc] + ([] if isctx else [vw]), writes=[po])

                stage_a(0)
                for h in range(12):
                    if h + 1 < 12:
                        stage_a(h + 1)
                    stage_b(h)

                rs = rs12[i % 2]
                S.op(S.dve, lambda e, rs=rs: e.reciprocal(rs[:, 12:24], rs[:, 0:12]), reads=[rs], writes=[rs])
                cat4 = cat[:, 256:1024].rearrange("p (a two d) -> p a two d", two=2, d=64)
                for par in range(2):
                    po = self.pb[par]
                    pov2 = po.t[:].bitcast(F32)
                    S.op(S.dve, lambda e, po=pov2, par=par, rs=rs, cat4=cat4: e.tensor_tensor(
                        cat4[:, :, par, :], po[:, 0:384].rearrange("p (a d) -> p a d", d=64),
                        rs[:, 12 + par * 6:18 + par * 6].rearrange("p (a o) -> p a o", o=1).to_broadcast([128, 6, 64]), ALU.mult),
                        reads=[po, rs], writes=[cat])
                self.out_proj_ln(l, i, cat, catT, wout, g1, x, z, o, sm, lng, lnb)
        self.src_is_input = False

    def out_proj_ln(self, l, i, cat, catT, wout, g1, x, z, o, sm, lng, lnb):
        S = self.S
        ptb = self.pb[1]

        def trc(e):
            for c in range(8):
                ins = e.transpose(ptb[:, c * 128:(c + 1) * 128], cat[:, c * 128:(c + 1) * 128], self.idb[:])
            return ins
        S.op(S.pe, trc, reads=[cat, self.idb], writes=[ptb])
        S.op(S.act, lambda e: e.copy(catT[:], ptb[:].rearrange("p (c t) -> p c t", c=8)), reads=[ptb], writes=[catT])
        for hf in range(2):
            po2 = self.ps[hf]

            def mmo(e, po2=po2, hf=hf):
                for k in range(8):
                    ins = e.matmul(po2[:], catT[:, k, :], wout[:, k, hf * 512:(hf + 1) * 512], start=(k == 0), stop=(k == 7))
                return ins
            S.op(S.pe, mmo, reads=[catT, wout], writes=[po2])
            S.op(S.dve, lambda e, po2=po2, hf=hf: e.tensor_tensor(z[:, hf * 512:(hf + 1) * 512], po2[:], g1[:, hf * 512:(hf + 1) * 512], ALU.mult),
                 reads=[po2, g1], writes=[z])
        if self.cfg.get("tap_ox"):
            if not hasattr(self, "_tox"):
                self._tox = self.tap("oxg", [T, D])
            S.dma(S.sp, self._tox[i * 128:(i + 1) * 128, :], z[:], z, reads=[z], is_output=True)
        S.op(S.dve, lambda e: e.scalar_tensor_tensor(z[:], x[:], ALPHA, z[:], ALU.mult, ALU.add), reads=[x, z], writes=[z])
        self.layernorm(z, o, lng, lnb, sm)
        S.dma(S.pool, self.Xt[i].t, o[:], o, reads=[o], writes=[self.Xt[i]])


    def mixer_odd(self, l, last):
        S, I = self.S, self.I
        j = l // 2
        QTv = self.QT[0:512, :].rearrange("(f p) t -> p f t", p=128)
        KTv = self.KT[0:512, :].rearrange("(f p) t -> p f t", p=128)
        QSCALE = 128.0 ** -0.5
        with S.scope():
            win = S.sbuf("gwin", [128, 8, 3104], BF16)
            wv = I["gla_w_in"][j].rearrange("(c p) n -> p c n", p=128)
            for b5 in range(6):
                S.dma(S.pool, win[:, :, b5 * 512:(b5 + 1) * 512], wv[:, :, b5 * 512:(b5 + 1) * 512], win, writes=[win])
            S.dma(S.pool, win[:, :, 3072:3104], wv[:, :, 3072:3104], win, writes=[win])
            s1 = S.sbuf("s1", [128, D], F32)
            h1 = S.sbuf("h1", [128, D], F32)
            xb = [S.sbuf(f"xb{i}", [128, D], F32) for i in range(2)]
            h32 = S.sbuf("h32", [128, D], F32)
            hb = [S.sbuf(f"hb{i}", [128, D], BF16) for i in range(2)]
            hTs = [S.sbuf(f"hT{i}", [128, 8, 512], BF16) for i in range(2)]
            qk32 = S.sbuf("qk32", [128, D], F32)
            tA = S.sbuf("tA", [128, 512], F32)
            tB = S.sbuf("tB", [128, 512], F32)
            qkb = [S.sbuf(f"qkb{i}", [128, D], BF16) for i in range(2)]
            qkT = [S.sbuf(f"qkT{i}", [128, 8, 128], BF16) for i in range(2)]
            vrs = [S.sbuf(f"vr{i}", [128, 2 * D], BF16) for i in range(2)]
            css = [S.sbuf(f"cs{i}", [128, 2, 64], F32) for i in range(2)]
            gsb = [S.sbuf(f"gsb{i}", [16, 2, 512], F32) for i in range(2)]
            kind = None
            nx = 0
            for s_ in range(9):
                tiles = list(range(4 * s_, 4 * s_ + 4)) if s_ < 8 else [32, 33]
                N = 128 * len(tiles)
                c0 = tiles[0] * 128
                hT = hTs[s_ % 2]
                k2 = 0 if s_ < 8 else 1
                if k2 != kind:
                    kind = k2
                    self.load_mod(s1, l, kind, 1)
                    self.load_mod(h1, l, kind, 0)
                for tt, i in enumerate(tiles):
                    x = xb[nx % 2]
                    hbb = hb[nx % 2]
                    nx += 1
                    S.dma(S.sp, x[:], self.xsrc(i).t, x, reads=[self.xsrc(i)], writes=[x])
                    S.op(S.dve, lambda e, x=x: e.tensor_tensor(h32[:], x[:], s1[:], ALU.mult), reads=[x, s1], writes=[h32])
                    S.op(S.dve, lambda e, hbb=hbb: e.tensor_tensor(hbb[:], h32[:], h1[:], ALU.add), reads=[h32, h1], writes=[hbb])
                    pt = self.pb[0]

                    def tr(e, hbb=hbb, pt=pt):
                        for c in range(8):
                            ins = e.transpose(pt[:, c * 128:(c + 1) * 128], hbb[:, c * 128:(c + 1) * 128], self.idb[:])
                        return ins
                    S.op(S.pe, tr, reads=[hbb, self.idb], writes=[pt])
                    S.op(S.act, lambda e, pt=pt, hT=hT, tt=tt: e.copy(hT[:, :, tt * 128:(tt + 1) * 128],
                                                                      pt[:].rearrange("p (c t) -> p c t", c=8)),
                         reads=[pt], writes=[hT])
                gs_ = gsb[s_ % 2]
                for z in range(2):
                    pgz = self.ps[4 + z]

                    def mmg(e, z=z, pgz=pgz, hT=hT, N=N):
                        for k in range(8):
                            ins = e.matmul(pgz[0:16, 0:N], win[:, k, 3072 + 16 * z:3088 + 16 * z], hT[:, k, 0:N], start=(k == 0), stop=(k == 7))
                        return ins
                    S.op(S.pe, mmg, reads=[hT, win], writes=[pgz])
                    S.op(S.act, lambda e, z=z, pgz=pgz, gs_=gs_, N=N: e.copy(gs_[:, z, 0:N], pgz[0:16, 0:N]), reads=[pgz], writes=[gs_])
                S.dma(S.sp, self.GS[:, :, c0:c0 + N].rearrange("z r t -> r z t"), gs_[:, :, 0:N], gs_, reads=[gs_], writes=[self.GSs[s_]])
                for tt, i in enumerate(tiles):
                    isctx = i >= NTL
                    tc_ = slice(tt * 128, (tt + 1) * 128)
                    vr = vrs[i % 2]
                    qb = qkb[i % 2]
                    qT_ = qkT[i % 2]
                    for blk in range(6):
                        pz = self.ps[blk % 4]

                        def mmt(e, blk=blk, pz=pz, hT=hT, tc_=tc_):
                            for k in range(8):
                                ins = e.matmul(pz[:], hT[:, k, tc_], win[:, k, blk * 512:(blk + 1) * 512], start=(k == 0), stop=(k == 7))
                            return ins
                        S.op(S.pe, mmt, reads=[hT, win], writes=[pz])
                        if blk == 0:
                            S.op(S.act, lambda e, pz=pz: e.mul(qk32[:, 0:512], pz[:], QSCALE), reads=[pz], writes=[qk32])
                        elif blk == 1:
                            S.op(S.act, lambda e, pz=pz: e.copy(qk32[:, 512:1024], pz[:]), reads=[pz], writes=[qk32])
                        elif blk % 2 == 0:
                            S.op(S.dve, lambda e, pz=pz, blk=blk, vr=vr: e.tensor_copy(vr[:, (blk - 2) * 512:(blk - 1) * 512], pz[:]), reads=[pz], writes=[vr])
                        else:
                            S.op(S.act, lambda e, pz=pz, blk=blk, vr=vr: e.copy(vr[:, (blk - 2) * 512:(blk - 1) * 512], pz[:]), reads=[pz], writes=[vr])
                    S.dma(S.sp, self.V[i * 128:(i + 1) * 128, :], vr[:, 0:D], vr, reads=[vr], writes=[self.Vt[i]])
                    S.dma(S.sp, self.U[i * 128:(i + 1) * 128, :], vr[:, D:2 * D], vr, reads=[vr], writes=[self.Ut[i]])
                    if isctx:
                        S.op(S.dve, lambda e, qb=qb: e.tensor_copy(qb[:], qk32[:]), reads=[qk32], writes=[qb])
                    else:
                        cs = css[i % 2]
                        S.dma(S.sp, cs[:], I["ropecs"][i * 128:(i + 1) * 128], cs, writes=[cs])
                        q4 = qk32[:].rearrange("p (h two d) -> p h two d", two=2, d=64)
                        o4 = qb[:].rearrange("p (h two d) -> p h two d", two=2, d=64)
                        x1, x2 = q4[:, :, 0, :], q4[:, :, 1, :]
                        cosb = cs[:, 0:1, :].to_broadcast([128, 8, 64])
                        sinb = cs[:, 1:2, :].to_broadcast([128, 8, 64])
                        a3 = tA[:].rearrange("p (h d) -> p h d", d=64)
                        b3 = tB[:].rearrange("p (h d) -> p h d", d=64)
                        S.op(S.dve, lambda e: e.tensor_tensor(a3, x1, cosb, ALU.mult), reads=[qk32, cs], writes=[tA])
                        S.op(S.pool, lambda e: e.tensor_tensor(b3, x2, sinb, ALU.mult), reads=[qk32, cs], writes=[tB])
                        S.op(S.dve, lambda e, o4=o4: e.tensor_tensor(o4[:, :, 0, :], a3, b3, ALU.subtract), reads=[tA, tB], writes=[qb])
                        S.op(S.dve, lambda e: e.tensor_tensor(a3, x1, sinb, ALU.mult), reads=[qk32, cs], writes=[tA])
                        S.op(S.pool, lambda e: e.tensor_tensor(b3, x2, cosb, ALU.mult), reads=[qk32, cs], writes=[tB])
                        S.op(S.dve, lambda e, o4=o4: e.tensor_tensor(o4[:, :, 1, :], a3, b3, ALU.add), reads=[tA, tB], writes=[qb])
                    pt = self.pb[1]

                    def trq(e, qb=qb, pt=pt):
                        for c in range(8):
                            ins = e.transpose(pt[:, c * 128:(c + 1) * 128], qb[:, c * 128:(c + 1) * 128], self.idb[:])
                        return ins
                    S.op(S.pe, trq, reads=[qb, self.idb], writes=[pt])
                    S.op(S.dve, lambda e, pt=pt, qT_=qT_: e.tensor_copy(qT_[:], pt[:].rearrange("p (c t) -> p c t", c=8)), reads=[pt], writes=[qT_])
                    S.dma(S.sp, QTv[:, :, i * 128:(i + 1) * 128], qT_[:, 0:4, :], qT_, reads=[qT_], writes=[self.QTs[s_]])
                    S.dma(S.sp, KTv[:, :, i * 128:(i + 1) * 128], qT_[:, 4:8, :], qT_, reads=[qT_], writes=[self.KTs[s_]])
        with S.scope():
            wg = S.sbuf("wg", [16, 2, 512], F32)
            negb = S.sbuf("negb", [128, 2, 4], F32)
            msk = S.sbuf("msk", [128, 2, 128], F32)
            ones = S.sbuf("ones", [128, 128], F32)
            S.dma(S.sp, wg[:], I["gla_w_gate"][j].rearrange("z r e -> r z e"), wg, writes=[wg])
            S.dma(S.sp, negb[:], I["gla_bgL"][j], negb, writes=[negb])
            S.op(S.dve, lambda e: e.tensor_scalar_mul(negb[:], negb[:], -1.0), reads=[negb], writes=[negb])
            S.dma(S.sp, msk[:], I["trimask"][0:2].rearrange("z s t -> s z t"), msk, writes=[msk])
            S.op(S.dve, lambda e: e.memset(ones[:], 1.0), writes=[ones])
            S32 = [[S.sbuf(f"S32_{z}{h}", [128, 256], F32) for h in range(4)] for z in range(2)]
            Sb = [[S.sbuf(f"Sb_{z}{h}", [128, 256], BF16) for h in range(4)] for z in range(2)]
            for z in range(2):
                for h in range(4):
                    S.op(S.dve, lambda e, z=z, h=h: e.memset(S32[z][h][:], 0.0), writes=[S32[z][h]])
                    S.op(S.pool, lambda e, z=z, h=h: e.memset(Sb[z][h][:], 0.0), writes=[Sb[z][h]])
            qts = [[S.sbuf(f"gq{z}{i}", [128, 4, 128], BF16) for i in range(2)] for z in range(2)]
            kts = [[S.sbuf(f"gk{z}{i}", [128, 4, 128], BF16) for i in range(2)] for z in range(2)]
            vts = [[S.sbuf(f"gv{z}{i}", [128, D], BF16) for i in range(2)] for z in range(2)]
            gts = [[S.sbuf(f"gg{z}{i}", [16, 128], F32) for i in range(2)] for z in range(2)]
            osb = [[S.sbuf(f"go{z}{i}", [128, D], F32) for i in range(2)] for z in range(2)]
            W = {}
            for a in range(4):
                for nm, sh, dt in (("e1", [128, 128], F32), ("sp", [128, 128], F32), ("cum", [128, 128], F32),
                                   ("Eq", [128, 128], F32), ("Ek", [128, 128], F32), ("El", [128, 128], F32),
                                   ("sm", [128, 4], F32), ("qe", [128, 128], BF16), ("ke", [128, 128], BF16),
                                   ("kl", [128, 128], BF16), ("klT", [128, 128], BF16), ("attm", [128, 128], BF16)):
                    W[(nm, a)] = S.sbuf(f"g{nm}{a}", sh, dt)
            order = [[32, 33] + list(range(32)), [33, 32] + list(range(31, -1, -1))]
            na = 0
            def g2_loads(step):
                for z in range(2):
                    i = order[z][step]
                    sl = step % 2
                    st_i = i // 4 if i < NTL else 8
                    cs_ = slice(i * 128, (i + 1) * 128)
                    S.dma(S.sp, qts[z][sl][:], QTv[:, :, cs_], qts[z][sl], reads=[self.QTs[st_i]], writes=[qts[z][sl]])
                    S.dma(S.sp, kts[z][sl][:], KTv[:, :, cs_], kts[z][sl], reads=[self.KTs[st_i]], writes=[kts[z][sl]])
                    S.dma(S.sp, vts[z][sl][:], self.V[cs_, :], vts[z][sl], reads=[self.Vt[i]], writes=[vts[z][sl]])
                    S.dma(S.sp, gts[z][sl][:], self.GS[z, :, cs_], gts[z][sl], reads=[self.GSs[st_i]], writes=[gts[z][sl]])
            ust = {}

            def unit_a(step, h, z):
                nonlocal na
                i = order[z][step]
                sl = step % 2
                a = na % 4
                na += 1
                qt, kt, vt, gt, ot = qts[z][sl], kts[z][sl], vts[z][sl], gts[z][sl], osb[z][sl]
                e1, sp, cum, Eq, Ek, El, sm = (W[(n_, a)] for n_ in ("e1", "sp", "cum", "Eq", "Ek", "El", "sm"))
                qe, ke, kl, klT, attm = (W[(n_, a)] for n_ in ("qe", "ke", "kl", "klT", "attm"))
                if a < 3:
                    bA, bB = self.ps[a * 2], self.ps[a * 2 + 1]
                    vA, vB, vT = bA.t[:], bB.t[:], bA.t[:].bitcast(BF16)
                else:
                    bA, bB = self.pb[0], self.pb[1]
                    vA, vB, vT = bA.t[:].bitcast(F32), bB.t[:].bitcast(F32), bA.t[:]
                pla = patt = ptb = bA
                pos = bB
                s32, sb_ = S32[z][h], Sb[z][h]
                S.op(S.pe, lambda e, z=z, h=h, gt=gt, vA=vA: e.matmul(vA[:, 0:128], wg[:, z, h * 128:(h + 1) * 128], gt[:], start=True, stop=True),
                     reads=[wg, gt], writes=[pla])
                S.op(S.act, lambda e, e1=e1, vA=vA, z=z, h=h: e.activation(e1[:], vA[:, 0:128], AF.Exp, bias=negb[:, z, h:h + 1], scale=-1.0),
                     reads=[pla, negb], writes=[e1])
                S.op(S.act, lambda e, e1=e1, sp=sp: e.activation(sp[:], e1[:], AF.Ln, bias=1.0, scale=1.0), reads=[e1], writes=[sp])
                S.op(S.dve, lambda e, cum=cum, sp=sp: e.tensor_tensor_scan(cum[:], ones[:], sp[:], 0.0, ALU.mult, ALU.add), reads=[ones, sp], writes=[cum])
                S.op(S.dve, lambda e, sm=sm, cum=cum: e.tensor_copy(sm[:, 0:1], cum[:, 127:128]), reads=[cum], writes=[sm])
                S.op(S.dve, lambda e, sm=sm: e.tensor_scalar_mul(sm[:, 1:2], sm[:, 0:1], -1.0 / 16.0), reads=[sm], writes=[sm])
                if z == 1:
                    S.op(S.dve, lambda e, cum=cum, sm=sm: e.tensor_scalar(cum[:], cum[:], -1.0, sm[:, 0:1], ALU.mult, ALU.add), reads=[cum, sm], writes=[cum])
                    S.op(S.dve, lambda e, cum=cum, sp=sp: e.tensor_tensor(cum[:], cum[:], sp[:], ALU.add), reads=[cum, sp], writes=[cum])
                S.op(S.act, lambda e, Eq=Eq, cum=cum: e.activation(Eq[:], cum[:], AF.Exp, scale=-1.0 / 16.0), reads=[cum], writes=[Eq])
                S.op(S.act, lambda e, Ek=Ek, cum=cum: e.activation(Ek[:], cum[:], AF.Exp, scale=1.0 / 16.0), reads=[cum], writes=[Ek])
                S.op(S.act, lambda e, El=El, cum=cum, sm=sm: e.activation(El[:], cum[:], AF.Exp, bias=sm[:, 1:2], scale=1.0 / 16.0), reads=[cum, sm], writes=[El])
                S.op(S.act, lambda e, sm=sm: e.activation(sm[:, 2:3], sm[:, 0:1], AF.Exp, scale=-1.0 / 16.0), reads=[sm], writes=[sm])
                S.op(S.dve, lambda e, qe=qe, qt=qt, Eq=Eq, h=h: e.tensor_tensor(qe[:], qt[:, h, :], Eq[:], ALU.mult), reads=[qt, Eq], writes=[qe])
                S.op(S.pool, lambda e, ke=ke, kt=kt, Ek=Ek, h=h: e.tensor_tensor(ke[:], kt[:, h, :], Ek[:], ALU.mult), reads=[kt, Ek], writes=[ke])
                S.op(S.pool, lambda e, kl=kl, kt=kt, El=El, h=h: e.tensor_tensor(kl[:], kt[:, h, :], El[:], ALU.mult), reads=[kt, El], writes=[kl])
                ust[(step, h, z)] = dict(kl=kl, vT=vT, klT=klT, ke=ke, qe=qe, vA=vA, vB=vB, attm=attm, vt=vt, sb_=sb_, ot=ot, s32=s32, sm=sm, ptb=ptb, pos=pos, patt=patt, pla=pla, bA=bA, bB=bB)

            def unit_b(step, h, z):
                L = ust.pop((step, h, z))
                kl, vT, klT, ke, qe, vA, vB, attm, vt, sb_, ot, s32, sm, ptb, pos, patt, pla, bA, bB = (L['kl'], L['vT'], L['klT'], L['ke'], L['qe'], L['vA'], L['vB'], L['attm'], L['vt'], L['sb_'], L['ot'], L['s32'], L['sm'], L['ptb'], L['pos'], L['patt'], L['pla'], L['bA'], L['bB'])
                S.op(S.pe, lambda e, kl=kl, vT=vT: e.transpose(vT[:, 512:640], kl[:], self.idb[:]), reads=[kl, self.idb], writes=[ptb])
                S.op(S.act, lambda e, klT=klT, vT=vT: e.copy(klT[:], vT[:, 512:640]), reads=[ptb], writes=[klT])
                S.op(S.pe, lambda e, ke=ke, qe=qe, vA=vA: e.matmul(vA[:, 128:256], ke[:], qe[:], start=True, stop=True), reads=[ke, qe], writes=[patt])
                S.op(S.dve, lambda e, attm=attm, vA=vA, z=z: e.tensor_tensor(attm[:], vA[:, 128:256], msk[:, z, :], ALU.mult), reads=[patt, msk], writes=[attm])
                vh = slice(h * 256, (h + 1) * 256)

                def mmo(e, attm=attm, vt=vt, qe=qe, sb_=sb_, vB=vB, vh=vh):
                    e.matmul(vB[:, 0:256], attm[:], vt[:, vh], start=True, stop=False)
                    return e.matmul(vB[:, 0:256], qe[:], sb_[:], start=False, stop=True)
                S.op(S.pe, mmo, reads=[attm, vt, qe, sb_], writes=[pos])
                S.op(S.act, lambda e, ot=ot, vB=vB, vh=vh: e.copy(ot[:, vh], vB[:, 0:256]), reads=[pos], writes=[ot])
                S.op(S.pe, lambda e, klT=klT, vt=vt, vB=vB, vh=vh: e.matmul(vB[:, 256:512], klT[:], vt[:, vh], start=True, stop=True),
                     reads=[klT, vt], writes=[pos])
                S.op(S.dve, lambda e, s32=s32, sm=sm, vB=vB: e.scalar_tensor_tensor(s32[:], s32[:], sm[:, 2:3], vB[:, 256:512], ALU.mult, ALU.add),
                     reads=[s32, sm, pos], writes=[s32])
                S.op(S.act, lambda e, sb_=sb_, s32=s32: e.copy(sb_[:], s32[:]), reads=[s32], writes=[sb_])

            units = [(st_, h_, z_) for st_ in range(NT) for h_ in range(4) for z_ in range(2)]
            g2_loads(0)
            unit_a(*units[0])
            for n_, (step, h, z) in enumerate(units):
                if h == 0 and z == 0 and step + 1 < NT:
                    g2_loads(step + 1)
                if n_ + 1 < len(units):
                    unit_a(*units[n_ + 1])
                unit_b(step, h, z)
                if h == 3 and z == 1:
                    for z in range(2):
                        i = order[z][step]
                        ot = osb[z][step % 2]
                        S.dma(S.sp, self.OF[z, i * 128:(i + 1) * 128, :], ot[:], ot, reads=[ot], writes=[self.OFt[z][i]])

        with S.scope():
            wout = S.sbuf("gwout", [128, 8, D], BF16)
            S.dma(S.pool, wout[:], I["gla_w_out"][j].rearrange("(c p) n -> p c n", p=128), wout, writes=[wout])
            ngb = S.sbuf("ngb", [128, 256], F32)
            g1 = S.sbuf("g1", [128, D], F32)
            lng = S.sbuf("lng", [128, D], F32)
            lnb = S.sbuf("lnb", [128, D], F32)
            self.load_vec(ngb, I["gla_norm_g"][j])
            self.load_vec(lng, I["ln_g"][l, 0])
            self.load_vec(lnb, I["ln_b"][l, 0])
            xb = [S.sbuf(f"xb{i}", [128, D], F32) for i in range(2)]
            zb = [S.sbuf(f"zb{i}", [128, D], F32) for i in range(2)]
            ob = [S.sbuf(f"ob{i}", [128, D], F32) for i in range(2)]
            ofb = [S.sbuf(f"ofb{i}", [128, D], F32) for i in range(2)]
            obb = [S.sbuf(f"obb{i}", [128, D], F32) for i in range(2)]
            rb = [S.sbuf(f"rb{i}", [128, D], BF16) for i in range(2)]
            sr = S.sbuf("sr", [128, D], F32)
            junk = S.sbuf("junk", [128, 256], F32)
            sms = [S.sbuf(f"sml{i}", [128, 16], F32) for i in range(2)]
            st8 = [S.sbuf(f"st8{i}", [128, 8], F32) for i in range(2)]
            cats = [S.sbuf(f"cat{i}", [128, D], BF16) for i in range(2)]
            catT = S.sbuf("catT", [128, 8, 128], BF16)
            ntile = NTL if last else NT
            kind = None
            for i in range(ntile):
                k2 = 1 if i >= NTL else 0
                if k2 != kind:
                    kind = k2
                    self.load_mod(g1, l, kind, 2)
                x, z_, o, sm = xb[i % 2], zb[i % 2], ob[i % 2], sms[i % 2]
                of_, ob_, r_, st, cat = ofb[i % 2], obb[i % 2], rb[i % 2], st8[i % 2], cats[i % 2]
                S.dma(S.sp, x[:], self.xsrc(i).t, x, reads=[self.xsrc(i)], writes=[x])
                S.dma(S.sp, of_[:], self.OF[0, i * 128:(i + 1) * 128, :], of_, reads=[self.OFt[0][i]], writes=[of_])
                S.dma(S.sp, ob_[:], self.OF[1, i * 128:(i + 1) * 128, :], ob_, reads=[self.OFt[1][i]], writes=[ob_])
                S.dma(S.sp, r_[:], self.U[i * 128:(i + 1) * 128, :], r_, reads=[self.Ut[i]], writes=[r_])
                S.op(S.dve, lambda e, of_=of_, ob_=ob_: e.tensor_tensor(of_[:], of_[:], ob_[:], ALU.add), reads=[of_, ob_], writes=[of_])
                for h in range(4):
                    S.op(S.act, lambda e, of_=of_, st=st, h=h: e.activation(junk[:], of_[:, h * 256:(h + 1) * 256], AF.Square, accum_out=st[:, h:h + 1]),
                         reads=[of_], writes=[junk, st])
                S.op(S.dve, lambda e, st=st: e.tensor_scalar(st[:, 4:8], st[:, 0:4], 1.0 / 256.0, 1e-6, ALU.mult, ALU.add), reads=[st], writes=[st])
                S.op(S.act, lambda e, st=st: e.sqrt(st[:, 4:8], st[:, 4:8]), reads=[st], writes=[st])
                S.op(S.dve, lambda e, st=st: e.reciprocal(st[:, 4:8], st[:, 4:8]), reads=[st], writes=[st])
                S.op(S.act, lambda e, r_=r_: e.activation(sr[:], r_[:], AF.Silu), reads=[r_], writes=[sr])
                for h in range(4):
                    hs = slice(h * 256, (h + 1) * 256)
                    S.op(S.dve, lambda e, of_=of_, st=st, h=h, hs=hs: e.scalar_tensor_tensor(of_[:, hs], of_[:, hs], st[:, 4 + h:5 + h], ngb[:], ALU.mult, ALU.mult),
                         reads=[of_, st, ngb], writes=[of_])
                S.op(S.dve, lambda e, of_=of_, cat=cat: e.tensor_tensor(cat[:], of_[:], sr[:], ALU.mult), reads=[of_, sr], writes=[cat])
                self.out_proj_ln(l, i, cat, catT, wout, g1, x, z_, o, sm, lng, lnb)
        self.src_is_input = False

    def build(self):
        cfg = self.cfg
        layers = cfg.get("layers", list(range(DEPTH)))
        self.adaln(layers)
        for l in layers:
            last = l == DEPTH - 1
            if cfg.get("mixer", True):
                if l % 2 == 0:
                    self.mixer_even(l, last)
                else:
                    self.mixer_odd(l, last)
            if cfg.get("ffn", True):
                self.ffn(l, last)
        if cfg.get("dump_mod"):
            S = self.S
            tm = self.tap("moddump", [DEPTH, 2, 6 * D])
            with S.scope():
                mb = S.sbuf("mdump", [2, 6 * D], F32)
                for l in layers:
                    S.dma(S.sp, mb[:], self.MOD[l], mb, reads=[self.MODb[l]], writes=[mb])
                    S.dma(S.sp, tm[l], mb[:], mb, reads=[mb], is_output=True)
        if cfg.get("dump_x"):
            S = self.S
            tx = self.tap("xdump", [T, D])
            with S.scope():
                xb = [S.sbuf(f"dx{i}", [128, D], F32) for i in range(2)]
                for i in range(NT):
                    b = xb[i % 2]
                    S.dma(S.sp, b[:], self.Xt[i].t, b, reads=[self.Xt[i]], writes=[b])
                    S.dma(S.sp, tx[i * 128:(i + 1) * 128, :], b[:], b, reads=[b], is_output=True)
        self.S.finish()
        return self.nc


def na_bias_tables(rpb):
    out = np.full((rpb.shape[0], 5, 12, 128, 576), -30000.0, np.float32)
    qi = np.arange(128)
    ki = np.arange(576)
    for p, i in enumerate((0, 1, 2, 30, 31)):
        r0 = 2 * i
        kr0 = min(max(2 * i - 4, 0), 55)
        r = r0 + qi // 64
        col = qi % 64
        krow = kr0 + ki // 64
        kcol = ki % 64
        rs = np.clip(r - 4, 0, 56)
        ws = np.clip(col - 8, 0, 48)
        ok = ((krow[None, :] >= rs[:, None]) & (krow[None, :] < rs[:, None] + 8) &
              (kcol[None, :] >= ws[:, None]) & (kcol[None, :] < ws[:, None] + 16))
        drow = np.clip(krow[None, :] - r[:, None] + 7, 0, 14)
        dcol = np.clip(kcol[None, :] - col[:, None] + 15, 0, 30)
        g = rpb[:, :, drow, dcol]
        out[:, p] = np.where(ok[None, None], g, np.float32(-30000.0))
    return out


def pool_matrices():
    A = np.zeros((4, 5, 128, 128), np.float64)
    Tseq = 128 * 4
    for g, w in enumerate((2, 4, 8, 16)):
        for var, (ti, nb) in enumerate(((1, -1), (1, 0), (1, 1), (0, 0), (3, 0))):
            for tl in range(128):
                t = ti * 128 + tl
                lo = min(max(t - w // 2, 0), Tseq)
                hi = min(max(t - w // 2 + w, 0), Tseq)
                for sg in range(lo, hi):
                    sl = sg - (ti + nb) * 128
                    if 0 <= sl < 128:
                        A[g, var, sl, tl] += 1.0 / (hi - lo)
                if nb == 0:
                    A[g, var, tl, tl] -= 1.0
    return np.ascontiguousarray(A.reshape(20, 128, 128).astype(np.float32))


def rope_tables():
    t = np.arange(TL)
    row = (t // GW).astype(np.float32)
    col = (t % GW).astype(np.float32)
    nf = 32
    freqs = (np.float32(10000.0) ** (-np.arange(nf, dtype=np.float32) / nf)).astype(np.float32)
    ang = np.concatenate([row[:, None] * freqs, col[:, None] * freqs], axis=-1).astype(np.float32)
    return np.ascontiguousarray(np.stack([np.cos(ang), np.sin(ang)], axis=1).astype(np.float32))


def tri_masks():
    s_ = np.arange(128)[:, None]
    t_ = np.arange(128)[None, :]
    return np.ascontiguousarray(np.stack([(s_ <= t_), (s_ >= t_), (s_ < t_)], 0).astype(np.float32))


def host_inputs(inputs, cores):
    f = lambda a: np.ascontiguousarray(np.asarray(a, dtype=np.float32))
    shared = {
        "ada_w": f(inputs["ada_w"]), "ada_b": f(inputs["ada_b"]),
        "ln_g": f(inputs["ln_g"]), "ln_b": f(inputs["ln_b"]),
        "router_w": f(inputs["router_w"]), "router_b": f(inputs["router_b"]),
        "exp_w1": f(inputs["exp_w1"]), "exp_w2": f(inputs["exp_w2"]), "exp_b2": f(inputs["exp_b2"]),
        "exp_b1L": f(np.asarray(inputs["exp_b1"]).reshape(DEPTH, NE, 16, 128).transpose(0, 3, 1, 2)),
        "ident": np.eye(128, dtype=np.float32),
        "ab_w_in": f(inputs["ab_w_in"]), "ab_pool_w": f(inputs["ab_pool_w"]), "ab_pool_scale": f(inputs["ab_pool_scale"]),
        "ab_w_out": f(inputs["ab_w_out"]), "nabias": na_bias_tables(np.asarray(inputs["ab_rpb"], dtype=np.float32)),
        "poolA": pool_matrices(),
        "gla_w_in": f(inputs["gla_w_in"]), "gla_w_gate": f(inputs["gla_w_gate"]),
        "gla_bgL": f(np.asarray(inputs["gla_b_gate"]).reshape(2, 2, 4, 128).transpose(0, 3, 1, 2)),
        "gla_norm_g": f(inputs["gla_norm_g"]), "gla_w_out": f(inputs["gla_w_out"]),
        "ropecs": rope_tables(), "trimask": tri_masks(),
        "blkstart": (np.arange(128, dtype=np.float32) * BLK).reshape(128, 1),
        "rowbase": np.ascontiguousarray(np.concatenate([np.arange(8)[None, :] * 128 + np.arange(128)[:, None],
                                                        np.arange(128)[:, None]], axis=1).astype(np.float32)),
        "exp_b1T": f(np.asarray(inputs["exp_b1"]).reshape(DEPTH, NE, 16, 128).transpose(0, 1, 3, 2).reshape(DEPTH, NE * 128, 16)),
    }
    maps = []
    cc = np.asarray(inputs["c_ctx"], dtype=np.float32).reshape(8, 128).T
    for b in cores:
        m = dict(shared)
        m["xc"] = f(np.concatenate([inputs["x"][b], inputs["ctx"][b]], axis=0))
        cb = np.asarray(inputs["c"][b], dtype=np.float32).reshape(8, 128).T
        m["cvec"] = f(np.stack([cb, cc], axis=-1))
        maps.append(m)
    return maps


def kernel(**inputs):
    prog = Prog({})
    nc = prog.build()
    maps = host_inputs(inputs, list(range(8)))
    res = run_bass_kernel_spmd(nc, maps, core_ids=list(range(8)))
    out = np.stack([np.asarray(r["out"]) for r in res.results], axis=0)
    return out.astype(np.float32)
```
